# Optimizing a Trainium2 kernel written in Bass

```python
import jax
import jax.numpy as jnp
from jax import lax
import numpy as np

D_MODEL = 1024
BATCH = 1
SEQ = 16384
DEPTH = 2

EPS = 1e-6
MLA_HEADS = 8
MLA_Q_LORA = 256
MLA_KV_LORA = 128
MLA_NOPE = 128
MLA_ROPE = 64
MLA_V = 128
MLA_QK = MLA_NOPE + MLA_ROPE
MLA_IN = MLA_Q_LORA + MLA_KV_LORA + MLA_ROPE
ROPE_THETA = 10000.0
Q_BLOCK = 128
MLSTM_HEADS = 8
MLSTM_V = D_MODEL // MLSTM_HEADS
MLSTM_QK = MLSTM_V // 2
MLSTM_QK_TOT = MLSTM_HEADS * MLSTM_QK
MLSTM_V_TOT = MLSTM_HEADS * MLSTM_V
MLSTM_IN = 2 * MLSTM_QK_TOT + MLSTM_V_TOT + D_MODEL + 2 * MLSTM_HEADS
MLSTM_CHUNK = 64
CONV_W = 4
GATE_CAP = 15.0
D_FF = 7 * D_MODEL // 2
N_EXPERTS = 8
TOP_K = 2
N_EVEN = (DEPTH + 1) // 2
N_ODD = DEPTH // 2

kernel_name = 'hybrid_mla_mlstm_moe_trunk'


def rmsnorm(x, g):
    xf = x.astype(jnp.float32)
    y = xf * lax.rsqrt(jnp.mean(xf * xf, axis=-1, keepdims=True) + EPS)
    return (y * g.astype(jnp.float32)).astype(x.dtype)


def rope_tables(positions):
    inv = 1.0 / (ROPE_THETA ** (jnp.arange(0, MLA_ROPE, 2, dtype=jnp.float32) / MLA_ROPE))
    ang = positions.astype(jnp.float32)[..., None] * inv
    return jnp.cos(ang), jnp.sin(ang)


def apply_rope(t, cos, sin):
    tf = t.astype(jnp.float32)
    t1, t2 = jnp.split(tf, 2, axis=-1)
    return jnp.concatenate([t1 * cos - t2 * sin, t1 * sin + t2 * cos], axis=-1).astype(t.dtype)


def causal_block_attention(q, k, v):
    B, S, H, DQ = q.shape
    DV = v.shape[-1]
    nb = S // Q_BLOCK
    scale = DQ ** -0.5
    kf = k.astype(jnp.float32)
    vf = v.astype(jnp.float32)
    qb = q.astype(jnp.float32).reshape(B, nb, Q_BLOCK, H, DQ).transpose(1, 0, 2, 3, 4)
    starts = jnp.arange(nb, dtype=jnp.int32) * Q_BLOCK
    kpos = jnp.arange(S, dtype=jnp.int32)

    def one_block(args):
        q_blk, start = args
        s = jnp.einsum('bqhd,bkhd->bhqk', q_blk, kf) * scale
        qpos = start + jnp.arange(Q_BLOCK, dtype=jnp.int32)
        s = jnp.where(kpos[None, :] <= qpos[:, None], s, -jnp.inf)
        p = jax.nn.softmax(s, axis=-1)
        return jnp.einsum('bhqk,bkhd->bqhd', p, vf)

    out = lax.map(one_block, (qb, starts))
    return out.transpose(1, 0, 2, 3, 4).reshape(B, S, H, DV)


def mla_mixer(xn, cos, sin, w_in, q_norm, w_qb, kv_norm, w_kvb, w_out):
    B, S, _ = xn.shape
    proj = xn @ w_in
    c_q = proj[..., :MLA_Q_LORA]
    c_kv = proj[..., MLA_Q_LORA:MLA_Q_LORA + MLA_KV_LORA]
    k_rope = proj[..., MLA_Q_LORA + MLA_KV_LORA:]
    q = (rmsnorm(c_q, q_norm) @ w_qb).reshape(B, S, MLA_HEADS, MLA_QK)
    kv = (rmsnorm(c_kv, kv_norm) @ w_kvb).reshape(B, S, MLA_HEADS, MLA_NOPE + MLA_V)
    q_rot = apply_rope(q[..., MLA_NOPE:], cos[:, :, None, :], sin[:, :, None, :])
    k_rot = apply_rope(k_rope, cos, sin)[:, :, None, :]
    qh = jnp.concatenate([q[..., :MLA_NOPE], q_rot], axis=-1)
    kh = jnp.concatenate([kv[..., :MLA_NOPE], jnp.broadcast_to(k_rot, (B, S, MLA_HEADS, MLA_ROPE))], axis=-1)
    vh = kv[..., MLA_NOPE:]
    o = causal_block_attention(qh, kh, vh)
    return o.reshape(B, S, MLA_HEADS * MLA_V).astype(xn.dtype) @ w_out


def mlstm_chunkwise(q, k, v, i_pre, log_f):
    B, S, H, DK = q.shape
    DV = v.shape[-1]
    L = MLSTM_CHUNK
    NC = S // L
    f32 = jnp.float32
    q = q.astype(f32).transpose(0, 2, 1, 3).reshape(B, H, NC, L, DK) * (DK ** -0.5)
    k = k.astype(f32).transpose(0, 2, 1, 3).reshape(B, H, NC, L, DK)
    v = v.astype(f32).transpose(0, 2, 1, 3).reshape(B, H, NC, L, DV)
    ig = i_pre.astype(f32).transpose(0, 2, 1).reshape(B, H, NC, L)
    lf = log_f.astype(f32).transpose(0, 2, 1).reshape(B, H, NC, L)
    b = jnp.cumsum(lf, axis=-1)
    b_last = b[..., -1]
    a = b_last[..., None] - b + ig
    m_loc = jnp.max(a, axis=-1)
    w = jnp.exp(a - m_loc[..., None])
    c_loc = jnp.einsum('bhcl,bhcld,bhcle->bhcde', w, k, v)
    n_loc = jnp.einsum('bhcl,bhcld->bhcd', w, k)

    def step(carry, inp):
        c, n, m = carry
        cl, nl, ml, bl = inp
        m_new = jnp.maximum(bl + m, ml)
        s_prev = jnp.exp(bl + m - m_new)
        s_loc = jnp.exp(ml - m_new)
        c_new = s_prev[..., None, None] * c + s_loc[..., None, None] * cl
        n_new = s_prev[..., None] * n + s_loc[..., None] * nl
        return (c_new, n_new, m_new), (c, n, m)

    init = (jnp.zeros((B, H, DK, DV), f32), jnp.zeros((B, H, DK), f32), jnp.zeros((B, H), f32))
    xs = (c_loc.transpose(2, 0, 1, 3, 4), n_loc.transpose(2, 0, 1, 3), m_loc.transpose(2, 0, 1), b_last.transpose(2, 0, 1))
    _, (c_prev, n_prev, m_prev) = lax.scan(step, init, xs)
    c_prev = c_prev.transpose(1, 2, 0, 3, 4)
    n_prev = n_prev.transpose(1, 2, 0, 3)
    m_prev = m_prev.transpose(1, 2, 0)
    causal = jnp.tril(jnp.ones((L, L), dtype=bool))
    d = jnp.where(causal, b[..., :, None] - b[..., None, :] + ig[..., None, :], -jnp.inf)
    inter = b + m_prev[..., None]
    m_t = jnp.maximum(inter, jnp.max(d, axis=-1))
    wts = jnp.exp(d - m_t[..., None]) * jnp.einsum('bhctd,bhcsd->bhcts', q, k)
    s_inter = jnp.exp(inter - m_t)
    num = jnp.einsum('bhcts,bhcse->bhcte', wts, v) + s_inter[..., None] * jnp.einsum('bhctd,bhcde->bhcte', q, c_prev)
    den = jnp.sum(wts, axis=-1) + s_inter * jnp.einsum('bhctd,bhcd->bhct', q, n_prev)
    h = num / jnp.maximum(jnp.abs(den), jnp.exp(-m_t))[..., None]
    return h.reshape(B, H, S, DV).transpose(0, 2, 1, 3)


def mlstm_mixer(xn, w_in, conv_w, conv_b, gate_b, head_norm, w_out):
    B, S, _ = xn.shape
    proj = xn @ w_in
    o1 = 2 * MLSTM_QK_TOT
    o2 = o1 + MLSTM_V_TOT
    o3 = o2 + D_MODEL
    qk_raw = proj[..., :o1]
    v = proj[..., o1:o2]
    o_pre = proj[..., o2:o3]
    g_pre = proj[..., o3:]
    qk = lax.conv_general_dilated(qk_raw, conv_w[:, None, :], window_strides=(1,), padding=[(CONV_W - 1, 0)],
                                  dimension_numbers=('NWC', 'WIO', 'NWC'), feature_group_count=o1)
    qk = jax.nn.silu(qk + conv_b)
    q = qk[..., :MLSTM_QK_TOT].reshape(B, S, MLSTM_HEADS, MLSTM_QK)
    k = qk[..., MLSTM_QK_TOT:].reshape(B, S, MLSTM_HEADS, MLSTM_QK)
    gates = g_pre.astype(jnp.float32) + gate_b.astype(jnp.float32)
    gates = GATE_CAP * jnp.tanh(gates / GATE_CAP)
    i_pre = gates[..., :MLSTM_HEADS]
    log_f = jax.nn.log_sigmoid(gates[..., MLSTM_HEADS:])
    h = mlstm_chunkwise(q, k, v.reshape(B, S, MLSTM_HEADS, MLSTM_V), i_pre, log_f)
    h = rmsnorm(h, head_norm.reshape(MLSTM_HEADS, MLSTM_V)).reshape(B, S, MLSTM_V_TOT)
    y = jax.nn.sigmoid(o_pre.astype(jnp.float32)) * h
    return y.astype(xn.dtype) @ w_out


def swiglu(h, wg, wu, wd):
    return (jax.nn.silu(h @ wg) * (h @ wu)) @ wd


def moe_swiglu(h, router, wg, wu, wd):
    logits = (h @ router).astype(jnp.float32)
    top_v, top_i = lax.top_k(logits, TOP_K)
    gates = jax.nn.softmax(top_v, axis=-1)
    combine = jnp.sum(jax.nn.one_hot(top_i, N_EXPERTS, dtype=jnp.float32) * gates[..., None], axis=-2)
    y = jnp.zeros(h.shape, jnp.float32)
    for e in range(N_EXPERTS):
        y = y + combine[..., e:e + 1] * swiglu(h, wg[e], wu[e], wd[e]).astype(jnp.float32)
    return y.astype(h.dtype)


def setup_inputs(seed: int = 0) -> dict:
    key = jax.random.key(seed)
    ks = jax.random.split(key, 32)
    f32 = jnp.float32

    def nrm(k, shape, scale):
        return jax.random.normal(k, shape, f32) * scale

    x = nrm(ks[0], (BATCH, SEQ, D_MODEL), 1.0)
    offs = jax.random.randint(ks[1], (BATCH, 1), 0, 1024, dtype=jnp.int32)
    positions = offs + jnp.arange(SEQ, dtype=jnp.int32)[None, :]
    norm_mix = 1.0 + nrm(ks[2], (DEPTH, D_MODEL), 0.02)
    norm_ffn = 1.0 + nrm(ks[3], (DEPTH, D_MODEL), 0.02)
    final_norm = 1.0 + nrm(ks[4], (D_MODEL,), 0.02)
    mla_w_in = nrm(ks[5], (N_EVEN, D_MODEL, MLA_IN), D_MODEL ** -0.5)
    mla_q_norm = 1.0 + nrm(ks[6], (N_EVEN, MLA_Q_LORA), 0.02)
    mla_w_qb = nrm(ks[7], (N_EVEN, MLA_Q_LORA, MLA_HEADS * MLA_QK), MLA_Q_LORA ** -0.5)
    mla_kv_norm = 1.0 + nrm(ks[8], (N_EVEN, MLA_KV_LORA), 0.02)
    mla_w_kvb = nrm(ks[9], (N_EVEN, MLA_KV_LORA, MLA_HEADS * (MLA_NOPE + MLA_V)), MLA_KV_LORA ** -0.5)
    mla_w_out = nrm(ks[10], (N_EVEN, MLA_HEADS * MLA_V, D_MODEL), (MLA_HEADS * MLA_V) ** -0.5)
    mlstm_w_in = nrm(ks[11], (N_ODD, D_MODEL, MLSTM_IN), D_MODEL ** -0.5)
    mlstm_conv_w = nrm(ks[12], (N_ODD, CONV_W, 2 * MLSTM_QK_TOT), CONV_W ** -0.5)
    mlstm_conv_b = nrm(ks[13], (N_ODD, 2 * MLSTM_QK_TOT), 0.02)
    i_bias = nrm(ks[14], (N_ODD, MLSTM_HEADS), 0.1)
    f_bias = jnp.linspace(3.0, 6.0, MLSTM_HEADS, dtype=f32)[None, :] + nrm(ks[15], (N_ODD, MLSTM_HEADS), 0.1)
    mlstm_gate_b = jnp.concatenate([i_bias, f_bias], axis=-1)
    mlstm_head_norm = 1.0 + nrm(ks[16], (N_ODD, MLSTM_V_TOT), 0.02)
    mlstm_w_out = nrm(ks[17], (N_ODD, MLSTM_V_TOT, D_MODEL), MLSTM_V_TOT ** -0.5)
    ffn_w_gate = nrm(ks[18], (N_EVEN, D_MODEL, D_FF), D_MODEL ** -0.5)
    ffn_w_up = nrm(ks[19], (N_EVEN, D_MODEL, D_FF), D_MODEL ** -0.5)
    ffn_w_down = nrm(ks[20], (N_EVEN, D_FF, D_MODEL), D_FF ** -0.5)
    moe_router = nrm(ks[21], (N_ODD, D_MODEL, N_EXPERTS), D_MODEL ** -0.5)
    moe_w_gate = nrm(ks[22], (N_ODD, N_EXPERTS, D_MODEL, D_FF), D_MODEL ** -0.5)
    moe_w_up = nrm(ks[23], (N_ODD, N_EXPERTS, D_MODEL, D_FF), D_MODEL ** -0.5)
    moe_w_down = nrm(ks[24], (N_ODD, N_EXPERTS, D_FF, D_MODEL), D_FF ** -0.5)
    return {'x': x, 'positions': positions, 'norm_mix': norm_mix, 'norm_ffn': norm_ffn, 'final_norm': final_norm,
            'mla_w_in': mla_w_in, 'mla_q_norm': mla_q_norm, 'mla_w_qb': mla_w_qb, 'mla_kv_norm': mla_kv_norm,
            'mla_w_kvb': mla_w_kvb, 'mla_w_out': mla_w_out, 'mlstm_w_in': mlstm_w_in, 'mlstm_conv_w': mlstm_conv_w,
            'mlstm_conv_b': mlstm_conv_b, 'mlstm_gate_b': mlstm_gate_b, 'mlstm_head_norm': mlstm_head_norm,
            'mlstm_w_out': mlstm_w_out, 'ffn_w_gate': ffn_w_gate, 'ffn_w_up': ffn_w_up, 'ffn_w_down': ffn_w_down,
            'moe_router': moe_router, 'moe_w_gate': moe_w_gate, 'moe_w_up': moe_w_up, 'moe_w_down': moe_w_down}


def reference(x, positions, norm_mix, norm_ffn, final_norm, mla_w_in, mla_q_norm, mla_w_qb, mla_kv_norm,
              mla_w_kvb, mla_w_out, mlstm_w_in, mlstm_conv_w, mlstm_conv_b, mlstm_gate_b, mlstm_head_norm,
              mlstm_w_out, ffn_w_gate, ffn_w_up, ffn_w_down, moe_router, moe_w_gate, moe_w_up, moe_w_down):
    cos, sin = rope_tables(positions)
    for layer in range(DEPTH):
        j = layer // 2
        h = rmsnorm(x, norm_mix[layer])
        if layer % 2 == 0:
            y = mla_mixer(h, cos, sin, mla_w_in[j], mla_q_norm[j], mla_w_qb[j], mla_kv_norm[j], mla_w_kvb[j], mla_w_out[j])
        else:
            y = mlstm_mixer(h, mlstm_w_in[j], mlstm_conv_w[j], mlstm_conv_b[j], mlstm_gate_b[j], mlstm_head_norm[j], mlstm_w_out[j])
        x = x + y.astype(x.dtype)
        h = rmsnorm(x, norm_ffn[layer])
        if layer % 2 == 0:
            y = swiglu(h, ffn_w_gate[j], ffn_w_up[j], ffn_w_down[j])
        else:
            y = moe_swiglu(h, moe_router[j], moe_w_gate[j], moe_w_up[j], moe_w_down[j])
        x = x + y.astype(x.dtype)
    return rmsnorm(x, final_norm)
```

```python
from contextlib import ExitStack
import numpy as np
import concourse.bass as bass
import concourse.mybir as mybir
from concourse.bass_utils import run_bass_kernel_spmd

F32 = mybir.dt.float32
BF16 = mybir.dt.bfloat16
I32 = mybir.dt.int32
AF = mybir.ActivationFunctionType
ALU = mybir.AluOpType
AX = mybir.AxisListType


class T:
    def __init__(self, t, name):
        self.t = t
        self.name = name
        self.w = {}
        self.r = {}

    def __getitem__(self, k):
        return self.t[k]


class Eng:
    def __init__(self, prog, name, eng, nring):
        self.p = prog
        self.name = name
        self.eng = eng
        self.sem = prog.new_sem("c_" + name)
        self.count = 0
        self.seen = {}
        self.ring = [prog.new_sem("d_%s%d" % (name, i)) for i in range(nring)]
        self.ring_cnt = [0] * nring
        self.ring_pos = 0

    def _wait(self, mark):
        if mark is None:
            return
        sem, val = mark
        k = id(sem)
        if self.seen.get(k, 0) < val:
            self.eng.wait_ge(sem, val)
            self.seen[k] = val

    def _deps(self, reads, writes, skip_self, dma_ring=None):
        for b in reads:
            for m in b.w.values():
                if not (skip_self and m[0] is self.sem):
                    self._wait(m)
        for b in writes:
            for m in b.w.values():
                if dma_ring is not None and id(m[0]) in dma_ring:
                    continue
                if not (skip_self and m[0] is self.sem):
                    self._wait(m)
            for m in b.r.values():
                if not (skip_self and m[0] is self.sem):
                    self._wait(m)

    def _mark(self, reads, writes, mark, dma_ring=None):
        for b in reads:
            b.r[id(mark[0])] = mark
        for b in writes:
            if dma_ring is not None:
                b.w = {k: v for k, v in b.w.items() if k in dma_ring}
            else:
                b.w = {}
            b.w[id(mark[0])] = mark
            b.r = {}

    def op(self, fn, reads=(), writes=()):
        ex = [b for b in reads if getattr(b, "excl", False)]
        if ex:
            reads = [b for b in reads if not getattr(b, "excl", False)]
            writes = list(writes) + ex
        self._deps(reads, writes, skip_self=(self.name == "pe"))
        inst = fn(self.eng)
        self.count += 1
        inst.then_inc(self.sem, 1)
        self._mark(reads, writes, (self.sem, self.count))

    def dma(self, out, in_, reads=(), writes=(), **kw):
        ring_ids = set(id(x) for x in self.ring)
        self._deps(reads, writes, skip_self=False, dma_ring=ring_ids)
        j = self.ring_pos
        self.ring_pos = (j + 1) % len(self.ring)
        sem = self.ring[j]
        if self.ring_cnt[j] > 0:
            self._wait((sem, self.ring_cnt[j]))
        inst = self.eng.dma_start(out=out, in_=in_, **kw)
        self.ring_cnt[j] += 16
        inst.then_inc(sem, 16)
        self._mark(reads, writes, (sem, self.ring_cnt[j]), dma_ring=ring_ids)

    def wait_all(self, marks):
        for m in marks:
            self._wait(m)


class Prog:
    def __init__(self):
        self.nc = bass.Bass("TRN2", target_bir_lowering=False)
        self.es = ExitStack()
        self.nsem = 0
        nc = self.nc
        self.pe = Eng(self, "pe", nc.tensor, 0)
        self.act = Eng(self, "act", nc.scalar, 8)
        self.dve = Eng(self, "dve", nc.vector, 0)
        self.pool = Eng(self, "pool", nc.gpsimd, 24)
        self.sp = Eng(self, "sp", nc.sync, 24)
        self.engs = [self.pe, self.act, self.dve, self.pool, self.sp]
        self.dram_ts = []

    def new_sem(self, name):
        self.nsem += 1
        return self.es.enter_context(self.nc.semaphore(name))

    def dram(self, name, shape, dtype, kind, **kw):
        t = T(self.nc.dram_tensor(name, list(shape), dtype, kind=kind, **kw).ap(), name)
        self.dram_ts.append(t)
        return t

    def sb(self, stack, name, shape, dtype):
        self.uid = getattr(self, "uid", 0) + 1
        name = "%s_u%d" % (name, self.uid)
        return T(stack.enter_context(self.nc.sbuf_tensor(name, list(shape), dtype)), name)

    def ps(self, stack, name, shape, dtype):
        self.uid = getattr(self, "uid", 0) + 1
        name = "%s_u%d" % (name, self.uid)
        t = T(stack.enter_context(self.nc.psum_tensor(name, list(shape), dtype)), name)
        t.excl = True
        return t

    def dram_internal(self, name, shape, dtype):
        t = T(self.nc.dram_tensor(name, list(shape), dtype).ap(), name)
        self.dram_ts.append(t)
        return t

    def allgather(self, src, dst):
        e = self.pool
        e._deps([src], [dst], False)
        sem = self.new_sem("cc%d" % self.nsem)
        inst = self.nc.gpsimd.collective_compute("AllGather", ALU.bypass, replica_groups=[list(range(8))],
                                                 ins=[src.t], outs=[dst.t])
        inst.then_inc(sem, 1)
        e._mark([src], [dst], (sem, 1))
        self.extra_marks = getattr(self, "extra_marks", []) + [(sem, 1)]

    def barrier(self):
        marks = list(getattr(self, "extra_marks", []))
        for e in self.engs:
            if e.count > 0:
                marks.append((e.sem, e.count))
            for j, s in enumerate(e.ring):
                if e.ring_cnt[j] > 0:
                    marks.append((s, e.ring_cnt[j]))
        for e in self.engs:
            e.wait_all(marks)

    def finish(self):
        self.barrier()
        self.es.close()
        return self.nc


D = 1024
DFF = 3584
EPS = 1e-6


def rms_rstd(p, ss, rstd, n, n_feat):
    p.act.op(lambda e: e.activation(out=rstd[:, 0:n], in_=ss[:, 0:n], func=AF.Ln,
                                    scale=1.0 / n_feat, bias=p.eps_t[:, 0:1]),
             reads=[ss, p.eps_t], writes=[rstd])
    p.act.op(lambda e: e.activation(out=rstd[:, 0:n], in_=rstd[:, 0:n], func=AF.Exp, scale=-0.5),
             reads=[rstd], writes=[rstd])


def setup_consts(p, stack):
    nc = p.nc
    p.eps_t = p.sb(stack, "eps_t", [128, 1], F32)
    p.pool.op(lambda e: e.memset(p.eps_t[:, :], EPS), writes=[p.eps_t])
    p.ident_f = p.sb(stack, "ident_f", [128, 128], F32)
    p.pool.op(lambda e: e.memset(p.ident_f[:, :], 1.0), writes=[p.ident_f])
    p.pool.op(lambda e: e.affine_select(out=p.ident_f[:, :], in_=p.ident_f[:, :], pattern=[[-1, 128]],
                                        compare_op=ALU.is_equal, fill=0.0, base=0, channel_multiplier=1),
              reads=[p.ident_f], writes=[p.ident_f])
    p.ident_b = p.sb(stack, "ident_b", [128, 128], BF16)
    p.pool.op(lambda e: e.tensor_copy(out=p.ident_b[:, :], in_=p.ident_f[:, :]), reads=[p.ident_f], writes=[p.ident_b])


def phase_ffn(p, xin, xout, g_norm, wg, wu, wd, E, routerT=None, final_g=None, NT=16, FCP=2, ymT=None, w_o=None, ymT_dyn=False):
    nc = p.nc
    NTOK = NT * 128
    NTG = NTOK // 512
    FS = FCP * 128
    NFS = DFF // FS
    with ExitStack() as st:
        xres = [p.sb(st, "xres%d" % t, [128, D], F32) for t in range(NT)]
        xnT = p.sb(st, "xnT", [128, 8, NTOK], BF16)
        gbc = p.sb(st, "gbc", [128, D], F32)
        ss = p.sb(st, "ss", [128, NT], F32)
        rstd = p.sb(st, "rstd", [128, NT], F32)
        comb = p.sb(st, "comb", [128, NT, 8], F32)
        psb = [p.ps(st, "psb%d" % i, [128, 512], F32) for i in range(8)]

        p.sp.dma(gbc[:, :], g_norm.t.partition_broadcast(128), reads=[g_norm], writes=[gbc])
        for t in range(NT):
            p.sp.dma(xres[t][:, :], xin[t * 128:(t + 1) * 128, :], reads=[xin], writes=[xres[t]])

        if ymT is not None:
            with ExitStack() as s0:
                ymT_b = p.sb(s0, "ymT_b", [128, 8, NTOK], BF16)
                wo_b = p.sb(s0, "wo_b", [128, 8, D], BF16)
                if ymT_dyn:
                    pid = p.nc.gpsimd.partition_id()
                    for hc in range(8):
                        p.pool.dma(ymT_b[:, hc, :], ymT.t[hc * 128:(hc + 1) * 128, bass.ds(pid * NTOK, NTOK)],
                                   reads=[ymT], writes=[ymT_b])
                else:
                    p.pool.dma(ymT_b[:, :, :], ymT.t.rearrange("(hc p) t -> p hc t", p=128), reads=[ymT], writes=[ymT_b])
                p.pool.dma(wo_b[:, :, :], w_o.t.rearrange("(hc p) d -> p hc d", p=128), reads=[w_o], writes=[wo_b])
                for t in range(NT):
                    for half in range(2):
                        pd = psb[4 + (2 * t + half) % 4]
                        for hc in range(8):
                            p.pe.op(lambda e, t=t, half=half, hc=hc, pd=pd: e.matmul(
                                out=pd[:, :], lhsT=ymT_b[:, hc, t * 128:(t + 1) * 128], rhs=wo_b[:, hc, half * 512:(half + 1) * 512],
                                start=(hc == 0), stop=(hc == 7)), reads=[ymT_b, wo_b], writes=[pd])
                        p.dve.op(lambda e, t=t, half=half, pd=pd: e.tensor_tensor(
                            out=xres[t][:, half * 512:(half + 1) * 512], in0=pd[:, :], in1=xres[t][:, half * 512:(half + 1) * 512],
                            op=ALU.add), reads=[pd, xres[t]], writes=[xres[t]])
                p.barrier()
        with ExitStack() as st2:
            junk = [p.sb(st2, "junk%d" % i, [128, D], BF16) for i in range(2)]
            xn = [p.sb(st2, "xn%d" % i, [128, D], BF16) for i in range(2)]
            for t in range(NT):
                j = junk[t % 2]
                p.act.op(lambda e, t=t, j=j: e.activation(out=j[:, :], in_=xres[t][:, :], func=AF.Square,
                                                         accum_out=ss[:, t:t + 1]),
                         reads=[xres[t]], writes=[j, ss])
            rms_rstd(p, ss, rstd, NT, D)
            if E > 1:
                xn32 = [p.sb(st2, "xn32_%d" % i, [128, D], F32) for i in range(2)]
                rT = p.sb(st2, "rT", [128, 8, D], F32)
                logit = p.sb(st2, "logit", [128, NT, 8], F32)
                for e_ in range(8):
                    p.sp.dma(rT[:, e_, :], routerT.t[e_].partition_broadcast(128), reads=[routerT], writes=[rT])
            for t in range(NT):
                x_n = xn[t % 2]
                p.dve.op(lambda e, t=t, x_n=x_n: e.scalar_tensor_tensor(
                    out=x_n[:, :], in0=xres[t][:, :], scalar=rstd[:, t:t + 1], in1=gbc[:, :],
                    op0=ALU.mult, op1=ALU.mult), reads=[xres[t], rstd, gbc], writes=[x_n])
                pst = psb[t % 2]
                pv = pst.t[:, :].bitcast(BF16)
                for dc in range(8):
                    p.pe.op(lambda e, dc=dc, x_n=x_n, pv=pv: e.transpose(
                        out=pv[:, dc * 128:(dc + 1) * 128], in_=x_n[:, dc * 128:(dc + 1) * 128], identity=p.ident_b[:, :]),
                        reads=[x_n, p.ident_b], writes=[pst])
                ev = p.act if t % 2 == 0 else p.dve
                if ev is p.act:
                    ev.op(lambda e, t=t, pv=pv: e.copy(out=xnT[:, :, t * 128:(t + 1) * 128],
                                                      in_=pv.rearrange("p (c n) -> p c n", c=8)),
                          reads=[pst], writes=[xnT])
                else:
                    ev.op(lambda e, t=t, pv=pv: e.tensor_copy(out=xnT[:, :, t * 128:(t + 1) * 128],
                                                             in_=pv.rearrange("p (c n) -> p c n", c=8)),
                          reads=[pst], writes=[xnT])
                if E > 1:
                    x32 = xn32[t % 2]
                    p.pool.op(lambda e, t=t, x32=x32: e.tensor_scalar(
                        out=x32[:, :], in0=xres[t][:, :], scalar1=rstd[:, t:t + 1], scalar2=None, op0=ALU.mult),
                        reads=[xres[t], rstd], writes=[x32])
                    p.pool.op(lambda e, x32=x32: e.tensor_tensor(out=x32[:, :], in0=x32[:, :], in1=gbc[:, :], op=ALU.mult),
                              reads=[x32, gbc], writes=[x32])
                    for e_ in range(8):
                        jj = junk[e_ % 2]
                        p.dve.op(lambda e, t=t, e_=e_, x32=x32, jj=jj: e.scalar_tensor_tensor(
                            out=jj[:, :], in0=x32[:, :], scalar=1.0, in1=rT[:, e_, :], op0=ALU.mult, op1=ALU.mult,
                            accum_out=logit[:, t, e_:e_ + 1]), reads=[x32, rT], writes=[jj, logit])
            if E > 1:
                m1 = p.sb(st2, "m1", [128, NT], F32)
                m2 = p.sb(st2, "m2", [128, NT], F32)
                tmp = p.sb(st2, "tmp", [128, NT, 8], F32)
                tmp2 = p.sb(st2, "tmp2", [128, NT, 8], F32)
                sh = [128, NT, 8]
                p.dve.op(lambda e: e.tensor_reduce(out=m1[:, :], in_=logit[:, :, :], axis=AX.X, op=ALU.max),
                         reads=[logit], writes=[m1])
                p.dve.op(lambda e: e.tensor_tensor(out=tmp[:, :, :], in0=logit[:, :, :],
                                                   in1=m1[:, :].unsqueeze(2).to_broadcast(sh), op=ALU.is_equal),
                         reads=[logit, m1], writes=[tmp])
                p.dve.op(lambda e: e.scalar_tensor_tensor(out=tmp[:, :, :], in0=tmp[:, :, :], scalar=-1e30,
                                                          in1=logit[:, :, :], op0=ALU.mult, op1=ALU.add),
                         reads=[tmp, logit], writes=[tmp])
                p.dve.op(lambda e: e.tensor_reduce(out=m2[:, :], in_=tmp[:, :, :], axis=AX.X, op=ALU.max),
                         reads=[tmp], writes=[m2])
                p.dve.op(lambda e: e.tensor_tensor(out=tmp[:, :, :], in0=logit[:, :, :],
                                                   in1=m2[:, :].unsqueeze(2).to_broadcast(sh), op=ALU.is_ge),
                         reads=[logit, m2], writes=[tmp])
                p.dve.op(lambda e: e.tensor_tensor(out=tmp2[:, :, :], in0=logit[:, :, :],
                                                   in1=m1[:, :].unsqueeze(2).to_broadcast(sh), op=ALU.subtract),
                         reads=[logit, m1], writes=[tmp2])
                p.act.op(lambda e: e.activation(out=tmp2[:, :, :], in_=tmp2[:, :, :], func=AF.Exp),
                         reads=[tmp2], writes=[tmp2])
                p.dve.op(lambda e: e.tensor_tensor(out=tmp2[:, :, :], in0=tmp2[:, :, :], in1=tmp[:, :, :], op=ALU.mult),
                         reads=[tmp2, tmp], writes=[tmp2])
                p.dve.op(lambda e: e.tensor_reduce(out=m1[:, :], in_=tmp2[:, :, :], axis=AX.X, op=ALU.add),
                         reads=[tmp2], writes=[m1])
                p.dve.op(lambda e: e.reciprocal(out=m1[:, :], in_=m1[:, :]), reads=[m1], writes=[m1])
                p.dve.op(lambda e: e.tensor_tensor(out=comb[:, :, :], in0=tmp2[:, :, :],
                                                   in1=m1[:, :].unsqueeze(2).to_broadcast(sh), op=ALU.mult),
                         reads=[tmp2, m1], writes=[comb])
            p.barrier()

        with ExitStack() as st3:
            NB = 3
            wgb = [p.sb(st3, "wgb%d" % i, [128, 8, FS], BF16) for i in range(NB)]
            wub = [p.sb(st3, "wub%d" % i, [128, 8, FS], BF16) for i in range(NB)]
            wdb = [p.sb(st3, "wdb%d" % i, [128, FCP, D], BF16) for i in range(NB)]
            sg = [p.sb(st3, "sg%d" % i, [128, 512], BF16) for i in range(3)]
            hT = [p.sb(st3, "hT%d" % i, [128, FCP, 512], BF16) for i in range(3)]

            units = [(e_, fs) for e_ in range(E) for fs in range(NFS)]

            def load(ui):
                e_, fs = units[ui]
                b = ui % NB
                p.pool.dma(wgb[b][:, :, :], wg.t[e_].rearrange("(dc p) f -> p dc f", p=128)[:, :, fs * FS:(fs + 1) * FS],
                           reads=[wg], writes=[wgb[b]])
                p.pool.dma(wub[b][:, :, :], wu.t[e_].rearrange("(dc p) f -> p dc f", p=128)[:, :, fs * FS:(fs + 1) * FS],
                           reads=[wu], writes=[wub[b]])
                p.pool.dma(wdb[b][:, :, :], wd.t[e_].rearrange("(fc p) d -> p fc d", p=128)[:, fs * FCP:(fs + 1) * FCP, :],
                           reads=[wd], writes=[wdb[b]])

            work = [(ui, tg) for ui in range(len(units)) for tg in range(NTG)]
            gu_i = [0]

            def GU(wi):
                ui, tg = work[wi]
                b = ui % NB
                hb = hT[wi % 3]
                for fc in range(FCP):
                    k = gu_i[0]
                    gu_i[0] += 1
                    pg = psb[(k % 2) * 2]
                    pu = psb[(k % 2) * 2 + 1]
                    for dc in range(8):
                        p.pe.op(lambda e, dc=dc, fc=fc, pg=pg: e.matmul(
                            out=pg[:, :], lhsT=wgb[b][:, dc, fc * 128:(fc + 1) * 128],
                            rhs=xnT[:, dc, tg * 512:(tg + 1) * 512], start=(dc == 0), stop=(dc == 7)),
                            reads=[wgb[b], xnT], writes=[pg])
                    for dc in range(8):
                        p.pe.op(lambda e, dc=dc, fc=fc, pu=pu: e.matmul(
                            out=pu[:, :], lhsT=wub[b][:, dc, fc * 128:(fc + 1) * 128],
                            rhs=xnT[:, dc, tg * 512:(tg + 1) * 512], start=(dc == 0), stop=(dc == 7)),
                            reads=[wub[b], xnT], writes=[pu])
                    s = sg[k % 3]
                    p.act.op(lambda e, s=s, pg=pg: e.activation(out=s[:, :], in_=pg[:, :], func=AF.Silu),
                             reads=[pg], writes=[s])
                    p.dve.op(lambda e, s=s, pu=pu, fc=fc: e.tensor_tensor(out=hb[:, fc, :], in0=pu[:, :], in1=s[:, :], op=ALU.mult),
                             reads=[pu, s], writes=[hb])

            dn_i = [0]

            def DN(wi):
                ui, tg = work[wi]
                e_, fs = units[ui]
                b = ui % NB
                hb = hT[wi % 3]
                for tl in range(4):
                    t = tg * 4 + tl
                    for half in range(2):
                        k = dn_i[0]
                        dn_i[0] += 1
                        pd = psb[4 + k % 4]
                        for fc in range(FCP):
                            p.pe.op(lambda e, fc=fc, pd=pd, tl=tl, half=half: e.matmul(
                                out=pd[:, :], lhsT=hb[:, fc, tl * 128:(tl + 1) * 128],
                                rhs=wdb[b][:, fc, half * 512:(half + 1) * 512], start=(fc == 0), stop=(fc == FCP - 1)),
                                reads=[hb, wdb[b]], writes=[pd])
                        sc = comb[:, t, e_:e_ + 1] if E > 1 else 1.0
                        rd = [pd, xres[t]] + ([comb] if E > 1 else [])
                        p.dve.op(lambda e, pd=pd, t=t, half=half, sc=sc: e.scalar_tensor_tensor(
                            out=xres[t][:, half * 512:(half + 1) * 512], in0=pd[:, :], scalar=sc,
                            in1=xres[t][:, half * 512:(half + 1) * 512], op0=ALU.mult, op1=ALU.add),
                            reads=rd, writes=[xres[t]])

            nload = 0
            for _ in range(min(NB - 1, len(units))):
                load(nload)
                nload += 1
            GU(0)
            for wi in range(len(work)):
                ui, tg = work[wi]
                if tg == 0 and nload < len(units):
                    load(nload)
                    nload += 1
                if wi + 1 < len(work):
                    GU(wi + 1)
                DN(wi)

            if final_g is not None:
                p.sp.dma(gbc[:, :], final_g.t.partition_broadcast(128), reads=[final_g], writes=[gbc])
                fj = [p.sb(st3, "fj%d" % i, [128, D], BF16) for i in range(2)]
                for t in range(NT):
                    j = fj[t % 2]
                    p.act.op(lambda e, t=t, j=j: e.activation(out=j[:, :], in_=xres[t][:, :], func=AF.Square,
                                                             accum_out=ss[:, t:t + 1]),
                             reads=[xres[t]], writes=[j, ss])
                rms_rstd(p, ss, rstd, NT, D)
                for t in range(NT):
                    p.dve.op(lambda e, t=t: e.scalar_tensor_tensor(
                        out=xres[t][:, :], in0=xres[t][:, :], scalar=rstd[:, t:t + 1], in1=gbc[:, :],
                        op0=ALU.mult, op1=ALU.mult), reads=[xres[t], rstd, gbc], writes=[xres[t]])
            for t in range(NT):
                p.sp.dma(xout[t * 128:(t + 1) * 128, :], xres[t][:, :], reads=[xres[t]], writes=[xout])
            p.barrier()


def phase_norm_send(p, xin, g_norm, xnT_send, NT=16):
    with ExitStack() as st:
        gbc = p.sb(st, "ns_gbc", [128, D], F32)
        xs = [p.sb(st, "ns_x%d" % i, [128, D], F32) for i in range(3)]
        junk = [p.sb(st, "ns_j%d" % i, [128, D], BF16) for i in range(2)]
        xn = [p.sb(st, "ns_xn%d" % i, [128, D], BF16) for i in range(2)]
        xT = [p.sb(st, "ns_xT%d" % i, [128, D], BF16) for i in range(2)]
        ss = [p.sb(st, "ns_ss%d" % i, [128, 2], F32) for i in range(2)]
        psb = [p.ps(st, "ns_ps%d" % i, [128, 512], F32) for i in range(2)]
        p.sp.dma(gbc[:, :], g_norm.t.partition_broadcast(128), reads=[g_norm], writes=[gbc])
        for t in range(NT):
            x = xs[t % 3]
            s_ = ss[t % 2]
            j = junk[t % 2]
            x_n = xn[t % 2]
            xt = xT[t % 2]
            p.sp.dma(x[:, :], xin[t * 128:(t + 1) * 128, :], reads=[xin], writes=[x])
            p.act.op(lambda e, x=x, j=j, s_=s_: e.activation(out=j[:, :], in_=x[:, :], func=AF.Square, accum_out=s_[:, 0:1]),
                     reads=[x], writes=[j, s_])
            p.act.op(lambda e, s_=s_: e.activation(out=s_[:, 1:2], in_=s_[:, 0:1], func=AF.Ln, scale=1.0 / D, bias=p.eps_t[:, 0:1]),
                     reads=[s_, p.eps_t], writes=[s_])
            p.act.op(lambda e, s_=s_: e.activation(out=s_[:, 1:2], in_=s_[:, 1:2], func=AF.Exp, scale=-0.5), reads=[s_], writes=[s_])
            p.dve.op(lambda e, x=x, x_n=x_n, s_=s_: e.scalar_tensor_tensor(
                out=x_n[:, :], in0=x[:, :], scalar=s_[:, 1:2], in1=gbc[:, :], op0=ALU.mult, op1=ALU.mult),
                reads=[x, s_, gbc], writes=[x_n])
            pb = psb[t % 2]
            pv = pb.t[:, :].bitcast(BF16)
            for dc in range(8):
                p.pe.op(lambda e, dc=dc, x_n=x_n, pv=pv: e.transpose(out=pv[:, dc * 128:(dc + 1) * 128], in_=x_n[:, dc * 128:(dc + 1) * 128],
                                                                    identity=p.ident_b[:, :]), reads=[x_n, p.ident_b], writes=[pb])
            p.act.op(lambda e, xt=xt, pv=pv: e.copy(out=xt[:, :], in_=pv), reads=[pb], writes=[xt])
            p.sp.dma(xnT_send.t[t * 128:(t + 1) * 128, :], xt[:, :], reads=[xt], writes=[xnT_send])
        p.barrier()

import math
ASTEP = 99

S_ALL = 16384
NTILE = 128
NSLOT = 16
MAGIC = 12582912.0
TWO_PI_HI = 6.28125
TWO_PI_LO = 2.0 * math.pi - 6.28125
SCALE = 192.0 ** -0.5


def trig_tables(p, st, posf_ap, n, inv_bc, cos_t, sin_t, reads):
    sh = [128, n, 32]
    ang = p.sb(st, "tg_ang", sh, F32)
    a2 = p.sb(st, "tg_a2", sh, F32)
    nn = p.sb(st, "tg_n", sh, F32)
    p.dve.op(lambda e: e.tensor_tensor(out=ang[:, :, :], in0=posf_ap.unsqueeze(2).to_broadcast(sh),
                                       in1=inv_bc[:, :].unsqueeze(1).to_broadcast(sh), op=ALU.mult),
             reads=reads + [inv_bc], writes=[ang])
    for which, dst in ((0, sin_t), (1, cos_t)):
        if which == 1:
            p.dve.op(lambda e: e.tensor_scalar(out=ang[:, :, :], in0=ang[:, :, :], scalar1=math.pi / 2, scalar2=None,
                                               op0=ALU.add), reads=[ang], writes=[ang])
        p.dve.op(lambda e: e.tensor_scalar(out=nn[:, :, :], in0=ang[:, :, :], scalar1=1.0 / (2 * math.pi), scalar2=None,
                                           op0=ALU.mult), reads=[ang], writes=[nn])
        p.dve.op(lambda e: e.tensor_scalar(out=nn[:, :, :], in0=nn[:, :, :], scalar1=MAGIC, scalar2=None, op0=ALU.add),
                 reads=[nn], writes=[nn])
        p.dve.op(lambda e: e.tensor_scalar(out=nn[:, :, :], in0=nn[:, :, :], scalar1=MAGIC, scalar2=None, op0=ALU.subtract),
                 reads=[nn], writes=[nn])
        p.dve.op(lambda e: e.scalar_tensor_tensor(out=a2[:, :, :], in0=nn[:, :, :], scalar=-TWO_PI_HI, in1=ang[:, :, :],
                                                  op0=ALU.mult, op1=ALU.add), reads=[nn, ang], writes=[a2])
        p.dve.op(lambda e: e.scalar_tensor_tensor(out=a2[:, :, :], in0=nn[:, :, :], scalar=-TWO_PI_LO, in1=a2[:, :, :],
                                                  op0=ALU.mult, op1=ALU.add), reads=[nn, a2], writes=[a2])
        p.dve.op(lambda e: e.tensor_scalar(out=a2[:, :, :], in0=a2[:, :, :], scalar1=-3.14159, scalar2=3.14159,
                                           op0=ALU.max, op1=ALU.min), reads=[a2], writes=[a2])
        p.act.op(lambda e, dst=dst: e.activation(out=dst[:, :, :], in_=a2[:, :, :], func=AF.Sin),
                 reads=[a2], writes=[dst])


def phase_attn(p, x_full, x_own, posT, pos_own, inv_freq, masks, g_norm, w_in, q_norm, w_qb, kv_norm, w_kvb, w_out,
               xa_out, NTILE=NTILE, NSLOT=NSLOT, STOP=99):
    nc = p.nc
    NKEY = NTILE * 128
    with ExitStack() as st:
        KlatT = p.sb(st, "KlatT", [128, NKEY], BF16)
        KrotT = p.sb(st, "KrotT", [128, NKEY], BF16)
        Vlat = p.sb(st, "Vlat", [128, NTILE, 128], BF16)
        w_in_b = p.sb(st, "w_in_b", [128, 8, 448], BF16)
        w_qb_b = p.sb(st, "w_qb_b", [128, 2, 1536], BF16)
        w_kvb_b = p.sb(st, "w_kvb_b", [128, 2048], BF16)
        WkbT = p.sb(st, "WkbT", [128, 8, 128], BF16)
        w_out_b = p.sb(st, "w_out_b", [128, 8, 1024], BF16)
        gbc = p.sb(st, "a_gbc", [128, D], F32)
        qn_bc = p.sb(st, "qn_bc", [128, 256], F32)
        kvn_bc = p.sb(st, "kvn_bc", [128, 128], F32)
        inv_bc = p.sb(st, "inv_bc", [128, 32], F32)
        mask_b = p.sb(st, "mask_b", [128, 8, 512], BF16)
        ones_b = p.sb(st, "ones_b", [128, 128], BF16)
        cos_o = p.sb(st, "cos_o", [128, NSLOT, 32], BF16)
        sin_o = p.sb(st, "sin_o", [128, NSLOT, 32], BF16)
        ps = [p.ps(st, "aps%d" % i, [128, 512], F32) for i in range(8)]

        def psbf(i):
            return ps[i].t[:, :].bitcast(BF16)

        p.pool.dma(w_in_b[:, :, :], w_in.t.rearrange("(dc p) f -> p dc f", p=128), reads=[w_in], writes=[w_in_b])
        p.pool.dma(w_qb_b[:, :, :], w_qb.t.rearrange("(dc p) f -> p dc f", p=128), reads=[w_qb], writes=[w_qb_b])
        p.pool.dma(w_kvb_b[:, :], w_kvb.t, reads=[w_kvb], writes=[w_kvb_b])
        p.pool.dma(w_out_b[:, :, :], w_out.t.rearrange("(h p) d -> p h d", p=128), reads=[w_out], writes=[w_out_b])
        p.pool.dma(mask_b[:, :, :], masks.t.rearrange("j p n -> p j n"), reads=[masks], writes=[mask_b])
        p.sp.dma(gbc[:, :], g_norm.t.partition_broadcast(128), reads=[g_norm], writes=[gbc])
        p.sp.dma(qn_bc[:, :], q_norm.t.partition_broadcast(128), reads=[q_norm], writes=[qn_bc])
        p.sp.dma(kvn_bc[:, :], kv_norm.t.partition_broadcast(128), reads=[kv_norm], writes=[kvn_bc])
        p.sp.dma(inv_bc[:, :], inv_freq.t.partition_broadcast(128), reads=[inv_freq], writes=[inv_bc])
        p.pool.op(lambda e: e.memset(ones_b[:, :], 1.0), writes=[ones_b])
        pv = psbf(0)
        for h in range(8):
            p.pe.op(lambda e, h=h: e.transpose(out=pv[:, h * 128:(h + 1) * 128], in_=w_kvb_b[:, h * 256:h * 256 + 128],
                                               identity=p.ident_b[:, :]), reads=[w_kvb_b, p.ident_b], writes=[ps[0]])
        p.dve.op(lambda e: e.tensor_copy(out=WkbT[:, :, :], in_=pv.rearrange("p (h n) -> p h n", h=8)),
                 reads=[ps[0]], writes=[WkbT])

        if STOP <= 1:
            p.barrier()
            return
        with ExitStack() as sa:
            cos_a = p.sb(sa, "cos_a", [128, NTILE, 32], BF16)
            sin_a = p.sb(sa, "sin_a", [128, NTILE, 32], BF16)
            with ExitStack() as stt_:
                posi = p.sb(stt_, "posi", [128, NTILE], I32)
                posf = p.sb(stt_, "posf", [128, NTILE], F32)
                posoi = p.sb(stt_, "posoi", [128, NSLOT], I32)
                posof = p.sb(stt_, "posof", [128, NSLOT], F32)
                p.sp.dma(posi[:, :], posT.t, reads=[posT], writes=[posi])
                p.sp.dma(posoi[:, :], pos_own.t, reads=[pos_own], writes=[posoi])
                p.dve.op(lambda e: e.tensor_copy(out=posf[:, :], in_=posi[:, :]), reads=[posi], writes=[posf])
                p.dve.op(lambda e: e.tensor_copy(out=posof[:, :], in_=posoi[:, :]), reads=[posoi], writes=[posof])
                CH = min(32, NTILE)
                for c0 in range(0, NTILE, CH):
                    with ExitStack() as s3:
                        ct = p.sb(s3, "ct", [128, CH, 32], BF16)
                        stt = p.sb(s3, "stt", [128, CH, 32], BF16)
                        trig_tables(p, s3, posf[:, c0:c0 + CH], CH, inv_bc, ct, stt, [posf])
                        p.pool.op(lambda e, c0=c0: e.tensor_copy(out=cos_a[:, c0:c0 + CH, :], in_=ct[:, :, :]),
                                  reads=[ct], writes=[cos_a])
                        p.pool.op(lambda e, c0=c0: e.tensor_copy(out=sin_a[:, c0:c0 + CH, :], in_=stt[:, :, :]),
                                  reads=[stt], writes=[sin_a])
                        p.barrier()
                with ExitStack() as s3:
                    trig_tables(p, s3, posof[:, :], NSLOT, inv_bc, cos_o, sin_o, [posof])
                    p.barrier()

            if STOP <= 2:
                p.barrier()
                return
            xs = [p.sb(sa, "xs%d" % i, [128, D], F32) for i in range(3)]
            junk = [p.sb(sa, "ajunk%d" % i, [128, D], BF16) for i in range(2)]
            xn = [p.sb(sa, "axn%d" % i, [128, D], BF16) for i in range(2)]
            xnT = [p.sb(sa, "axnT%d" % i, [128, 8, 128], BF16) for i in range(2)]
            ssA = [p.sb(sa, "ssA%d" % i, [128, 4], F32) for i in range(3)]
            ckn = [p.sb(sa, "ckn%d" % i, [128, 128], BF16) for i in range(2)]
            krA = [p.sb(sa, "krA%d" % i, [128, 2, 32], F32) for i in range(2)]
            krB = [p.sb(sa, "krB%d" % i, [128, 2, 32], F32) for i in range(2)]
            krot = [p.sb(sa, "krot%d" % i, [128, 128], BF16) for i in range(2)]
            for kk in krot:
                p.pool.op(lambda e, kk=kk: e.memset(kk[:, :], 0.0), writes=[kk])

            def load_x(T):
                p.sp.dma(xs[T % 3][:, :], x_full[T * 128:(T + 1) * 128, :], reads=[x_full], writes=[xs[T % 3]])

            junk2 = [p.sb(sa, "ajunk2_%d" % i, [128, 128], BF16) for i in range(2)]
            load_x(0)
            load_x(1)

            def A1(T):
                if T + 2 < NTILE:
                    load_x(T + 2)
                x = xs[T % 3]
                s_ = ssA[T % 3]
                j = junk[T % 2]
                x_n = xn[T % 2]
                xT = xnT[T % 2]
                p.act.op(lambda e, x=x, j=j, s_=s_: e.activation(out=j[:, :], in_=x[:, :], func=AF.Square, accum_out=s_[:, 0:1]),
                         reads=[x], writes=[j, s_])
                p.act.op(lambda e, s_=s_: e.activation(out=s_[:, 1:2], in_=s_[:, 0:1], func=AF.Ln, scale=1.0 / D, bias=p.eps_t[:, 0:1]),
                         reads=[s_, p.eps_t], writes=[s_])
                p.act.op(lambda e, s_=s_: e.activation(out=s_[:, 1:2], in_=s_[:, 1:2], func=AF.Exp, scale=-0.5),
                         reads=[s_], writes=[s_])
                p.dve.op(lambda e, x=x, x_n=x_n, s_=s_: e.scalar_tensor_tensor(
                    out=x_n[:, :], in0=x[:, :], scalar=s_[:, 1:2], in1=gbc[:, :], op0=ALU.mult, op1=ALU.mult),
                    reads=[x, s_, gbc], writes=[x_n])
                pb = ps[T % 2]
                pvv = psbf(T % 2)
                for dc in range(8):
                    p.pe.op(lambda e, dc=dc, x_n=x_n, pvv=pvv: e.transpose(
                        out=pvv[:, dc * 128:(dc + 1) * 128], in_=x_n[:, dc * 128:(dc + 1) * 128], identity=p.ident_b[:, :]),
                        reads=[x_n, p.ident_b], writes=[pb])
                p.act.op(lambda e, xT=xT, pvv=pvv: e.copy(out=xT[:, :, :], in_=pvv.rearrange("p (c n) -> p c n", c=8)),
                         reads=[pb], writes=[xT])

            def A2(T):
                s_ = ssA[T % 3]
                j = junk2[T % 2]
                xT = xnT[T % 2]
                pk = ps[2 + T % 2]
                for dc in range(8):
                    p.pe.op(lambda e, dc=dc, xT=xT, pk=pk: e.matmul(out=pk[:, 0:192], lhsT=xT[:, dc, :], rhs=w_in_b[:, dc, 256:448],
                                                                   start=(dc == 0), stop=(dc == 7)),
                            reads=[xT, w_in_b], writes=[pk])
                p.act.op(lambda e, pk=pk, j=j, s_=s_: e.activation(out=j[:, 0:128], in_=pk[:, 0:128], func=AF.Square,
                                                                  accum_out=s_[:, 2:3]), reads=[pk], writes=[j, s_])
                p.act.op(lambda e, s_=s_: e.activation(out=s_[:, 3:4], in_=s_[:, 2:3], func=AF.Ln, scale=1.0 / 128, bias=p.eps_t[:, 0:1]),
                         reads=[s_, p.eps_t], writes=[s_])
                p.act.op(lambda e, s_=s_: e.activation(out=s_[:, 3:4], in_=s_[:, 3:4], func=AF.Exp, scale=-0.5),
                         reads=[s_], writes=[s_])
                ck = ckn[T % 2]
                p.dve.op(lambda e, pk=pk, ck=ck, s_=s_: e.scalar_tensor_tensor(
                    out=ck[:, :], in0=pk[:, 0:128], scalar=s_[:, 3:4], in1=kvn_bc[:, :], op0=ALU.mult, op1=ALU.mult),
                    reads=[pk, s_, kvn_bc], writes=[ck])
                p.pool.op(lambda e, T=T, ck=ck: e.tensor_copy(out=Vlat[:, T, :], in_=ck[:, :]), reads=[ck], writes=[Vlat])
                A = krA[T % 2]
                B = krB[T % 2]
                kr = krot[T % 2]
                kv3 = pk[:, 128:192].rearrange("p (a f) -> p a f", a=2)
                sh = [128, 2, 32]
                p.dve.op(lambda e, A=A, kv3=kv3, T=T: e.tensor_tensor(
                    out=A[:, :, :], in0=kv3, in1=cos_a[:, T, :].unsqueeze(1).to_broadcast(sh), op=ALU.mult),
                    reads=[pk, cos_a], writes=[A])
                p.dve.op(lambda e, B=B, kv3=kv3, T=T: e.tensor_tensor(
                    out=B[:, :, :], in0=kv3, in1=sin_a[:, T, :].unsqueeze(1).to_broadcast(sh), op=ALU.mult),
                    reads=[pk, sin_a], writes=[B])
                p.pool.op(lambda e, A=A, B=B, kr=kr: e.tensor_tensor(out=kr[:, 0:32], in0=A[:, 0, :], in1=B[:, 1, :], op=ALU.subtract),
                          reads=[A, B], writes=[kr])
                p.pool.op(lambda e, A=A, B=B, kr=kr: e.tensor_tensor(out=kr[:, 32:64], in0=B[:, 0, :], in1=A[:, 1, :], op=ALU.add),
                          reads=[A, B], writes=[kr])

            def A3(T):
                ck = ckn[T % 2]
                kr = krot[T % 2]
                pt_ = ps[4 + T % 2]
                ptv = psbf(4 + T % 2)
                p.pe.op(lambda e, ck=ck, ptv=ptv: e.transpose(out=ptv[:, 0:128], in_=ck[:, :], identity=p.ident_b[:, :]),
                        reads=[ck, p.ident_b], writes=[pt_])
                p.pe.op(lambda e, kr=kr, ptv=ptv: e.transpose(out=ptv[:, 128:256], in_=kr[:, :], identity=p.ident_b[:, :]),
                        reads=[kr, p.ident_b], writes=[pt_])
                p.act.op(lambda e, T=T, ptv=ptv: e.copy(out=KlatT[:, T * 128:(T + 1) * 128], in_=ptv[:, 0:128]),
                         reads=[pt_], writes=[KlatT])
                p.dve.op(lambda e, T=T, ptv=ptv: e.tensor_copy(out=KrotT[:, T * 128:(T + 1) * 128], in_=ptv[:, 128:256]),
                         reads=[pt_], writes=[KrotT])

            for it in range(NTILE + 2):
                if 0 <= it - 2 < NTILE:
                    A3(it - 2)
                if 0 <= it - 1 < NTILE:
                    A2(it - 1)
                if it < NTILE:
                    A1(it)
            p.barrier()

        if STOP <= 3:
            p.barrier()
            return
        with ExitStack() as sb_:
            xo = [p.sb(sb_, "xo%d" % i, [128, D], F32) for i in range(2)]
            junk = p.sb(sb_, "bjunk", [128, D], BF16)
            xn = p.sb(sb_, "bxn", [128, D], BF16)
            xnT = p.sb(sb_, "bxnT", [128, 8, 128], BF16)
            ssB = p.sb(sb_, "ssB", [128, 4], F32)
            cqn = p.sb(sb_, "cqn", [128, 256], BF16)
            cqnT = p.sb(sb_, "cqnT", [128, 2, 128], BF16)
            qnT = p.sb(sb_, "qnT", [128, 8, 128], BF16)
            Q1T = p.sb(sb_, "Q1T", [128, 1024], BF16)
            Q2T = p.sb(sb_, "Q2T", [128, 1024], BF16)
            dacc = p.sb(sb_, "dacc", [128, 512], F32)
            ones_f32 = p.sb(sb_, "ones_f32", [128, 128], F32)
            p.pool.op(lambda e: e.memset(ones_f32[:, :], 1.0), writes=[ones_f32])
            qA = p.sb(sb_, "qA", [128, 8, 2, 32], F32)
            qB = p.sb(sb_, "qB", [128, 8, 2, 32], F32)
            qrot = p.sb(sb_, "qrot", [128, 8, 128], BF16)
            p.pool.op(lambda e: e.memset(qrot[:, :, :], 0.0), writes=[qrot])
            PT = [p.sb(sb_, "PT%d" % i, [128, 512], BF16) for i in range(4)]
            rden = p.sb(sb_, "rden", [128, 512], F32)
            OnT = p.sb(sb_, "OnT", [128, 1024], BF16)
            ohT = p.sb(sb_, "ohT", [128, 8, 128], BF16)
            xout_t = [p.sb(sb_, "xout_t%d" % i, [128, D], F32) for i in range(2)]

            p.sp.dma(xo[0][:, :], x_own[0:128, :], reads=[x_own], writes=[xo[0]])
            pt_i = 0
            for i in range(NSLOT):
                if i + 1 < NSLOT:
                    p.sp.dma(xo[(i + 1) % 2][:, :], x_own[(i + 1) * 128:(i + 2) * 128, :], reads=[x_own], writes=[xo[(i + 1) % 2]])
                x = xo[i % 2]
                p.act.op(lambda e, x=x: e.activation(out=junk[:, :], in_=x[:, :], func=AF.Square, accum_out=ssB[:, 0:1]),
                         reads=[x], writes=[junk, ssB])
                p.act.op(lambda e: e.activation(out=ssB[:, 1:2], in_=ssB[:, 0:1], func=AF.Ln, scale=1.0 / D, bias=p.eps_t[:, 0:1]),
                         reads=[ssB, p.eps_t], writes=[ssB])
                p.act.op(lambda e: e.activation(out=ssB[:, 1:2], in_=ssB[:, 1:2], func=AF.Exp, scale=-0.5), reads=[ssB], writes=[ssB])
                p.dve.op(lambda e, x=x: e.scalar_tensor_tensor(out=xn[:, :], in0=x[:, :], scalar=ssB[:, 1:2], in1=gbc[:, :],
                                                               op0=ALU.mult, op1=ALU.mult), reads=[x, ssB, gbc], writes=[xn])
                pvv = psbf(7)
                for dc in range(8):
                    p.pe.op(lambda e, dc=dc: e.transpose(out=pvv[:, dc * 128:(dc + 1) * 128], in_=xn[:, dc * 128:(dc + 1) * 128],
                                                         identity=p.ident_b[:, :]), reads=[xn, p.ident_b], writes=[ps[7]])
                p.act.op(lambda e: e.copy(out=xnT[:, :, :], in_=pvv.rearrange("p (c n) -> p c n", c=8)), reads=[ps[7]], writes=[xnT])
                for dc in range(8):
                    p.pe.op(lambda e, dc=dc: e.matmul(out=ps[6][:, 0:256], lhsT=xnT[:, dc, :], rhs=w_in_b[:, dc, 0:256],
                                                      start=(dc == 0), stop=(dc == 7)), reads=[xnT, w_in_b], writes=[ps[6]])
                p.act.op(lambda e: e.activation(out=junk[:, 0:256], in_=ps[6][:, 0:256], func=AF.Square, accum_out=ssB[:, 2:3]),
                         reads=[ps[6]], writes=[junk, ssB])
                p.act.op(lambda e: e.activation(out=ssB[:, 3:4], in_=ssB[:, 2:3], func=AF.Ln, scale=1.0 / 256, bias=p.eps_t[:, 0:1]),
                         reads=[ssB, p.eps_t], writes=[ssB])
                p.act.op(lambda e: e.activation(out=ssB[:, 3:4], in_=ssB[:, 3:4], func=AF.Exp, scale=-0.5), reads=[ssB], writes=[ssB])
                p.dve.op(lambda e: e.scalar_tensor_tensor(out=cqn[:, :], in0=ps[6][:, 0:256], scalar=ssB[:, 3:4], in1=qn_bc[:, :],
                                                          op0=ALU.mult, op1=ALU.mult), reads=[ps[6], ssB, qn_bc], writes=[cqn])
                for kc in range(2):
                    p.pe.op(lambda e, kc=kc: e.transpose(out=pvv[:, kc * 128:(kc + 1) * 128], in_=cqn[:, kc * 128:(kc + 1) * 128],
                                                         identity=p.ident_b[:, :]), reads=[cqn, p.ident_b], writes=[ps[7]])
                p.act.op(lambda e: e.copy(out=cqnT[:, :, :], in_=pvv[:, 0:256].rearrange("p (c n) -> p c n", c=2)),
                         reads=[ps[7]], writes=[cqnT])
                for h in range(8):
                    pb = ps[h // 4]
                    for kc in range(2):
                        p.pe.op(lambda e, h=h, kc=kc, pb=pb: e.matmul(
                            out=pb[:, (h % 4) * 128:(h % 4 + 1) * 128], lhsT=w_qb_b[:, kc, h * 192:h * 192 + 128],
                            rhs=cqnT[:, kc, :], start=(kc == 0), stop=(kc == 1)), reads=[w_qb_b, cqnT], writes=[pb])
                p.act.op(lambda e: e.copy(out=qnT[:, 0:4, :], in_=ps[0][:, :].rearrange("p (h n) -> p h n", h=4)),
                         reads=[ps[0]], writes=[qnT])
                p.dve.op(lambda e: e.tensor_copy(out=qnT[:, 4:8, :], in_=ps[1][:, :].rearrange("p (h n) -> p h n", h=4)),
                         reads=[ps[1]], writes=[qnT])
                for h in range(8):
                    pb = ps[2 + h // 4]
                    p.pe.op(lambda e, h=h, pb=pb: e.matmul(out=pb[:, (h % 4) * 128:(h % 4 + 1) * 128], lhsT=WkbT[:, h, :],
                                                           rhs=qnT[:, h, :], start=True, stop=True), reads=[WkbT, qnT], writes=[pb])
                p.act.op(lambda e: e.copy(out=Q1T[:, 0:512], in_=ps[2][:, :]), reads=[ps[2]], writes=[Q1T])
                p.dve.op(lambda e: e.tensor_copy(out=Q1T[:, 512:1024], in_=ps[3][:, :]), reads=[ps[3]], writes=[Q1T])
                wr = w_qb_b[:, :, :].rearrange("p c (h f) -> p c h f", h=8)
                for kc in range(2):
                    p.pe.op(lambda e, kc=kc: e.matmul(out=ps[6][:, :].rearrange("p (h f) -> p h f", h=8), lhsT=cqnT[:, kc, :],
                                                      rhs=wr[:, kc, :, 128:192], start=(kc == 0), stop=(kc == 1)),
                            reads=[cqnT, w_qb_b], writes=[ps[6]])
                q4 = ps[6][:, :].rearrange("p (h a f) -> p h a f", h=8, a=2)
                sh4 = [128, 8, 2, 32]
                cb = cos_o[:, i, :].unsqueeze(1).unsqueeze(1).to_broadcast(sh4)
                sbb = sin_o[:, i, :].unsqueeze(1).unsqueeze(1).to_broadcast(sh4)
                p.dve.op(lambda e, cb=cb: e.tensor_tensor(out=qA[:, :, :, :], in0=q4, in1=cb, op=ALU.mult),
                         reads=[ps[6], cos_o], writes=[qA])
                p.dve.op(lambda e, sbb=sbb: e.tensor_tensor(out=qB[:, :, :, :], in0=q4, in1=sbb, op=ALU.mult),
                         reads=[ps[6], sin_o], writes=[qB])
                p.pool.op(lambda e: e.tensor_tensor(out=qrot[:, :, 0:32], in0=qA[:, :, 0, :], in1=qB[:, :, 1, :], op=ALU.subtract),
                          reads=[qA, qB], writes=[qrot])
                p.pool.op(lambda e: e.tensor_tensor(out=qrot[:, :, 32:64], in0=qB[:, :, 0, :], in1=qA[:, :, 1, :], op=ALU.add),
                          reads=[qA, qB], writes=[qrot])
                for h in range(8):
                    p.pe.op(lambda e, h=h: e.transpose(out=pvv[:, h * 128:(h + 1) * 128], in_=qrot[:, h, :],
                                                       identity=p.ident_b[:, :]), reads=[qrot, p.ident_b], writes=[ps[7]])
                p.act.op(lambda e: e.copy(out=Q2T[:, :], in_=pvv[:, :]), reads=[ps[7]], writes=[Q2T])

                nkb = 8 * i + 8
                for g in range(2):
                    po = ps[3 + g]
                    pd = ps[5 + g]
                    qs = slice(g * 512, (g + 1) * 512)

                    def S(kb, g=g, qs=qs):
                        pS = ps[kb % 3]
                        masked = kb >= 8 * i
                        p.pe.op(lambda e: e.matmul(out=pS[:, :], lhsT=KlatT[:, kb * 128:(kb + 1) * 128], rhs=Q1T[:, qs],
                                                   start=True, stop=False), reads=[KlatT, Q1T], writes=[pS])
                        p.pe.op(lambda e: e.matmul(out=pS[:, :], lhsT=KrotT[:, kb * 128:(kb + 1) * 128], rhs=Q2T[:, qs],
                                                   start=False, stop=not masked), reads=[KrotT, Q2T], writes=[pS])
                        if masked:
                            p.pe.op(lambda e: e.matmul(out=pS[:, :], lhsT=p.ident_b[:, :], rhs=mask_b[:, kb - 8 * i, :],
                                                       start=False, stop=True), reads=[p.ident_b, mask_b], writes=[pS])

                    S(0)
                    if nkb > 1:
                        S(1)
                    for kb in range(nkb):
                        if kb + 2 < nkb:
                            S(kb + 2)
                        pS = ps[kb % 3]
                        Pt = PT[pt_i % 4]
                        pt_i += 1
                        p.act.op(lambda e, pS=pS, Pt=Pt: e.activation(out=Pt[:, :], in_=pS[:, :], func=AF.Exp, scale=SCALE),
                                 reads=[pS], writes=[Pt])
                        p.pe.op(lambda e, kb=kb, Pt=Pt, po=po: e.matmul(out=po[:, :], lhsT=Vlat[:, kb, :], rhs=Pt[:, :],
                                                                        start=(kb == 0), stop=(kb == nkb - 1)),
                                reads=[Vlat, Pt], writes=[po])
                        if kb == 0:
                            p.dve.op(lambda e, Pt=Pt: e.tensor_copy(out=dacc[:, :], in_=Pt[:, :]), reads=[Pt], writes=[dacc])
                        else:
                            p.dve.op(lambda e, Pt=Pt: e.tensor_tensor(out=dacc[:, :], in0=dacc[:, :], in1=Pt[:, :], op=ALU.add),
                                     reads=[dacc, Pt], writes=[dacc])
                    p.pe.op(lambda e, pd=pd: e.matmul(out=pd[:, :], lhsT=ones_f32[:, :], rhs=dacc[:, :], start=True, stop=True),
                            reads=[ones_f32, dacc], writes=[pd])
                    p.dve.op(lambda e, pd=pd: e.reciprocal(out=rden[:, :], in_=pd[:, :]), reads=[pd], writes=[rden])
                    p.dve.op(lambda e, po=po, qs=qs: e.tensor_tensor(out=OnT[:, qs], in0=po[:, :], in1=rden[:, :], op=ALU.mult),
                             reads=[po, rden], writes=[OnT])
                for h in range(8):
                    pb = ps[h // 4]
                    p.pe.op(lambda e, h=h, pb=pb: e.matmul(out=pb[:, (h % 4) * 128:(h % 4 + 1) * 128],
                                                           lhsT=w_kvb_b[:, h * 256 + 128:(h + 1) * 256],
                                                           rhs=OnT[:, h * 128:(h + 1) * 128], start=True, stop=True),
                            reads=[w_kvb_b, OnT], writes=[pb])
                p.act.op(lambda e: e.copy(out=ohT[:, 0:4, :], in_=ps[0][:, :].rearrange("p (h n) -> p h n", h=4)),
                         reads=[ps[0]], writes=[ohT])
                p.dve.op(lambda e: e.tensor_copy(out=ohT[:, 4:8, :], in_=ps[1][:, :].rearrange("p (h n) -> p h n", h=4)),
                         reads=[ps[1]], writes=[ohT])
                xt = xout_t[i % 2]
                for half in range(2):
                    pb = ps[6 + half]
                    for h in range(8):
                        p.pe.op(lambda e, h=h, half=half, pb=pb: e.matmul(out=pb[:, :], lhsT=ohT[:, h, :],
                                                                          rhs=w_out_b[:, h, half * 512:(half + 1) * 512],
                                                                          start=(h == 0), stop=(h == 7)),
                                reads=[ohT, w_out_b], writes=[pb])
                    p.dve.op(lambda e, half=half, pb=pb, xt=xt, x=x: e.tensor_tensor(
                        out=xt[:, half * 512:(half + 1) * 512], in0=pb[:, :], in1=x[:, half * 512:(half + 1) * 512], op=ALU.add),
                        reads=[pb, x], writes=[xt])
                p.sp.dma(xa_out[i * 128:(i + 1) * 128, :], xt[:, :], reads=[xt], writes=[xa_out])
            p.barrier()


NC_TRI, NC_BLK, NC_NEG, NC_MA2, NC_MB2, NC_M3 = 0, 128, 256, 384, 512, 640
NCONST = 643


def mlstm_consts_np():
    s = np.arange(128)[:, None]
    t = np.arange(128)[None, :]
    same = (s // 64) == (t // 64)
    c = np.zeros((128, NCONST), np.float32)
    c[:, NC_TRI:NC_TRI + 128] = (same & (s <= t))
    c[:, NC_BLK:NC_BLK + 128] = same
    c[:, NC_NEG:NC_NEG + 128] = np.where(same & (s <= t), 0.0, -30000.0)
    c[:, NC_MA2:NC_MA2 + 128] = (s < 64) & (t < 64)
    c[:, NC_MB2:NC_MB2 + 128] = (s >= 64) & (t >= 64)
    c[:, NC_M3 + 0] = 1.0
    c[:, NC_M3 + 1] = (np.arange(128) < 64)
    c[:, NC_M3 + 2] = (np.arange(128) >= 64)
    return c


def phase_mlstm(p, x_full, g_norm, w_h, cw, cb, gate_b, head_norm, consts, y_out, NTILE=128, xnT_all=None, yT_send=None):
    nc = p.nc
    with ExitStack() as st:
        w_b = p.sb(st, "m_w", [128, 8, 514], BF16)
        gbc = p.sb(st, "m_gbc", [128, D], F32)
        cst = p.sb(st, "m_cst", [128, NCONST], F32)
        cw_t = p.sb(st, "m_cw", [128, 8], F32)
        cb_t = p.sb(st, "m_cb", [128, 2], F32)
        gb_bc = p.sb(st, "m_gb", [128, 2], F32)
        hn_bc = p.sb(st, "m_hn", [128, 128], F32)
        ones_f = p.sb(st, "m_ones", [128, 128], F32)
        one_c = p.sb(st, "m_one_c", [128, 1], F32)
        ps = [p.ps(st, "mps%d" % i, [128, 512], F32) for i in range(8)]

        p.pool.dma(w_b[:, :, :], w_h.t.rearrange("(dc p) f -> p dc f", p=128), reads=[w_h], writes=[w_b])
        p.sp.dma(gbc[:, :], g_norm.t.partition_broadcast(128), reads=[g_norm], writes=[gbc])
        p.sp.dma(cst[:, :], consts.t, reads=[consts], writes=[cst])
        p.sp.dma(cw_t[:, :], cw.t, reads=[cw], writes=[cw_t])
        p.sp.dma(cb_t[:, :], cb.t, reads=[cb], writes=[cb_t])
        p.sp.dma(gb_bc[:, :], gate_b.t.partition_broadcast(128), reads=[gate_b], writes=[gb_bc])
        p.sp.dma(hn_bc[:, :], head_norm.t.partition_broadcast(128), reads=[head_norm], writes=[hn_bc])
        p.pool.op(lambda e: e.memset(ones_f[:, :], 1.0), writes=[ones_f])
        p.pool.op(lambda e: e.memset(one_c[:, :], 1.0), writes=[one_c])

        Tri = cst[:, NC_TRI:NC_TRI + 128]
        Blk = cst[:, NC_BLK:NC_BLK + 128]
        Neg = cst[:, NC_NEG:NC_NEG + 128]
        MA2 = cst[:, NC_MA2:NC_MA2 + 128]
        MB2 = cst[:, NC_MB2:NC_MB2 + 128]
        M3 = cst[:, NC_M3:NC_M3 + 3]

        xs = [p.sb(st, "m_xs%d" % i, [128, D], F32) for i in range(3)]
        junk = [p.sb(st, "m_junk%d" % i, [128, D], BF16) for i in range(2)]
        xn = [p.sb(st, "m_xn%d" % i, [128, D], BF16) for i in range(2)]
        xnT = [p.sb(st, "m_xnT%d" % i, [128, 8, 128], BF16) for i in range(2)]
        ss = [p.sb(st, "m_ss%d" % i, [128, 2], F32) for i in range(2)]
        raw = [p.sb(st, "m_raw%d" % i, [128, 2, 131], F32) for i in range(2)]
        acc = p.sb(st, "m_acc", [128, 2, 128], F32)
        sg = p.sb(st, "m_sg", [128, 2, 128], F32)
        qT_l = [p.sb(st, "m_qT%d" % i, [128, 128], F32) for i in range(2)]
        kT_l = [p.sb(st, "m_kT%d" % i, [128, 128], F32) for i in range(2)]
        qbA_l = [p.sb(st, "m_qbA%d" % i, [128, 128], F32) for i in range(2)]
        qbB_l = [p.sb(st, "m_qbB%d" % i, [128, 128], F32) for i in range(2)]
        kwA_l = [p.sb(st, "m_kwA%d" % i, [128, 128], F32) for i in range(2)]
        kwB_l = [p.sb(st, "m_kwB%d" % i, [128, 128], F32) for i in range(2)]
        v1_l = [p.sb(st, "m_v1%d" % i, [128, 129], F32) for i in range(2)]
        so_l = [p.sb(st, "m_so%d" % i, [128, 128], F32) for i in range(3)]
        gt = p.sb(st, "m_gt", [128, 2], F32)
        g2 = p.sb(st, "m_g2", [128, 2], F32)
        lf3 = p.sb(st, "m_lf3", [128, 3], F32)
        lfc = p.sb(st, "m_lfc", [128, 1], F32)
        g4 = p.sb(st, "m_g4", [128, 4], F32)
        F2_l = [p.sb(st, "m_F2%d" % i, [128, 2], F32) for i in range(2)]
        acol = p.sb(st, "m_acol", [128, 1], F32)
        wcol = p.sb(st, "m_wcol", [128, 1], F32)
        wAB = p.sb(st, "m_wAB", [128, 2], F32)
        eb_l = [p.sb(st, "m_eb%d" % i, [128, 1], F32) for i in range(2)]
        trilf = p.sb(st, "m_trilf", [128, 128], F32)
        DT_l = [p.sb(st, "m_DT%d" % i, [128, 128], F32) for i in range(2)]
        wts = p.sb(st, "m_wts", [128, 128], F32)
        intra = p.sb(st, "m_intra", [128, 129], F32)
        tot_l = [p.sb(st, "m_tot%d" % i, [128, 129], F32) for i in range(2)]
        jB = p.sb(st, "m_jB", [128, 128], BF16)
        sm = p.sb(st, "m_sm", [128, 6], F32)
        yt = [p.sb(st, "m_yt%d" % i, [128, 128], F32) for i in range(2)]
        SA = p.sb(st, "m_SA", [128, 129], F32)
        SB = p.sb(st, "m_SB", [128, 129], F32)

        p.pool.op(lambda e: e.memset(SA[:, :], 0.0), writes=[SA])
        p.pool.op(lambda e: e.memset(raw[0][:, :, :], 0.0), writes=[raw[0]])
        for v1 in v1_l:
            p.pool.op(lambda e, v1=v1: e.memset(v1[:, 128:129], 1.0), writes=[v1])

        xT3 = [p.sb(st, "m_xT3_%d" % i, [128, 8, 128], BF16) for i in range(3)] if xnT_all is not None else None
        yTb = [p.sb(st, "m_yTb%d" % i, [128, 128], BF16) for i in range(2)] if yT_send is not None else None

        def load_x(T):
            if xnT_all is not None:
                rb = ((T % 8) * 16 + T // 8) * 128
                p.sp.dma(xT3[T % 3][:, :, :], xnT_all.t[rb:rb + 128, :].rearrange("p (c n) -> p c n", c=8),
                         reads=[xnT_all], writes=[xT3[T % 3]])
            else:
                p.sp.dma(xs[T % 3][:, :], x_full[T * 128:(T + 1) * 128, :], reads=[x_full], writes=[xs[T % 3]])

        load_x(0)
        if NTILE > 1:
            load_x(1)

        def FRONT(T):
            if T + 2 < NTILE:
                load_x(T + 2)
            qT = qT_l[T % 2]; kT = kT_l[T % 2]; qbA = qbA_l[T % 2]; qbB = qbB_l[T % 2]; kwA = kwA_l[T % 2]; kwB = kwB_l[T % 2]
            v1 = v1_l[T % 2]; so = so_l[T % 3]; F2 = F2_l[T % 2]; eb = eb_l[T % 2]; DT = DT_l[T % 2]
            x = xs[T % 3]
            s_ = ss[T % 2]
            j = junk[T % 2]
            x_n = xn[T % 2]
            xT = xnT[T % 2]
            rw = raw[T % 2]
            rwn = raw[(T + 1) % 2]
            if xnT_all is not None:
                xT = xT3[T % 3]
            else:
                p.act.op(lambda e, x=x, j=j, s_=s_: e.activation(out=j[:, :], in_=x[:, :], func=AF.Square, accum_out=s_[:, 0:1]),
                         reads=[x], writes=[j, s_])
                p.act.op(lambda e, s_=s_: e.activation(out=s_[:, 1:2], in_=s_[:, 0:1], func=AF.Ln, scale=1.0 / D, bias=p.eps_t[:, 0:1]),
                         reads=[s_, p.eps_t], writes=[s_])
                p.act.op(lambda e, s_=s_: e.activation(out=s_[:, 1:2], in_=s_[:, 1:2], func=AF.Exp, scale=-0.5), reads=[s_], writes=[s_])
                p.dve.op(lambda e, x=x, x_n=x_n, s_=s_: e.scalar_tensor_tensor(
                    out=x_n[:, :], in0=x[:, :], scalar=s_[:, 1:2], in1=gbc[:, :], op0=ALU.mult, op1=ALU.mult),
                    reads=[x, s_, gbc], writes=[x_n])
                pvv = ps[0].t[:, :].bitcast(BF16)
                for dc in range(8):
                    p.pe.op(lambda e, dc=dc, x_n=x_n: e.transpose(out=pvv[:, dc * 128:(dc + 1) * 128], in_=x_n[:, dc * 128:(dc + 1) * 128],
                                                                 identity=p.ident_b[:, :]), reads=[x_n, p.ident_b], writes=[ps[0]])
                p.act.op(lambda e, xT=xT: e.copy(out=xT[:, :, :], in_=pvv.rearrange("p (c n) -> p c n", c=8)), reads=[ps[0]], writes=[xT])
            for qk in range(2):
                for dc in range(8):
                    p.pe.op(lambda e, dc=dc, qk=qk, xT=xT: e.matmul(out=ps[1][:, qk * 128:(qk + 1) * 128],
                                                                    lhsT=w_b[:, dc, qk * 128:(qk + 1) * 128], rhs=xT[:, dc, :],
                                                                    start=(dc == 0), stop=(dc == 7)), reads=[w_b, xT], writes=[ps[1]])
            for dc in range(8):
                p.pe.op(lambda e, dc=dc, xT=xT: e.matmul(out=ps[2][:, 0:258], lhsT=xT[:, dc, :], rhs=w_b[:, dc, 256:514],
                                                         start=(dc == 0), stop=(dc == 7)), reads=[xT, w_b], writes=[ps[2]])
            p.dve.op(lambda e: e.tensor_tensor(out=gt[:, :], in0=ps[2][:, 256:258], in1=gb_bc[:, :], op=ALU.add),
                     reads=[ps[2], gb_bc], writes=[gt])
            p.act.op(lambda e: e.activation(out=g2[:, :], in_=gt[:, :], func=AF.Exp, scale=2.0 / 15.0), reads=[gt], writes=[g2])
            p.dve.op(lambda e: e.tensor_scalar(out=g2[:, :], in0=g2[:, :], scalar1=1.0, scalar2=None, op0=ALU.add), reads=[g2], writes=[g2])
            p.dve.op(lambda e: e.reciprocal(out=g2[:, :], in_=g2[:, :]), reads=[g2], writes=[g2])
            p.dve.op(lambda e: e.tensor_scalar(out=gt[:, :], in0=g2[:, :], scalar1=-30.0, scalar2=15.0, op0=ALU.mult, op1=ALU.add),
                     reads=[g2], writes=[gt])
            p.act.op(lambda e: e.activation(out=lfc[:, :], in_=gt[:, 1:2], func=AF.Exp, scale=-1.0), reads=[gt], writes=[lfc])
            p.act.op(lambda e: e.activation(out=lfc[:, :], in_=lfc[:, :], func=AF.Ln, bias=one_c[:, 0:1]), reads=[lfc, one_c], writes=[lfc])
            p.dve.op(lambda e: e.tensor_scalar(out=lf3[:, :], in0=M3, scalar1=lfc[:, 0:1], scalar2=-1.0, op0=ALU.mult, op1=ALU.mult),
                     reads=[cst, lfc], writes=[lf3])
            p.dve.op(lambda e: e.tensor_scalar(out=trilf[:, :], in0=Tri, scalar1=lf3[:, 0:1], scalar2=None, op0=ALU.mult),
                     reads=[cst, lf3], writes=[trilf])
            p.pe.op(lambda e: e.matmul(out=ps[3][:, 0:1], lhsT=Tri, rhs=lf3[:, 0:1], start=True, stop=True), reads=[cst, lf3], writes=[ps[3]])
            p.pe.op(lambda e: e.matmul(out=ps[3][:, 1:2], lhsT=Blk, rhs=lf3[:, 0:1], start=True, stop=True), reads=[cst, lf3], writes=[ps[3]])
            p.pe.op(lambda e: e.matmul(out=ps[3][:, 2:4], lhsT=ones_f[:, :], rhs=lf3[:, 1:3], start=True, stop=True),
                    reads=[ones_f, lf3], writes=[ps[3]])
            p.act.op(lambda e: e.copy(out=g4[:, :], in_=ps[3][:, 0:4]), reads=[ps[3]], writes=[g4])
            p.act.op(lambda e: e.activation(out=F2[:, :], in_=g4[:, 2:4], func=AF.Exp), reads=[g4], writes=[F2])
            p.dve.op(lambda e: e.tensor_tensor(out=acol[:, :], in0=gt[:, 0:1], in1=g4[:, 0:1], op=ALU.subtract), reads=[gt, g4], writes=[acol])
            p.act.op(lambda e: e.activation(out=wcol[:, :], in_=g4[:, 1:2], func=AF.Exp, bias=acol[:, 0:1]), reads=[g4, acol], writes=[wcol])
            p.act.op(lambda e: e.activation(out=eb[:, :], in_=g4[:, 0:1], func=AF.Exp), reads=[g4], writes=[eb])
            p.dve.op(lambda e: e.tensor_scalar(out=wAB[:, :], in0=M3[:, 1:3], scalar1=wcol[:, 0:1], scalar2=None, op0=ALU.mult),
                     reads=[cst, wcol], writes=[wAB])
            p.pe.op(lambda e: e.matmul(out=ps[4][:, 0:128], lhsT=ones_f[:, :], rhs=trilf[:, :], start=True, stop=False),
                    reads=[ones_f, trilf], writes=[ps[4]])
            p.pe.op(lambda e: e.matmul(out=ps[4][:, 0:128], lhsT=p.ident_f[:, :], rhs=Neg, start=False, stop=True),
                    reads=[p.ident_f, cst], writes=[ps[4]])
            p.act.op(lambda e: e.activation(out=DT[:, :], in_=ps[4][:, 0:128], func=AF.Exp, bias=acol[:, 0:1]),
                     reads=[ps[4], acol], writes=[DT])
            p.act.op(lambda e, rw=rw: e.copy(out=rw[:, :, 3:131], in_=ps[1][:, 0:256].rearrange("p (a n) -> p a n", a=2)),
                     reads=[ps[1]], writes=[rw])
            p.pool.op(lambda e, rw=rw, rwn=rwn: e.tensor_copy(out=rwn[:, :, 0:3], in_=rw[:, :, 128:131]), reads=[rw], writes=[rwn])
            for qk in range(2):
                p.dve.op(lambda e, qk=qk, rw=rw: e.tensor_scalar(out=acc[:, qk, :], in0=rw[:, qk, 3:131], scalar1=cw_t[:, qk * 4 + 3:qk * 4 + 4],
                                                                 scalar2=cb_t[:, qk:qk + 1], op0=ALU.mult, op1=ALU.add),
                         reads=[rw, cw_t, cb_t], writes=[acc])
                for dlt in range(1, 4):
                    p.dve.op(lambda e, qk=qk, rw=rw, dlt=dlt: e.scalar_tensor_tensor(
                        out=acc[:, qk, :], in0=rw[:, qk, 3 - dlt:131 - dlt], scalar=cw_t[:, qk * 4 + 3 - dlt:qk * 4 + 4 - dlt],
                        in1=acc[:, qk, :], op0=ALU.mult, op1=ALU.add), reads=[rw, cw_t, acc], writes=[acc])
            p.act.op(lambda e: e.activation(out=sg[:, :, :], in_=acc[:, :, :], func=AF.Exp, scale=-1.0), reads=[acc], writes=[sg])
            p.dve.op(lambda e: e.tensor_scalar(out=sg[:, :, :], in0=sg[:, :, :], scalar1=1.0, scalar2=None, op0=ALU.add), reads=[sg], writes=[sg])
            p.dve.op(lambda e: e.reciprocal(out=sg[:, :, :], in_=sg[:, :, :]), reads=[sg], writes=[sg])
            p.dve.op(lambda e: e.scalar_tensor_tensor(out=qT[:, :], in0=acc[:, 0, :], scalar=0.125, in1=sg[:, 0, :], op0=ALU.mult, op1=ALU.mult),
                     reads=[acc, sg], writes=[qT])
            p.dve.op(lambda e: e.tensor_tensor(out=kT[:, :], in0=acc[:, 1, :], in1=sg[:, 1, :], op=ALU.mult), reads=[acc, sg], writes=[kT])
            p.pool.op(lambda e: e.tensor_tensor(out=qbA[:, :], in0=qT[:, :], in1=MA2, op=ALU.mult), reads=[qT, cst], writes=[qbA])
            p.pool.op(lambda e: e.tensor_tensor(out=qbB[:, :], in0=qT[:, :], in1=MB2, op=ALU.mult), reads=[qT, cst], writes=[qbB])
            p.pe.op(lambda e: e.transpose(out=ps[3][:, 128:256], in_=kT[:, :], identity=p.ident_f[:, :]), reads=[kT, p.ident_f], writes=[ps[3]])
            p.act.op(lambda e: e.activation(out=kwA[:, :], in_=ps[3][:, 128:256], func=AF.Copy, scale=wAB[:, 0:1]), reads=[ps[3], wAB], writes=[kwA])
            p.dve.op(lambda e: e.tensor_scalar(out=kwB[:, :], in0=ps[3][:, 128:256], scalar1=wAB[:, 1:2], scalar2=None, op0=ALU.mult),
                     reads=[ps[3], wAB], writes=[kwB])
            p.act.op(lambda e: e.copy(out=v1[:, 0:128], in_=ps[2][:, 0:128]), reads=[ps[2]], writes=[v1])
            p.act.op(lambda e: e.activation(out=so[:, :], in_=ps[2][:, 128:256], func=AF.Exp, scale=-1.0), reads=[ps[2]], writes=[so])
            p.pool.op(lambda e: e.tensor_scalar(out=so[:, :], in0=so[:, :], scalar1=1.0, scalar2=None, op0=ALU.add), reads=[so], writes=[so])
            p.dve.op(lambda e: e.reciprocal(out=so[:, :], in_=so[:, :]), reads=[so], writes=[so])

        def MIDDLE(T):
            qT = qT_l[T % 2]; kT = kT_l[T % 2]; qbA = qbA_l[T % 2]; qbB = qbB_l[T % 2]; kwA = kwA_l[T % 2]; kwB = kwB_l[T % 2]
            v1 = v1_l[T % 2]; F2 = F2_l[T % 2]; eb = eb_l[T % 2]; DT = DT_l[T % 2]; tot = tot_l[T % 2]
            p.pe.op(lambda e: e.matmul(out=ps[5][:, 0:128], lhsT=kT[:, :], rhs=qT[:, :], start=True, stop=True), reads=[kT, qT], writes=[ps[5]])
            p.dve.op(lambda e: e.scalar_tensor_tensor(out=wts[:, :], in0=ps[5][:, 0:128], scalar=0.5, in1=DT[:, :], op0=ALU.mult, op1=ALU.mult),
                     reads=[ps[5], DT], writes=[wts])
            p.pe.op(lambda e: e.matmul(out=ps[7][:, 0:129], lhsT=kwA[:, :], rhs=v1[:, :], start=True, stop=True), reads=[kwA, v1], writes=[ps[7]])
            p.dve.op(lambda e: e.scalar_tensor_tensor(out=SB[:, :], in0=SA[:, :], scalar=F2[:, 0:1], in1=ps[7][:, 0:129], op0=ALU.mult, op1=ALU.add),
                     reads=[SA, F2, ps[7]], writes=[SB])
            p.pe.op(lambda e: e.matmul(out=ps[6][:, 0:129], lhsT=wts[:, :], rhs=v1[:, :], start=True, stop=True), reads=[wts, v1], writes=[ps[6]])
            p.pe.op(lambda e: e.matmul(out=ps[6][:, 256:385], lhsT=qbA[:, :], rhs=SA[:, :], start=True, stop=False), reads=[qbA, SA], writes=[ps[6]])
            p.pe.op(lambda e: e.matmul(out=ps[6][:, 256:385], lhsT=qbB[:, :], rhs=SB[:, :], start=False, stop=True), reads=[qbB, SB], writes=[ps[6]])
            p.act.op(lambda e: e.copy(out=intra[:, :], in_=ps[6][:, 0:129]), reads=[ps[6]], writes=[intra])
            p.dve.op(lambda e: e.scalar_tensor_tensor(out=tot[:, :], in0=ps[6][:, 256:385], scalar=eb[:, 0:1], in1=intra[:, :], op0=ALU.mult, op1=ALU.add),
                     reads=[ps[6], eb, intra], writes=[tot])
            p.pe.op(lambda e: e.matmul(out=ps[7][:, 256:385], lhsT=kwB[:, :], rhs=v1[:, :], start=True, stop=True), reads=[kwB, v1], writes=[ps[7]])
            p.dve.op(lambda e: e.scalar_tensor_tensor(out=SA[:, :], in0=SB[:, :], scalar=F2[:, 1:2], in1=ps[7][:, 256:385], op0=ALU.mult, op1=ALU.add),
                     reads=[SB, F2, ps[7]], writes=[SA])

        def BACK(T):
            so = so_l[T % 3]; tot = tot_l[T % 2]; j = jB
            y = yt[T % 2]
            p.dve.op(lambda e: e.scalar_tensor_tensor(out=sm[:, 0:1], in0=tot[:, 128:129], scalar=-1.0, in1=tot[:, 128:129], op0=ALU.mult, op1=ALU.max), reads=[tot], writes=[sm])
            p.dve.op(lambda e: e.tensor_scalar(out=sm[:, 0:1], in0=sm[:, 0:1], scalar1=1.0, scalar2=None, op0=ALU.max), reads=[sm], writes=[sm])
            p.dve.op(lambda e: e.reciprocal(out=sm[:, 1:2], in_=sm[:, 0:1]), reads=[sm], writes=[sm])
            p.act.op(lambda e, j=j: e.activation(out=j[:, 0:128], in_=tot[:, 0:128], func=AF.Square, accum_out=sm[:, 2:3]), reads=[tot], writes=[j, sm])
            p.dve.op(lambda e: e.tensor_scalar(out=sm[:, 3:4], in0=sm[:, 1:2], scalar1=sm[:, 1:2], scalar2=1.0 / 128, op0=ALU.mult, op1=ALU.mult),
                     reads=[sm], writes=[sm])
            p.act.op(lambda e: e.activation(out=sm[:, 4:5], in_=sm[:, 2:3], func=AF.Ln, scale=sm[:, 3:4], bias=p.eps_t[:, 0:1]),
                     reads=[sm, p.eps_t], writes=[sm])
            p.act.op(lambda e: e.activation(out=sm[:, 4:5], in_=sm[:, 4:5], func=AF.Exp, scale=-0.5), reads=[sm], writes=[sm])
            p.dve.op(lambda e: e.tensor_tensor(out=sm[:, 5:6], in0=sm[:, 4:5], in1=sm[:, 1:2], op=ALU.mult), reads=[sm], writes=[sm])
            p.dve.op(lambda e, y=y: e.scalar_tensor_tensor(out=y[:, :], in0=tot[:, 0:128], scalar=sm[:, 5:6], in1=hn_bc[:, :], op0=ALU.mult, op1=ALU.mult),
                     reads=[tot, sm, hn_bc], writes=[y])
            p.pool.op(lambda e, y=y: e.tensor_tensor(out=y[:, :], in0=y[:, :], in1=so[:, :], op=ALU.mult), reads=[y, so], writes=[y])
            if yT_send is not None:
                yb = yTb[T % 2]
                p.pe.op(lambda e, y=y: e.transpose(out=ps[0][:, 0:128], in_=y[:, :], identity=p.ident_f[:, :]),
                        reads=[y, p.ident_f], writes=[ps[0]])
                p.act.op(lambda e, yb=yb: e.copy(out=yb[:, :], in_=ps[0][:, 0:128]), reads=[ps[0]], writes=[yb])
                c0 = (T % 8) * 2048 + (T // 8) * 128
                p.sp.dma(yT_send.t[:, c0:c0 + 128], yb[:, :], reads=[yb], writes=[yT_send])
            else:
                p.sp.dma(y_out[T * 128:(T + 1) * 128, :], y[:, :], reads=[y], writes=[y_out])

        for it in range(NTILE + 2):
            if it < NTILE:
                FRONT(it)
            if 0 <= it - 1 < NTILE:
                MIDDLE(it - 1)
            if 0 <= it - 2 < NTILE:
                BACK(it - 2)
        p.barrier()

N_CORES = 8
S_TOK = 16384


def _make_masks(c):
    m = np.zeros((8, 128, 4, 128), np.float32)
    k = np.arange(128)[:, None]
    q = np.arange(128)[None, :]
    for j in range(8):
        if j == c:
            m[j] = np.where(k <= q, 0.0, -30000.0)[:, None, :]
        elif j > c:
            m[j] = -30000.0
    return m.reshape(8, 128, 512)


def build_A():
    p = Prog()
    with ExitStack() as st:
        setup_consts(p, st)
        di = lambda n, s, dt=F32: p.dram(n, s, dt, "ExternalInput")
        x_full = di("x_full", [S_TOK, 1024]); x_own = di("x_own", [2048, 1024])
        posT = di("posT", [128, 128], I32); pos_own = di("pos_own", [128, 16], I32)
        inv_freq = di("inv_freq", [32]); masks = di("masks", [8, 128, 512])
        g_mix0 = di("g_mix0", [1024]); w_in = di("w_in", [1024, 448]); q_norm = di("q_norm", [256])
        w_qb = di("w_qb", [256, 1536]); kv_norm = di("kv_norm", [128]); w_kvb = di("w_kvb", [128, 2048])
        w_out = di("w_out", [1024, 1024])
        g_ffn0 = di("g_ffn0", [1024]); fwg = di("fwg", [1, 1024, 3584]); fwu = di("fwu", [1, 1024, 3584]); fwd = di("fwd", [1, 3584, 1024])
        g_mix1 = di("g_mix1", [1024])
        x1_s = p.dram("x1_s", [2048, 1024], F32, "ExternalOutput")
        xnT_send = p.dram("xnT_send", [2048, 1024], BF16, "ExternalOutput")
        xa_s = p.dram_internal("xa_s", [2048, 1024], F32)
        phase_attn(p, x_full, x_own, posT, pos_own, inv_freq, masks, g_mix0, w_in, q_norm, w_qb, kv_norm, w_kvb, w_out, xa_s)
        phase_ffn(p, xa_s, x1_s, g_ffn0, fwg, fwu, fwd, 1)
        phase_norm_send(p, x1_s, g_mix1, xnT_send)
        nc = p.finish()
    return nc


def build_B():
    p = Prog()
    with ExitStack() as st:
        setup_consts(p, st)
        di = lambda n, s, dt=F32: p.dram(n, s, dt, "ExternalInput")
        xnT_all = di("xnT_all", [8 * 2048, 1024], BF16)
        g_mix1 = di("g_mix1", [1024]); w_h = di("w_h", [1024, 514]); cw = di("cw", [128, 8]); cb = di("cb", [128, 2])
        gb = di("gate_b", [2]); hn = di("head_norm", [128]); cs = di("consts", [128, NCONST])
        yT_send = p.dram("yT_send", [128, S_TOK], BF16, "ExternalOutput")
        phase_mlstm(p, None, g_mix1, w_h, cw, cb, gb, hn, cs, None, xnT_all=xnT_all, yT_send=yT_send)
        nc = p.finish()
    return nc


def build_C():
    p = Prog()
    with ExitStack() as st:
        setup_consts(p, st)
        di = lambda n, s, dt=F32: p.dram(n, s, dt, "ExternalInput")
        x1_s = di("x1_s", [2048, 1024]); yT_all = di("yT_all", [8 * 128, S_TOK], BF16); m_wo = di("m_wo", [1024, 1024])
        g_ffn1 = di("g_ffn1", [1024]); mwg = di("mwg", [8, 1024, 3584]); mwu = di("mwu", [8, 1024, 3584]); mwd = di("mwd", [8, 3584, 1024])
        rt = di("rt", [8, 1024]); gf = di("gf", [1024])
        xout = p.dram("xout", [2048, 1024], F32, "ExternalOutput")
        phase_ffn(p, x1_s, xout, g_ffn1, mwg, mwu, mwd, 8, routerT=rt, final_g=gf, ymT=yT_all, w_o=m_wo, ymT_dyn=True)
        nc = p.finish()
    return nc


def make_maps(inp):
    x = np.ascontiguousarray(inp["x"][0].astype(np.float32, copy=False))
    xt = x.reshape(128, 128, 1024)
    post = np.ascontiguousarray(inp["positions"][0].astype(np.int32)).reshape(128, 128)
    inv = (1.0 / (10000.0 ** (np.arange(0, 64, 2, dtype=np.float32) / 64))).astype(np.float32)
    w_in = inp["mlstm_w_in"][0]; conv_w = inp["mlstm_conv_w"][0]; conv_b = inp["mlstm_conv_b"][0]
    gate_b = inp["mlstm_gate_b"][0]; head_norm = inp["mlstm_head_norm"][0]
    consts = mlstm_consts_np()
    posT = np.ascontiguousarray(post.T)
    rtT = np.ascontiguousarray(inp["moe_router"][0].T)
    maps = []
    for c in range(N_CORES):
        own = [8 * i + c for i in range(16)]
        h = c
        q = w_in[:, h * 64:(h + 1) * 64]; k = w_in[:, 512 + h * 64:512 + (h + 1) * 64]
        v = w_in[:, 1024 + h * 128:1024 + (h + 1) * 128]; o = w_in[:, 2048 + h * 128:2048 + (h + 1) * 128]
        ig = w_in[:, 3072 + h:3073 + h]; fg = w_in[:, 3080 + h:3081 + h]
        wh = np.ascontiguousarray(np.concatenate([q, q, k, k, v, o, ig, fg], axis=1))
        cq = conv_w[:, h * 64:(h + 1) * 64].T; ck = conv_w[:, 512 + h * 64:512 + (h + 1) * 64].T
        cwh = np.ascontiguousarray(np.concatenate([np.concatenate([cq, cq], 0), np.concatenate([ck, ck], 0)], axis=1))
        bq = conv_b[h * 64:(h + 1) * 64]; bk = conv_b[512 + h * 64:512 + (h + 1) * 64]
        cbh = np.ascontiguousarray(np.stack([np.concatenate([bq, bq]), np.concatenate([bk, bk])], axis=1))
        maps.append({
            "x_full": x, "x_own": np.ascontiguousarray(xt[own].reshape(-1, 1024)),
            "posT": posT, "pos_own": np.ascontiguousarray(post[own].T), "inv_freq": inv, "masks": _make_masks(c),
            "g_mix0": inp["norm_mix"][0], "w_in": inp["mla_w_in"][0], "q_norm": inp["mla_q_norm"][0], "w_qb": inp["mla_w_qb"][0],
            "kv_norm": inp["mla_kv_norm"][0], "w_kvb": inp["mla_w_kvb"][0], "w_out": inp["mla_w_out"][0],
            "g_ffn0": inp["norm_ffn"][0], "fwg": inp["ffn_w_gate"], "fwu": inp["ffn_w_up"], "fwd": inp["ffn_w_down"],
            "g_mix1": inp["norm_mix"][1], "w_h": wh, "cw": cwh, "cb": cbh,
            "gate_b": np.ascontiguousarray(np.stack([gate_b[h], gate_b[8 + h]])),
            "head_norm": np.ascontiguousarray(head_norm[h * 128:(h + 1) * 128]), "consts": consts, "m_wo": inp["mlstm_w_out"][0],
            "g_ffn1": inp["norm_ffn"][1], "mwg": inp["moe_w_gate"][0], "mwu": inp["moe_w_up"][0], "mwd": inp["moe_w_down"][0],
            "rt": rtT, "gf": inp["final_norm"],
        })
    return maps


def kernel(**inputs):
    inp = {k: np.asarray(v) for k, v in inputs.items()}
    maps = make_maps(inp)
    ka = ["x_full", "x_own", "posT", "pos_own", "inv_freq", "masks", "g_mix0", "w_in", "q_norm", "w_qb", "kv_norm", "w_kvb", "w_out",
          "g_ffn0", "fwg", "fwu", "fwd", "g_mix1"]
    ra = run_bass_kernel_spmd(build_A(), [{k: m[k] for k in ka} for m in maps], core_ids=list(range(N_CORES))).results
    xnT_all = np.concatenate([ra[c]["xnT_send"] for c in range(N_CORES)], axis=0)
    kb = ["g_mix1", "w_h", "cw", "cb", "gate_b", "head_norm", "consts"]
    rb = run_bass_kernel_spmd(build_B(), [dict({k: m[k] for k in kb}, xnT_all=xnT_all) for m in maps], core_ids=list(range(N_CORES))).results
    yT_all = np.concatenate([rb[c]["yT_send"] for c in range(N_CORES)], axis=0)
    kc = ["m_wo", "g_ffn1", "mwg", "mwu", "mwd", "rt", "gf"]
    rc = run_bass_kernel_spmd(build_C(), [dict({k: m[k] for k in kc}, x1_s=ra[c]["x1_s"], yT_all=yT_all) for c, m in enumerate(maps)],
                              core_ids=list(range(N_CORES))).results
    out = np.empty((128, 128, 1024), np.float32)
    for c in range(N_CORES):
        own = [8 * i + c for i in range(16)]
        out[own] = rc[c]["xout"].reshape(16, 128, 1024)
    return out.reshape(1, S_TOK, 1024)
```

```python
from contextlib import ExitStack
import numpy as np
import concourse.bass as bass
import concourse.mybir as mybir
from concourse.bass_utils import run_bass_kernel_spmd

F32 = mybir.dt.float32
BF16 = mybir.dt.bfloat16
I32 = mybir.dt.int32
AF = mybir.ActivationFunctionType
ALU = mybir.AluOpType
AX = mybir.AxisListType


class T:
    def __init__(self, t, name):
        self.t = t
        self.name = name
        self.w = {}
        self.r = {}

    def __getitem__(self, k):
        return self.t[k]


class Eng:
    def __init__(self, prog, name, eng, nring):
        self.p = prog
        self.name = name
        self.eng = eng
        self.sem = prog.new_sem("c_" + name)
        self.count = 0
        self.seen = {}
        self.ring = [prog.new_sem("d_%s%d" % (name, i)) for i in range(nring)]
        self.ring_cnt = [0] * nring
        self.ring_pos = 0

    def _wait(self, mark):
        if mark is None:
            return
        sem, val = mark
        k = id(sem)
        if self.seen.get(k, 0) < val:
            self.eng.wait_ge(sem, val)
            self.seen[k] = val

    def _deps(self, reads, writes, skip_self, dma_ring=None):
        for b in reads:
            for m in b.w.values():
                if not (skip_self and m[0] is self.sem):
                    self._wait(m)
        for b in writes:
            for m in b.w.values():
                if dma_ring is not None and id(m[0]) in dma_ring:
                    continue
                if not (skip_self and m[0] is self.sem):
                    self._wait(m)
            for m in b.r.values():
                if not (skip_self and m[0] is self.sem):
                    self._wait(m)

    def _mark(self, reads, writes, mark, dma_ring=None):
        for b in reads:
            b.r[id(mark[0])] = mark
        for b in writes:
            if dma_ring is not None:
                b.w = {k: v for k, v in b.w.items() if k in dma_ring}
            else:
                b.w = {}
            b.w[id(mark[0])] = mark
            b.r = {}

    def op(self, fn, reads=(), writes=()):
        ex = [b for b in reads if getattr(b, "excl", False)]
        if ex:
            reads = [b for b in reads if not getattr(b, "excl", False)]
            writes = list(writes) + ex
        self._deps(reads, writes, skip_self=(self.name == "pe"))
        inst = fn(self.eng)
        self.count += 1
        inst.then_inc(self.sem, 1)
        self._mark(reads, writes, (self.sem, self.count))

    def dma(self, out, in_, reads=(), writes=(), **kw):
        ring_ids = set(id(x) for x in self.ring)
        self._deps(reads, writes, skip_self=False, dma_ring=ring_ids)
        j = self.ring_pos
        self.ring_pos = (j + 1) % len(self.ring)
        sem = self.ring[j]
        if self.ring_cnt[j] > 0:
            self._wait((sem, self.ring_cnt[j]))
        inst = self.eng.dma_start(out=out, in_=in_, **kw)
        self.ring_cnt[j] += 16
        inst.then_inc(sem, 16)
        self._mark(reads, writes, (sem, self.ring_cnt[j]), dma_ring=ring_ids)

    def wait_all(self, marks):
        for m in marks:
            self._wait(m)


class Prog:
    def __init__(self):
        self.nc = bass.Bass("TRN2", target_bir_lowering=False)
        self.es = ExitStack()
        self.nsem = 0
        nc = self.nc
        self.pe = Eng(self, "pe", nc.tensor, 0)
        self.act = Eng(self, "act", nc.scalar, 8)
        self.dve = Eng(self, "dve", nc.vector, 0)
        self.pool = Eng(self, "pool", nc.gpsimd, 24)
        self.sp = Eng(self, "sp", nc.sync, 24)
        self.engs = [self.pe, self.act, self.dve, self.pool, self.sp]
        self.dram_ts = []

    def new_sem(self, name):
        self.nsem += 1
        return self.es.enter_context(self.nc.semaphore(name))

    def dram(self, name, shape, dtype, kind, **kw):
        t = T(self.nc.dram_tensor(name, list(shape), dtype, kind=kind, **kw).ap(), name)
        self.dram_ts.append(t)
        return t

    def sb(self, stack, name, shape, dtype):
        self.uid = getattr(self, "uid", 0) + 1
        name = "%s_u%d" % (name, self.uid)
        return T(stack.enter_context(self.nc.sbuf_tensor(name, list(shape), dtype)), name)

    def ps(self, stack, name, shape, dtype):
        self.uid = getattr(self, "uid", 0) + 1
        name = "%s_u%d" % (name, self.uid)
        t = T(stack.enter_context(self.nc.psum_tensor(name, list(shape), dtype)), name)
        t.excl = True
        return t

    def dram_internal(self, name, shape, dtype):
        t = T(self.nc.dram_tensor(name, list(shape), dtype).ap(), name)
        self.dram_ts.append(t)
        return t

    def allgather(self, src, dst):
        e = self.pool
        e._deps([src], [dst], False)
        sem = self.new_sem("cc%d" % self.nsem)
        inst = self.nc.gpsimd.collective_compute("AllGather", ALU.bypass, replica_groups=[list(range(8))],
                                                 ins=[src.t], outs=[dst.t])
        inst.then_inc(sem, 1)
        e._mark([src], [dst], (sem, 1))
        self.extra_marks = getattr(self, "extra_marks", []) + [(sem, 1)]

    def barrier(self):
        marks = list(getattr(self, "extra_marks", []))
        for e in self.engs:
            if e.count > 0:
                marks.append((e.sem, e.count))
            for j, s in enumerate(e.ring):
                if e.ring_cnt[j] > 0:
                    marks.append((s, e.ring_cnt[j]))
        for e in self.engs:
            e.wait_all(marks)

    def finish(self):
        self.barrier()
        self.es.close()
        return self.nc


D = 1024
DFF = 3584
EPS = 1e-6


def rms_rstd(p, ss, rstd, n, n_feat):
    p.act.op(lambda e: e.activation(out=rstd[:, 0:n], in_=ss[:, 0:n], func=AF.Ln,
                                    scale=1.0 / n_feat, bias=p.eps_t[:, 0:1]),
             reads=[ss, p.eps_t], writes=[rstd])
    p.act.op(lambda e: e.activation(out=rstd[:, 0:n], in_=rstd[:, 0:n], func=AF.Exp, scale=-0.5),
             reads=[rstd], writes=[rstd])


def setup_consts(p, stack):
    nc = p.nc
    p.eps_t = p.sb(stack, "eps_t", [128, 1], F32)
    p.pool.op(lambda e: e.memset(p.eps_t[:, :], EPS), writes=[p.eps_t])
    p.ident_f = p.sb(stack, "ident_f", [128, 128], F32)
    p.pool.op(lambda e: e.memset(p.ident_f[:, :], 1.0), writes=[p.ident_f])
    p.pool.op(lambda e: e.affine_select(out=p.ident_f[:, :], in_=p.ident_f[:, :], pattern=[[-1, 128]],
                                        compare_op=ALU.is_equal, fill=0.0, base=0, channel_multiplier=1),
              reads=[p.ident_f], writes=[p.ident_f])
    p.ident_b = p.sb(stack, "ident_b", [128, 128], BF16)
    p.pool.op(lambda e: e.tensor_copy(out=p.ident_b[:, :], in_=p.ident_f[:, :]), reads=[p.ident_f], writes=[p.ident_b])


def phase_ffn(p, xin, xout, g_norm, wg, wu, wd, E, routerT=None, final_g=None, NT=16, FCP=2, ymT=None, w_o=None, ymT_dyn=False):
    nc = p.nc
    NTOK = NT * 128
    NTG = NTOK // 512
    FS = FCP * 128
    NFS = DFF // FS
    with ExitStack() as st:
        xres = [p.sb(st, "xres%d" % t, [128, D], F32) for t in range(NT)]
        xnT = p.sb(st, "xnT", [128, 8, NTOK], BF16)
        gbc = p.sb(st, "gbc", [128, D], F32)
        ss = p.sb(st, "ss", [128, NT], F32)
        rstd = p.sb(st, "rstd", [128, NT], F32)
        comb = p.sb(st, "comb", [128, NT, 8], F32)
        psb = [p.ps(st, "psb%d" % i, [128, 512], F32) for i in range(8)]

        p.sp.dma(gbc[:, :], g_norm.t.partition_broadcast(128), reads=[g_norm], writes=[gbc])
        for t in range(NT):
            p.sp.dma(xres[t][:, :], xin[t * 128:(t + 1) * 128, :], reads=[xin], writes=[xres[t]])

        if ymT is not None:
            with ExitStack() as s0:
                ymT_b = p.sb(s0, "ymT_b", [128, 8, NTOK], BF16)
                wo_b = p.sb(s0, "wo_b", [128, 8, D], BF16)
                if ymT_dyn:
                    pid = p.nc.gpsimd.partition_id()
                    for hc in range(8):
                        p.pool.dma(ymT_b[:, hc, :], ymT.t[hc * 128:(hc + 1) * 128, bass.ds(pid * NTOK, NTOK)],
                                   reads=[ymT], writes=[ymT_b])
                else:
                    p.pool.dma(ymT_b[:, :, :], ymT.t.rearrange("(hc p) t -> p hc t", p=128), reads=[ymT], writes=[ymT_b])
                p.pool.dma(wo_b[:, :, :], w_o.t.rearrange("(hc p) d -> p hc d", p=128), reads=[w_o], writes=[wo_b])
                for t in range(NT):
                    for half in range(2):
                        pd = psb[4 + (2 * t + half) % 4]
                        for hc in range(8):
                            p.pe.op(lambda e, t=t, half=half, hc=hc, pd=pd: e.matmul(
                                out=pd[:, :], lhsT=ymT_b[:, hc, t * 128:(t + 1) * 128], rhs=wo_b[:, hc, half * 512:(half + 1) * 512],
                                start=(hc == 0), stop=(hc == 7)), reads=[ymT_b, wo_b], writes=[pd])
                        p.dve.op(lambda e, t=t, half=half, pd=pd: e.tensor_tensor(
                            out=xres[t][:, half * 512:(half + 1) * 512], in0=pd[:, :], in1=xres[t][:, half * 512:(half + 1) * 512],
                            op=ALU.add), reads=[pd, xres[t]], writes=[xres[t]])
                p.barrier()
        with ExitStack() as st2:
            junk = [p.sb(st2, "junk%d" % i, [128, D], BF16) for i in range(2)]
            xn = [p.sb(st2, "xn%d" % i, [128, D], BF16) for i in range(2)]
            for t in range(NT):
                j = junk[t % 2]
                p.act.op(lambda e, t=t, j=j: e.activation(out=j[:, :], in_=xres[t][:, :], func=AF.Square,
                                                         accum_out=ss[:, t:t + 1]),
                         reads=[xres[t]], writes=[j, ss])
            rms_rstd(p, ss, rstd, NT, D)
            if E > 1:
                xn32 = [p.sb(st2, "xn32_%d" % i, [128, D], F32) for i in range(2)]
                rT = p.sb(st2, "rT", [128, 8, D], F32)
                logit = p.sb(st2, "logit", [128, NT, 8], F32)
                for e_ in range(8):
                    p.sp.dma(rT[:, e_, :], routerT.t[e_].partition_broadcast(128), reads=[routerT], writes=[rT])
            for t in range(NT):
                x_n = xn[t % 2]
                p.dve.op(lambda e, t=t, x_n=x_n: e.scalar_tensor_tensor(
                    out=x_n[:, :], in0=xres[t][:, :], scalar=rstd[:, t:t + 1], in1=gbc[:, :],
                    op0=ALU.mult, op1=ALU.mult), reads=[xres[t], rstd, gbc], writes=[x_n])
                pst = psb[t % 2]
                pv = pst.t[:, :].bitcast(BF16)
                for dc in range(8):
                    p.pe.op(lambda e, dc=dc, x_n=x_n, pv=pv: e.transpose(
                        out=pv[:, dc * 128:(dc + 1) * 128], in_=x_n[:, dc * 128:(dc + 1) * 128], identity=p.ident_b[:, :]),
                        reads=[x_n, p.ident_b], writes=[pst])
                ev = p.act if t % 2 == 0 else p.dve
                if ev is p.act:
                    ev.op(lambda e, t=t, pv=pv: e.copy(out=xnT[:, :, t * 128:(t + 1) * 128],
                                                      in_=pv.rearrange("p (c n) -> p c n", c=8)),
                          reads=[pst], writes=[xnT])
                else:
                    ev.op(lambda e, t=t, pv=pv: e.tensor_copy(out=xnT[:, :, t * 128:(t + 1) * 128],
                                                             in_=pv.rearrange("p (c n) -> p c n", c=8)),
                          reads=[pst], writes=[xnT])
                if E > 1:
                    x32 = xn32[t % 2]
                    p.pool.op(lambda e, t=t, x32=x32: e.tensor_scalar(
                        out=x32[:, :], in0=xres[t][:, :], scalar1=rstd[:, t:t + 1], scalar2=None, op0=ALU.mult),
                        reads=[xres[t], rstd], writes=[x32])
                    p.pool.op(lambda e, x32=x32: e.tensor_tensor(out=x32[:, :], in0=x32[:, :], in1=gbc[:, :], op=ALU.mult),
                              reads=[x32, gbc], writes=[x32])
                    for e_ in range(8):
                        jj = junk[e_ % 2]
                        p.dve.op(lambda e, t=t, e_=e_, x32=x32, jj=jj: e.scalar_tensor_tensor(
                            out=jj[:, :], in0=x32[:, :], scalar=1.0, in1=rT[:, e_, :], op0=ALU.mult, op1=ALU.mult,
                            accum_out=logit[:, t, e_:e_ + 1]), reads=[x32, rT], writes=[jj, logit])
            if E > 1:
                m1 = p.sb(st2, "m1", [128, NT], F32)
                m2 = p.sb(st2, "m2", [128, NT], F32)
                tmp = p.sb(st2, "tmp", [128, NT, 8], F32)
                tmp2 = p.sb(st2, "tmp2", [128, NT, 8], F32)
                sh = [128, NT, 8]
                p.dve.op(lambda e: e.tensor_reduce(out=m1[:, :], in_=logit[:, :, :], axis=AX.X, op=ALU.max),
                         reads=[logit], writes=[m1])
                p.dve.op(lambda e: e.tensor_tensor(out=tmp[:, :, :], in0=logit[:, :, :],
                                                   in1=m1[:, :].unsqueeze(2).to_broadcast(sh), op=ALU.is_equal),
                         reads=[logit, m1], writes=[tmp])
                p.dve.op(lambda e: e.scalar_tensor_tensor(out=tmp[:, :, :], in0=tmp[:, :, :], scalar=-1e30,
                                                          in1=logit[:, :, :], op0=ALU.mult, op1=ALU.add),
                         reads=[tmp, logit], writes=[tmp])
                p.dve.op(lambda e: e.tensor_reduce(out=m2[:, :], in_=tmp[:, :, :], axis=AX.X, op=ALU.max),
                         reads=[tmp], writes=[m2])
                p.dve.op(lambda e: e.tensor_tensor(out=tmp[:, :, :], in0=logit[:, :, :],
                                                   in1=m2[:, :].unsqueeze(2).to_broadcast(sh), op=ALU.is_ge),
                         reads=[logit, m2], writes=[tmp])
                p.dve.op(lambda e: e.tensor_tensor(out=tmp2[:, :, :], in0=logit[:, :, :],
                                                   in1=m1[:, :].unsqueeze(2).to_broadcast(sh), op=ALU.subtract),
                         reads=[logit, m1], writes=[tmp2])
                p.act.op(lambda e: e.activation(out=tmp2[:, :, :], in_=tmp2[:, :, :], func=AF.Exp),
                         reads=[tmp2], writes=[tmp2])
                p.dve.op(lambda e: e.tensor_tensor(out=tmp2[:, :, :], in0=tmp2[:, :, :], in1=tmp[:, :, :], op=ALU.mult),
                         reads=[tmp2, tmp], writes=[tmp2])
                p.dve.op(lambda e: e.tensor_reduce(out=m1[:, :], in_=tmp2[:, :, :], axis=AX.X, op=ALU.add),
                         reads=[tmp2], writes=[m1])
                p.dve.op(lambda e: e.reciprocal(out=m1[:, :], in_=m1[:, :]), reads=[m1], writes=[m1])
                p.dve.op(lambda e: e.tensor_tensor(out=comb[:, :, :], in0=tmp2[:, :, :],
                                                   in1=m1[:, :].unsqueeze(2).to_broadcast(sh), op=ALU.mult),
                         reads=[tmp2, m1], writes=[comb])
            p.barrier()

        with ExitStack() as st3:
            NB = 3
            wgb = [p.sb(st3, "wgb%d" % i, [128, 8, FS], BF16) for i in range(NB)]
            wub = [p.sb(st3, "wub%d" % i, [128, 8, FS], BF16) for i in range(NB)]
            wdb = [p.sb(st3, "wdb%d" % i, [128, FCP, D], BF16) for i in range(NB)]
            sg = [p.sb(st3, "sg%d" % i, [128, 512], BF16) for i in range(3)]
            hT = [p.sb(st3, "hT%d" % i, [128, FCP, 512], BF16) for i in range(3)]

            units = [(e_, fs) for e_ in range(E) for fs in range(NFS)]

            def load(ui):
                e_, fs = units[ui]
                b = ui % NB
                p.pool.dma(wgb[b][:, :, :], wg.t[e_].rearrange("(dc p) f -> p dc f", p=128)[:, :, fs * FS:(fs + 1) * FS],
                           reads=[wg], writes=[wgb[b]])
                p.pool.dma(wub[b][:, :, :], wu.t[e_].rearrange("(dc p) f -> p dc f", p=128)[:, :, fs * FS:(fs + 1) * FS],
                           reads=[wu], writes=[wub[b]])
                p.pool.dma(wdb[b][:, :, :], wd.t[e_].rearrange("(fc p) d -> p fc d", p=128)[:, fs * FCP:(fs + 1) * FCP, :],
                           reads=[wd], writes=[wdb[b]])

            work = [(ui, tg) for ui in range(len(units)) for tg in range(NTG)]
            gu_i = [0]

            def GU(wi):
                ui, tg = work[wi]
                b = ui % NB
                hb = hT[wi % 3]
                for fc in range(FCP):
                    k = gu_i[0]
                    gu_i[0] += 1
                    pg = psb[(k % 2) * 2]
                    pu = psb[(k % 2) * 2 + 1]
                    for dc in range(8):
                        p.pe.op(lambda e, dc=dc, fc=fc, pg=pg: e.matmul(
                            out=pg[:, :], lhsT=wgb[b][:, dc, fc * 128:(fc + 1) * 128],
                            rhs=xnT[:, dc, tg * 512:(tg + 1) * 512], start=(dc == 0), stop=(dc == 7)),
                            reads=[wgb[b], xnT], writes=[pg])
                    for dc in range(8):
                        p.pe.op(lambda e, dc=dc, fc=fc, pu=pu: e.matmul(
                            out=pu[:, :], lhsT=wub[b][:, dc, fc * 128:(fc + 1) * 128],
                            rhs=xnT[:, dc, tg * 512:(tg + 1) * 512], start=(dc == 0), stop=(dc == 7)),
                            reads=[wub[b], xnT], writes=[pu])
                    s = sg[k % 3]
                    p.act.op(lambda e, s=s, pg=pg: e.activation(out=s[:, :], in_=pg[:, :], func=AF.Silu),
                             reads=[pg], writes=[s])
                    p.dve.op(lambda e, s=s, pu=pu, fc=fc: e.tensor_tensor(out=hb[:, fc, :], in0=pu[:, :], in1=s[:, :], op=ALU.mult),
                             reads=[pu, s], writes=[hb])

            dn_i = [0]

            def DN(wi):
                ui, tg = work[wi]
                e_, fs = units[ui]
                b = ui % NB
                hb = hT[wi % 3]
                for tl in range(4):
                    t = tg * 4 + tl
                    for half in range(2):
                        k = dn_i[0]
                        dn_i[0] += 1
                        pd = psb[4 + k % 4]
                        for fc in range(FCP):
                            p.pe.op(lambda e, fc=fc, pd=pd, tl=tl, half=half: e.matmul(
                                out=pd[:, :], lhsT=hb[:, fc, tl * 128:(tl + 1) * 128],
                                rhs=wdb[b][:, fc, half * 512:(half + 1) * 512], start=(fc == 0), stop=(fc == FCP - 1)),
                                reads=[hb, wdb[b]], writes=[pd])
                        sc = comb[:, t, e_:e_ + 1] if E > 1 else 1.0
                        rd = [pd, xres[t]] + ([comb] if E > 1 else [])
                        p.dve.op(lambda e, pd=pd, t=t, half=half, sc=sc: e.scalar_tensor_tensor(
                            out=xres[t][:, half * 512:(half + 1) * 512], in0=pd[:, :], scalar=sc,
                            in1=xres[t][:, half * 512:(half + 1) * 512], op0=ALU.mult, op1=ALU.add),
                            reads=rd, writes=[xres[t]])

            nload = 0
            for _ in range(min(NB - 1, len(units))):
                load(nload)
                nload += 1
            GU(0)
            for wi in range(len(work)):
                ui, tg = work[wi]
                if tg == 0 and nload < len(units):
                    load(nload)
                    nload += 1
                if wi + 1 < len(work):
                    GU(wi + 1)
                DN(wi)

            if final_g is not None:
                p.sp.dma(gbc[:, :], final_g.t.partition_broadcast(128), reads=[final_g], writes=[gbc])
                fj = [p.sb(st3, "fj%d" % i, [128, D], BF16) for i in range(2)]
                for t in range(NT):
                    j = fj[t % 2]
                    p.act.op(lambda e, t=t, j=j: e.activation(out=j[:, :], in_=xres[t][:, :], func=AF.Square,
                                                             accum_out=ss[:, t:t + 1]),
                             reads=[xres[t]], writes=[j, ss])
                rms_rstd(p, ss, rstd, NT, D)
                for t in range(NT):
                    p.dve.op(lambda e, t=t: e.scalar_tensor_tensor(
                        out=xres[t][:, :], in0=xres[t][:, :], scalar=rstd[:, t:t + 1], in1=gbc[:, :],
                        op0=ALU.mult, op1=ALU.mult), reads=[xres[t], rstd, gbc], writes=[xres[t]])
            for t in range(NT):
                p.sp.dma(xout[t * 128:(t + 1) * 128, :], xres[t][:, :], reads=[xres[t]], writes=[xout])
            p.barrier()


def phase_norm_send(p, xin, g_norm, xnT_send, NT=16):
    with ExitStack() as st:
        gbc = p.sb(st, "ns_gbc", [128, D], F32)
        xs = [p.sb(st, "ns_x%d" % i, [128, D], F32) for i in range(3)]
        junk = [p.sb(st, "ns_j%d" % i, [128, D], BF16) for i in range(2)]
        xn = [p.sb(st, "ns_xn%d" % i, [128, D], BF16) for i in range(2)]
        xT = [p.sb(st, "ns_xT%d" % i, [128, D], BF16) for i in range(2)]
        ss = [p.sb(st, "ns_ss%d" % i, [128, 2], F32) for i in range(2)]
        psb = [p.ps(st, "ns_ps%d" % i, [128, 512], F32) for i in range(2)]
        p.sp.dma(gbc[:, :], g_norm.t.partition_broadcast(128), reads=[g_norm], writes=[gbc])
        for t in range(NT):
            x = xs[t % 3]
            s_ = ss[t % 2]
            j = junk[t % 2]
            x_n = xn[t % 2]
            xt = xT[t % 2]
            p.sp.dma(x[:, :], xin[t * 128:(t + 1) * 128, :], reads=[xin], writes=[x])
            p.act.op(lambda e, x=x, j=j, s_=s_: e.activation(out=j[:, :], in_=x[:, :], func=AF.Square, accum_out=s_[:, 0:1]),
                     reads=[x], writes=[j, s_])
            p.act.op(lambda e, s_=s_: e.activation(out=s_[:, 1:2], in_=s_[:, 0:1], func=AF.Ln, scale=1.0 / D, bias=p.eps_t[:, 0:1]),
                     reads=[s_, p.eps_t], writes=[s_])
            p.act.op(lambda e, s_=s_: e.activation(out=s_[:, 1:2], in_=s_[:, 1:2], func=AF.Exp, scale=-0.5), reads=[s_], writes=[s_])
            p.dve.op(lambda e, x=x, x_n=x_n, s_=s_: e.scalar_tensor_tensor(
                out=x_n[:, :], in0=x[:, :], scalar=s_[:, 1:2], in1=gbc[:, :], op0=ALU.mult, op1=ALU.mult),
                reads=[x, s_, gbc], writes=[x_n])
            pb = psb[t % 2]
            pv = pb.t[:, :].bitcast(BF16)
            for dc in range(8):
                p.pe.op(lambda e, dc=dc, x_n=x_n, pv=pv: e.transpose(out=pv[:, dc * 128:(dc + 1) * 128], in_=x_n[:, dc * 128:(dc + 1) * 128],
                                                                    identity=p.ident_b[:, :]), reads=[x_n, p.ident_b], writes=[pb])
            p.act.op(lambda e, xt=xt, pv=pv: e.copy(out=xt[:, :], in_=pv), reads=[pb], writes=[xt])
            p.sp.dma(xnT_send.t[t * 128:(t + 1) * 128, :], xt[:, :], reads=[xt], writes=[xnT_send])
        p.barrier()

import math
ASTEP = 99

S_ALL = 16384
NTILE = 128
NSLOT = 16
MAGIC = 12582912.0
TWO_PI_HI = 6.28125
TWO_PI_LO = 2.0 * math.pi - 6.28125
SCALE = 192.0 ** -0.5


def trig_tables(p, st, posf_ap, n, inv_bc, cos_t, sin_t, reads):
    sh = [128, n, 32]
    ang = p.sb(st, "tg_ang", sh, F32)
    a2 = p.sb(st, "tg_a2", sh, F32)
    nn = p.sb(st, "tg_n", sh, F32)
    p.dve.op(lambda e: e.tensor_tensor(out=ang[:, :, :], in0=posf_ap.unsqueeze(2).to_broadcast(sh),
                                       in1=inv_bc[:, :].unsqueeze(1).to_broadcast(sh), op=ALU.mult),
             reads=reads + [inv_bc], writes=[ang])
    for which, dst in ((0, sin_t), (1, cos_t)):
        if which == 1:
            p.dve.op(lambda e: e.tensor_scalar(out=ang[:, :, :], in0=ang[:, :, :], scalar1=math.pi / 2, scalar2=None,
                                               op0=ALU.add), reads=[ang], writes=[ang])
        p.dve.op(lambda e: e.tensor_scalar(out=nn[:, :, :], in0=ang[:, :, :], scalar1=1.0 / (2 * math.pi), scalar2=None,
                                           op0=ALU.mult), reads=[ang], writes=[nn])
        p.dve.op(lambda e: e.tensor_scalar(out=nn[:, :, :], in0=nn[:, :, :], scalar1=MAGIC, scalar2=None, op0=ALU.add),
                 reads=[nn], writes=[nn])
        p.dve.op(lambda e: e.tensor_scalar(out=nn[:, :, :], in0=nn[:, :, :], scalar1=MAGIC, scalar2=None, op0=ALU.subtract),
                 reads=[nn], writes=[nn])
        p.dve.op(lambda e: e.scalar_tensor_tensor(out=a2[:, :, :], in0=nn[:, :, :], scalar=-TWO_PI_HI, in1=ang[:, :, :],
                                                  op0=ALU.mult, op1=ALU.add), reads=[nn, ang], writes=[a2])
        p.dve.op(lambda e: e.scalar_tensor_tensor(out=a2[:, :, :], in0=nn[:, :, :], scalar=-TWO_PI_LO, in1=a2[:, :, :],
                                                  op0=ALU.mult, op1=ALU.add), reads=[nn, a2], writes=[a2])
        p.dve.op(lambda e: e.tensor_scalar(out=a2[:, :, :], in0=a2[:, :, :], scalar1=-3.14159, scalar2=3.14159,
                                           op0=ALU.max, op1=ALU.min), reads=[a2], writes=[a2])
        p.act.op(lambda e, dst=dst: e.activation(out=dst[:, :, :], in_=a2[:, :, :], func=AF.Sin),
                 reads=[a2], writes=[dst])


def phase_attn(p, x_full, x_own, posT, pos_own, inv_freq, masks, g_norm, w_in, q_norm, w_qb, kv_norm, w_kvb, w_out,
               xa_out, NTILE=NTILE, NSLOT=NSLOT, STOP=99):
    nc = p.nc
    NKEY = NTILE * 128
    with ExitStack() as st:
        KlatT = p.sb(st, "KlatT", [128, NKEY], BF16)
        KrotT = p.sb(st, "KrotT", [128, NKEY], BF16)
        Vlat = p.sb(st, "Vlat", [128, NTILE, 128], BF16)
        w_in_b = p.sb(st, "w_in_b", [128, 8, 448], BF16)
        w_qb_b = p.sb(st, "w_qb_b", [128, 2, 1536], BF16)
        w_kvb_b = p.sb(st, "w_kvb_b", [128, 2048], BF16)
        WkbT = p.sb(st, "WkbT", [128, 8, 128], BF16)
        w_out_b = p.sb(st, "w_out_b", [128, 8, 1024], BF16)
        gbc = p.sb(st, "a_gbc", [128, D], F32)
        qn_bc = p.sb(st, "qn_bc", [128, 256], F32)
        kvn_bc = p.sb(st, "kvn_bc", [128, 128], F32)
        inv_bc = p.sb(st, "inv_bc", [128, 32], F32)
        mask_b = p.sb(st, "mask_b", [128, 8, 512], BF16)
        ones_b = p.sb(st, "ones_b", [128, 128], BF16)
        cos_o = p.sb(st, "cos_o", [128, NSLOT, 32], BF16)
        sin_o = p.sb(st, "sin_o", [128, NSLOT, 32], BF16)
        ps = [p.ps(st, "aps%d" % i, [128, 512], F32) for i in range(8)]

        def psbf(i):
            return ps[i].t[:, :].bitcast(BF16)

        p.pool.dma(w_in_b[:, :, :], w_in.t.rearrange("(dc p) f -> p dc f", p=128), reads=[w_in], writes=[w_in_b])
        p.pool.dma(w_qb_b[:, :, :], w_qb.t.rearrange("(dc p) f -> p dc f", p=128), reads=[w_qb], writes=[w_qb_b])
        p.pool.dma(w_kvb_b[:, :], w_kvb.t, reads=[w_kvb], writes=[w_kvb_b])
        p.pool.dma(w_out_b[:, :, :], w_out.t.rearrange("(h p) d -> p h d", p=128), reads=[w_out], writes=[w_out_b])
        p.pool.dma(mask_b[:, :, :], masks.t.rearrange("j p n -> p j n"), reads=[masks], writes=[mask_b])
        p.sp.dma(gbc[:, :], g_norm.t.partition_broadcast(128), reads=[g_norm], writes=[gbc])
        p.sp.dma(qn_bc[:, :], q_norm.t.partition_broadcast(128), reads=[q_norm], writes=[qn_bc])
        p.sp.dma(kvn_bc[:, :], kv_norm.t.partition_broadcast(128), reads=[kv_norm], writes=[kvn_bc])
        p.sp.dma(inv_bc[:, :], inv_freq.t.partition_broadcast(128), reads=[inv_freq], writes=[inv_bc])
        p.pool.op(lambda e: e.memset(ones_b[:, :], 1.0), writes=[ones_b])
        pv = psbf(0)
        for h in range(8):
            p.pe.op(lambda e, h=h: e.transpose(out=pv[:, h * 128:(h + 1) * 128], in_=w_kvb_b[:, h * 256:h * 256 + 128],
                                               identity=p.ident_b[:, :]), reads=[w_kvb_b, p.ident_b], writes=[ps[0]])
        p.dve.op(lambda e: e.tensor_copy(out=WkbT[:, :, :], in_=pv.rearrange("p (h n) -> p h n", h=8)),
                 reads=[ps[0]], writes=[WkbT])

        if STOP <= 1:
            p.barrier()
            return
        with ExitStack() as sa:
            cos_a = p.sb(sa, "cos_a", [128, NTILE, 32], BF16)
            sin_a = p.sb(sa, "sin_a", [128, NTILE, 32], BF16)
            with ExitStack() as stt_:
                posi = p.sb(stt_, "posi", [128, NTILE], I32)
                posf = p.sb(stt_, "posf", [128, NTILE], F32)
                posoi = p.sb(stt_, "posoi", [128, NSLOT], I32)
                posof = p.sb(stt_, "posof", [128, NSLOT], F32)
                p.sp.dma(posi[:, :], posT.t, reads=[posT], writes=[posi])
                p.sp.dma(posoi[:, :], pos_own.t, reads=[pos_own], writes=[posoi])
                p.dve.op(lambda e: e.tensor_copy(out=posf[:, :], in_=posi[:, :]), reads=[posi], writes=[posf])
                p.dve.op(lambda e: e.tensor_copy(out=posof[:, :], in_=posoi[:, :]), reads=[posoi], writes=[posof])
                CH = min(32, NTILE)
                for c0 in range(0, NTILE, CH):
                    with ExitStack() as s3:
                        ct = p.sb(s3, "ct", [128, CH, 32], BF16)
                        stt = p.sb(s3, "stt", [128, CH, 32], BF16)
                        trig_tables(p, s3, posf[:, c0:c0 + CH], CH, inv_bc, ct, stt, [posf])
                        p.pool.op(lambda e, c0=c0: e.tensor_copy(out=cos_a[:, c0:c0 + CH, :], in_=ct[:, :, :]),
                                  reads=[ct], writes=[cos_a])
                        p.pool.op(lambda e, c0=c0: e.tensor_copy(out=sin_a[:, c0:c0 + CH, :], in_=stt[:, :, :]),
                                  reads=[stt], writes=[sin_a])
                        p.barrier()
                with ExitStack() as s3:
                    trig_tables(p, s3, posof[:, :], NSLOT, inv_bc, cos_o, sin_o, [posof])
                    p.barrier()

            if STOP <= 2:
                p.barrier()
                return
            xs = [p.sb(sa, "xs%d" % i, [128, D], F32) for i in range(3)]
            junk = [p.sb(sa, "ajunk%d" % i, [128, D], BF16) for i in range(2)]
            xn = [p.sb(sa, "axn%d" % i, [128, D], BF16) for i in range(2)]
            xnT = [p.sb(sa, "axnT%d" % i, [128, 8, 128], BF16) for i in range(2)]
            ssA = [p.sb(sa, "ssA%d" % i, [128, 4], F32) for i in range(3)]
            ckn = [p.sb(sa, "ckn%d" % i, [128, 128], BF16) for i in range(2)]
            krA = [p.sb(sa, "krA%d" % i, [128, 2, 32], F32) for i in range(2)]
            krB = [p.sb(sa, "krB%d" % i, [128, 2, 32], F32) for i in range(2)]
            krot = [p.sb(sa, "krot%d" % i, [128, 128], BF16) for i in range(2)]
            for kk in krot:
                p.pool.op(lambda e, kk=kk: e.memset(kk[:, :], 0.0), writes=[kk])

            def load_x(T):
                p.sp.dma(xs[T % 3][:, :], x_full[T * 128:(T + 1) * 128, :], reads=[x_full], writes=[xs[T % 3]])

            junk2 = [p.sb(sa, "ajunk2_%d" % i, [128, 128], BF16) for i in range(2)]
            load_x(0)
            load_x(1)

            def A1(T):
                if T + 2 < NTILE:
                    load_x(T + 2)
                x = xs[T % 3]
                s_ = ssA[T % 3]
                j = junk[T % 2]
                x_n = xn[T % 2]
                xT = xnT[T % 2]
                p.act.op(lambda e, x=x, j=j, s_=s_: e.activation(out=j[:, :], in_=x[:, :], func=AF.Square, accum_out=s_[:, 0:1]),
                         reads=[x], writes=[j, s_])
                p.act.op(lambda e, s_=s_: e.activation(out=s_[:, 1:2], in_=s_[:, 0:1], func=AF.Ln, scale=1.0 / D, bias=p.eps_t[:, 0:1]),
                         reads=[s_, p.eps_t], writes=[s_])
                p.act.op(lambda e, s_=s_: e.activation(out=s_[:, 1:2], in_=s_[:, 1:2], func=AF.Exp, scale=-0.5),
                         reads=[s_], writes=[s_])
                p.dve.op(lambda e, x=x, x_n=x_n, s_=s_: e.scalar_tensor_tensor(
                    out=x_n[:, :], in0=x[:, :], scalar=s_[:, 1:2], in1=gbc[:, :], op0=ALU.mult, op1=ALU.mult),
                    reads=[x, s_, gbc], writes=[x_n])
                pb = ps[T % 2]
                pvv = psbf(T % 2)
                for dc in range(8):
                    p.pe.op(lambda e, dc=dc, x_n=x_n, pvv=pvv: e.transpose(
                        out=pvv[:, dc * 128:(dc + 1) * 128], in_=x_n[:, dc * 128:(dc + 1) * 128], identity=p.ident_b[:, :]),
                        reads=[x_n, p.ident_b], writes=[pb])
                p.act.op(lambda e, xT=xT, pvv=pvv: e.copy(out=xT[:, :, :], in_=pvv.rearrange("p (c n) -> p c n", c=8)),
                         reads=[pb], writes=[xT])

            def A2(T):
                s_ = ssA[T % 3]
                j = junk2[T % 2]
                xT = xnT[T % 2]
                pk = ps[2 + T % 2]
                for dc in range(8):
                    p.pe.op(lambda e, dc=dc, xT=xT, pk=pk: e.matmul(out=pk[:, 0:192], lhsT=xT[:, dc, :], rhs=w_in_b[:, dc, 256:448],
                                                                   start=(dc == 0), stop=(dc == 7)),
                            reads=[xT, w_in_b], writes=[pk])
                p.act.op(lambda e, pk=pk, j=j, s_=s_: e.activation(out=j[:, 0:128], in_=pk[:, 0:128], func=AF.Square,
                                                                  accum_out=s_[:, 2:3]), reads=[pk], writes=[j, s_])
                p.act.op(lambda e, s_=s_: e.activation(out=s_[:, 3:4], in_=s_[:, 2:3], func=AF.Ln, scale=1.0 / 128, bias=p.eps_t[:, 0:1]),
                         reads=[s_, p.eps_t], writes=[s_])
                p.act.op(lambda e, s_=s_: e.activation(out=s_[:, 3:4], in_=s_[:, 3:4], func=AF.Exp, scale=-0.5),
                         reads=[s_], writes=[s_])
                ck = ckn[T % 2]
                p.dve.op(lambda e, pk=pk, ck=ck, s_=s_: e.scalar_tensor_tensor(
                    out=ck[:, :], in0=pk[:, 0:128], scalar=s_[:, 3:4], in1=kvn_bc[:, :], op0=ALU.mult, op1=ALU.mult),
                    reads=[pk, s_, kvn_bc], writes=[ck])
                p.pool.op(lambda e, T=T, ck=ck: e.tensor_copy(out=Vlat[:, T, :], in_=ck[:, :]), reads=[ck], writes=[Vlat])
                A = krA[T % 2]
                B = krB[T % 2]
                kr = krot[T % 2]
                kv3 = pk[:, 128:192].rearrange("p (a f) -> p a f", a=2)
                sh = [128, 2, 32]
                p.dve.op(lambda e, A=A, kv3=kv3, T=T: e.tensor_tensor(
                    out=A[:, :, :], in0=kv3, in1=cos_a[:, T, :].unsqueeze(1).to_broadcast(sh), op=ALU.mult),
                    reads=[pk, cos_a], writes=[A])
                p.dve.op(lambda e, B=B, kv3=kv3, T=T: e.tensor_tensor(
                    out=B[:, :, :], in0=kv3, in1=sin_a[:, T, :].unsqueeze(1).to_broadcast(sh), op=ALU.mult),
                    reads=[pk, sin_a], writes=[B])
                p.pool.op(lambda e, A=A, B=B, kr=kr: e.tensor_tensor(out=kr[:, 0:32], in0=A[:, 0, :], in1=B[:, 1, :], op=ALU.subtract),
                          reads=[A, B], writes=[kr])
                p.pool.op(lambda e, A=A, B=B, kr=kr: e.tensor_tensor(out=kr[:, 32:64], in0=B[:, 0, :], in1=A[:, 1, :], op=ALU.add),
                          reads=[A, B], writes=[kr])

            def A3(T):
                ck = ckn[T % 2]
                kr = krot[T % 2]
                pt_ = ps[4 + T % 2]
                ptv = psbf(4 + T % 2)
                p.pe.op(lambda e, ck=ck, ptv=ptv: e.transpose(out=ptv[:, 0:128], in_=ck[:, :], identity=p.ident_b[:, :]),
                        reads=[ck, p.ident_b], writes=[pt_])
                p.pe.op(lambda e, kr=kr, ptv=ptv: e.transpose(out=ptv[:, 128:256], in_=kr[:, :], identity=p.ident_b[:, :]),
                        reads=[kr, p.ident_b], writes=[pt_])
                p.act.op(lambda e, T=T, ptv=ptv: e.copy(out=KlatT[:, T * 128:(T + 1) * 128], in_=ptv[:, 0:128]),
                         reads=[pt_], writes=[KlatT])
                p.dve.op(lambda e, T=T, ptv=ptv: e.tensor_copy(out=KrotT[:, T * 128:(T + 1) * 128], in_=ptv[:, 128:256]),
                         reads=[pt_], writes=[KrotT])

            for it in range(NTILE + 2):
                if 0 <= it - 2 < NTILE:
                    A3(it - 2)
                if 0 <= it - 1 < NTILE:
                    A2(it - 1)
                if it < NTILE:
                    A1(it)
            p.barrier()

        if STOP <= 3:
            p.barrier()
            return
        with ExitStack() as sb_:
            xo = [p.sb(sb_, "xo%d" % i, [128, D], F32) for i in range(2)]
            junk = p.sb(sb_, "bjunk", [128, D], BF16)
            xn = p.sb(sb_, "bxn", [128, D], BF16)
            xnT = p.sb(sb_, "bxnT", [128, 8, 128], BF16)
            ssB = p.sb(sb_, "ssB", [128, 4], F32)
            cqn = p.sb(sb_, "cqn", [128, 256], BF16)
            cqnT = p.sb(sb_, "cqnT", [128, 2, 128], BF16)
            qnT = p.sb(sb_, "qnT", [128, 8, 128], BF16)
            Q1T = p.sb(sb_, "Q1T", [128, 1024], BF16)
            Q2T = p.sb(sb_, "Q2T", [128, 1024], BF16)
            dacc = p.sb(sb_, "dacc", [128, 512], F32)
            ones_f32 = p.sb(sb_, "ones_f32", [128, 128], F32)
            p.pool.op(lambda e: e.memset(ones_f32[:, :], 1.0), writes=[ones_f32])
            qA = p.sb(sb_, "qA", [128, 8, 2, 32], F32)
            qB = p.sb(sb_, "qB", [128, 8, 2, 32], F32)
            qrot = p.sb(sb_, "qrot", [128, 8, 128], BF16)
            p.pool.op(lambda e: e.memset(qrot[:, :, :], 0.0), writes=[qrot])
            PT = [p.sb(sb_, "PT%d" % i, [128, 512], BF16) for i in range(4)]
            rden = p.sb(sb_, "rden", [128, 512], F32)
            OnT = p.sb(sb_, "OnT", [128, 1024], BF16)
            ohT = p.sb(sb_, "ohT", [128, 8, 128], BF16)
            xout_t = [p.sb(sb_, "xout_t%d" % i, [128, D], F32) for i in range(2)]

            p.sp.dma(xo[0][:, :], x_own[0:128, :], reads=[x_own], writes=[xo[0]])
            pt_i = 0
            for i in range(NSLOT):
                if i + 1 < NSLOT:
                    p.sp.dma(xo[(i + 1) % 2][:, :], x_own[(i + 1) * 128:(i + 2) * 128, :], reads=[x_own], writes=[xo[(i + 1) % 2]])
                x = xo[i % 2]
                p.act.op(lambda e, x=x: e.activation(out=junk[:, :], in_=x[:, :], func=AF.Square, accum_out=ssB[:, 0:1]),
                         reads=[x], writes=[junk, ssB])
                p.act.op(lambda e: e.activation(out=ssB[:, 1:2], in_=ssB[:, 0:1], func=AF.Ln, scale=1.0 / D, bias=p.eps_t[:, 0:1]),
                         reads=[ssB, p.eps_t], writes=[ssB])
                p.act.op(lambda e: e.activation(out=ssB[:, 1:2], in_=ssB[:, 1:2], func=AF.Exp, scale=-0.5), reads=[ssB], writes=[ssB])
                p.dve.op(lambda e, x=x: e.scalar_tensor_tensor(out=xn[:, :], in0=x[:, :], scalar=ssB[:, 1:2], in1=gbc[:, :],
                                                               op0=ALU.mult, op1=ALU.mult), reads=[x, ssB, gbc], writes=[xn])
                pvv = psbf(7)
                for dc in range(8):
                    p.pe.op(lambda e, dc=dc: e.transpose(out=pvv[:, dc * 128:(dc + 1) * 128], in_=xn[:, dc * 128:(dc + 1) * 128],
                                                         identity=p.ident_b[:, :]), reads=[xn, p.ident_b], writes=[ps[7]])
                p.act.op(lambda e: e.copy(out=xnT[:, :, :], in_=pvv.rearrange("p (c n) -> p c n", c=8)), reads=[ps[7]], writes=[xnT])
                for dc in range(8):
                    p.pe.op(lambda e, dc=dc: e.matmul(out=ps[6][:, 0:256], lhsT=xnT[:, dc, :], rhs=w_in_b[:, dc, 0:256],
                                                      start=(dc == 0), stop=(dc == 7)), reads=[xnT, w_in_b], writes=[ps[6]])
                p.act.op(lambda e: e.activation(out=junk[:, 0:256], in_=ps[6][:, 0:256], func=AF.Square, accum_out=ssB[:, 2:3]),
                         reads=[ps[6]], writes=[junk, ssB])
                p.act.op(lambda e: e.activation(out=ssB[:, 3:4], in_=ssB[:, 2:3], func=AF.Ln, scale=1.0 / 256, bias=p.eps_t[:, 0:1]),
                         reads=[ssB, p.eps_t], writes=[ssB])
                p.act.op(lambda e: e.activation(out=ssB[:, 3:4], in_=ssB[:, 3:4], func=AF.Exp, scale=-0.5), reads=[ssB], writes=[ssB])
                p.dve.op(lambda e: e.scalar_tensor_tensor(out=cqn[:, :], in0=ps[6][:, 0:256], scalar=ssB[:, 3:4], in1=qn_bc[:, :],
                                                          op0=ALU.mult, op1=ALU.mult), reads=[ps[6], ssB, qn_bc], writes=[cqn])
                for kc in range(2):
                    p.pe.op(lambda e, kc=kc: e.transpose(out=pvv[:, kc * 128:(kc + 1) * 128], in_=cqn[:, kc * 128:(kc + 1) * 128],
                                                         identity=p.ident_b[:, :]), reads=[cqn, p.ident_b], writes=[ps[7]])
                p.act.op(lambda e: e.copy(out=cqnT[:, :, :], in_=pvv[:, 0:256].rearrange("p (c n) -> p c n", c=2)),
                         reads=[ps[7]], writes=[cqnT])
                for h in range(8):
                    pb = ps[h // 4]
                    for kc in range(2):
                        p.pe.op(lambda e, h=h, kc=kc, pb=pb: e.matmul(
                            out=pb[:, (h % 4) * 128:(h % 4 + 1) * 128], lhsT=w_qb_b[:, kc, h * 192:h * 192 + 128],
                            rhs=cqnT[:, kc, :], start=(kc == 0), stop=(kc == 1)), reads=[w_qb_b, cqnT], writes=[pb])
                p.act.op(lambda e: e.copy(out=qnT[:, 0:4, :], in_=ps[0][:, :].rearrange("p (h n) -> p h n", h=4)),
                         reads=[ps[0]], writes=[qnT])
                p.dve.op(lambda e: e.tensor_copy(out=qnT[:, 4:8, :], in_=ps[1][:, :].rearrange("p (h n) -> p h n", h=4)),
                         reads=[ps[1]], writes=[qnT])
                for h in range(8):
                    pb = ps[2 + h // 4]
                    p.pe.op(lambda e, h=h, pb=pb: e.matmul(out=pb[:, (h % 4) * 128:(h % 4 + 1) * 128], lhsT=WkbT[:, h, :],
                                                           rhs=qnT[:, h, :], start=True, stop=True), reads=[WkbT, qnT], writes=[pb])
                p.act.op(lambda e: e.copy(out=Q1T[:, 0:512], in_=ps[2][:, :]), reads=[ps[2]], writes=[Q1T])
                p.dve.op(lambda e: e.tensor_copy(out=Q1T[:, 512:1024], in_=ps[3][:, :]), reads=[ps[3]], writes=[Q1T])
                wr = w_qb_b[:, :, :].rearrange("p c (h f) -> p c h f", h=8)
                for kc in range(2):
                    p.pe.op(lambda e, kc=kc: e.matmul(out=ps[6][:, :].rearrange("p (h f) -> p h f", h=8), lhsT=cqnT[:, kc, :],
                                                      rhs=wr[:, kc, :, 128:192], start=(kc == 0), stop=(kc == 1)),
                            reads=[cqnT, w_qb_b], writes=[ps[6]])
                q4 = ps[6][:, :].rearrange("p (h a f) -> p h a f", h=8, a=2)
                sh4 = [128, 8, 2, 32]
                cb = cos_o[:, i, :].unsqueeze(1).unsqueeze(1).to_broadcast(sh4)
                sbb = sin_o[:, i, :].unsqueeze(1).unsqueeze(1).to_broadcast(sh4)
                p.dve.op(lambda e, cb=cb: e.tensor_tensor(out=qA[:, :, :, :], in0=q4, in1=cb, op=ALU.mult),
                         reads=[ps[6], cos_o], writes=[qA])
                p.dve.op(lambda e, sbb=sbb: e.tensor_tensor(out=qB[:, :, :, :], in0=q4, in1=sbb, op=ALU.mult),
                         reads=[ps[6], sin_o], writes=[qB])
                p.pool.op(lambda e: e.tensor_tensor(out=qrot[:, :, 0:32], in0=qA[:, :, 0, :], in1=qB[:, :, 1, :], op=ALU.subtract),
                          reads=[qA, qB], writes=[qrot])
                p.pool.op(lambda e: e.tensor_tensor(out=qrot[:, :, 32:64], in0=qB[:, :, 0, :], in1=qA[:, :, 1, :], op=ALU.add),
                          reads=[qA, qB], writes=[qrot])
                for h in range(8):
                    p.pe.op(lambda e, h=h: e.transpose(out=pvv[:, h * 128:(h + 1) * 128], in_=qrot[:, h, :],
                                                       identity=p.ident_b[:, :]), reads=[qrot, p.ident_b], writes=[ps[7]])
                p.act.op(lambda e: e.copy(out=Q2T[:, :], in_=pvv[:, :]), reads=[ps[7]], writes=[Q2T])

                nkb = 8 * i + 8
                for g in range(2):
                    po = ps[3 + g]
                    pd = ps[5 + g]
                    qs = slice(g * 512, (g + 1) * 512)

                    def S(kb, g=g, qs=qs):
                        pS = ps[kb % 3]
                        masked = kb >= 8 * i
                        p.pe.op(lambda e: e.matmul(out=pS[:, :], lhsT=KlatT[:, kb * 128:(kb + 1) * 128], rhs=Q1T[:, qs],
                                                   start=True, stop=False), reads=[KlatT, Q1T], writes=[pS])
                        p.pe.op(lambda e: e.matmul(out=pS[:, :], lhsT=KrotT[:, kb * 128:(kb + 1) * 128], rhs=Q2T[:, qs],
                                                   start=False, stop=not masked), reads=[KrotT, Q2T], writes=[pS])
                        if masked:
                            p.pe.op(lambda e: e.matmul(out=pS[:, :], lhsT=p.ident_b[:, :], rhs=mask_b[:, kb - 8 * i, :],
                                                       start=False, stop=True), reads=[p.ident_b, mask_b], writes=[pS])

                    S(0)
                    if nkb > 1:
                        S(1)
                    for kb in range(nkb):
                        if kb + 2 < nkb:
                            S(kb + 2)
                        pS = ps[kb % 3]
                        Pt = PT[pt_i % 4]
                        pt_i += 1
                        p.act.op(lambda e, pS=pS, Pt=Pt: e.activation(out=Pt[:, :], in_=pS[:, :], func=AF.Exp, scale=SCALE),
                                 reads=[pS], writes=[Pt])
                        p.pe.op(lambda e, kb=kb, Pt=Pt, po=po: e.matmul(out=po[:, :], lhsT=Vlat[:, kb, :], rhs=Pt[:, :],
                                                                        start=(kb == 0), stop=(kb == nkb - 1)),
                                reads=[Vlat, Pt], writes=[po])
                        if kb == 0:
                            p.dve.op(lambda e, Pt=Pt: e.tensor_copy(out=dacc[:, :], in_=Pt[:, :]), reads=[Pt], writes=[dacc])
                        else:
                            p.dve.op(lambda e, Pt=Pt: e.tensor_tensor(out=dacc[:, :], in0=dacc[:, :], in1=Pt[:, :], op=ALU.add),
                                     reads=[dacc, Pt], writes=[dacc])
                    p.pe.op(lambda e, pd=pd: e.matmul(out=pd[:, :], lhsT=ones_f32[:, :], rhs=dacc[:, :], start=True, stop=True),
                            reads=[ones_f32, dacc], writes=[pd])
                    p.dve.op(lambda e, pd=pd: e.reciprocal(out=rden[:, :], in_=pd[:, :]), reads=[pd], writes=[rden])
                    p.dve.op(lambda e, po=po, qs=qs: e.tensor_tensor(out=OnT[:, qs], in0=po[:, :], in1=rden[:, :], op=ALU.mult),
                             reads=[po, rden], writes=[OnT])
                for h in range(8):
                    pb = ps[h // 4]
                    p.pe.op(lambda e, h=h, pb=pb: e.matmul(out=pb[:, (h % 4) * 128:(h % 4 + 1) * 128],
                                                           lhsT=w_kvb_b[:, h * 256 + 128:(h + 1) * 256],
                                                           rhs=OnT[:, h * 128:(h + 1) * 128], start=True, stop=True),
                            reads=[w_kvb_b, OnT], writes=[pb])
                p.act.op(lambda e: e.copy(out=ohT[:, 0:4, :], in_=ps[0][:, :].rearrange("p (h n) -> p h n", h=4)),
                         reads=[ps[0]], writes=[ohT])
                p.dve.op(lambda e: e.tensor_copy(out=ohT[:, 4:8, :], in_=ps[1][:, :].rearrange("p (h n) -> p h n", h=4)),
                         reads=[ps[1]], writes=[ohT])
                xt = xout_t[i % 2]
                for half in range(2):
                    pb = ps[6 + half]
                    for h in range(8):
                        p.pe.op(lambda e, h=h, half=half, pb=pb: e.matmul(out=pb[:, :], lhsT=ohT[:, h, :],
                                                                          rhs=w_out_b[:, h, half * 512:(half + 1) * 512],
                                                                          start=(h == 0), stop=(h == 7)),
                                reads=[ohT, w_out_b], writes=[pb])
                    p.dve.op(lambda e, half=half, pb=pb, xt=xt, x=x: e.tensor_tensor(
                        out=xt[:, half * 512:(half + 1) * 512], in0=pb[:, :], in1=x[:, half * 512:(half + 1) * 512], op=ALU.add),
                        reads=[pb, x], writes=[xt])
                p.sp.dma(xa_out[i * 128:(i + 1) * 128, :], xt[:, :], reads=[xt], writes=[xa_out])
            p.barrier()


NC_TRI, NC_BLK, NC_NEG, NC_MA2, NC_MB2, NC_M3 = 0, 128, 256, 384, 512, 640
NCONST = 643


def mlstm_consts_np():
    s = np.arange(128)[:, None]
    t = np.arange(128)[None, :]
    same = (s // 64) == (t // 64)
    c = np.zeros((128, NCONST), np.float32)
    c[:, NC_TRI:NC_TRI + 128] = (same & (s <= t))
    c[:, NC_BLK:NC_BLK + 128] = same
    c[:, NC_NEG:NC_NEG + 128] = np.where(same & (s <= t), 0.0, -30000.0)
    c[:, NC_MA2:NC_MA2 + 128] = (s < 64) & (t < 64)
    c[:, NC_MB2:NC_MB2 + 128] = (s >= 64) & (t >= 64)
    c[:, NC_M3 + 0] = 1.0
    c[:, NC_M3 + 1] = (np.arange(128) < 64)
    c[:, NC_M3 + 2] = (np.arange(128) >= 64)
    return c


def phase_mlstm(p, x_full, g_norm, w_h, cw, cb, gate_b, head_norm, consts, y_out, NTILE=128, xnT_all=None, yT_send=None):
    nc = p.nc
    with ExitStack() as st:
        w_b = p.sb(st, "m_w", [128, 8, 514], BF16)
        gbc = p.sb(st, "m_gbc", [128, D], F32)
        cst = p.sb(st, "m_cst", [128, NCONST], F32)
        cw_t = p.sb(st, "m_cw", [128, 8], F32)
        cb_t = p.sb(st, "m_cb", [128, 2], F32)
        gb_bc = p.sb(st, "m_gb", [128, 2], F32)
        hn_bc = p.sb(st, "m_hn", [128, 128], F32)
        ones_f = p.sb(st, "m_ones", [128, 128], F32)
        one_c = p.sb(st, "m_one_c", [128, 1], F32)
        ps = [p.ps(st, "mps%d" % i, [128, 512], F32) for i in range(8)]

        p.pool.dma(w_b[:, :, :], w_h.t.rearrange("(dc p) f -> p dc f", p=128), reads=[w_h], writes=[w_b])
        p.sp.dma(gbc[:, :], g_norm.t.partition_broadcast(128), reads=[g_norm], writes=[gbc])
        p.sp.dma(cst[:, :], consts.t, reads=[consts], writes=[cst])
        p.sp.dma(cw_t[:, :], cw.t, reads=[cw], writes=[cw_t])
        p.sp.dma(cb_t[:, :], cb.t, reads=[cb], writes=[cb_t])
        p.sp.dma(gb_bc[:, :], gate_b.t.partition_broadcast(128), reads=[gate_b], writes=[gb_bc])
        p.sp.dma(hn_bc[:, :], head_norm.t.partition_broadcast(128), reads=[head_norm], writes=[hn_bc])
        p.pool.op(lambda e: e.memset(ones_f[:, :], 1.0), writes=[ones_f])
        p.pool.op(lambda e: e.memset(one_c[:, :], 1.0), writes=[one_c])

        Tri = cst[:, NC_TRI:NC_TRI + 128]
        Blk = cst[:, NC_BLK:NC_BLK + 128]
        Neg = cst[:, NC_NEG:NC_NEG + 128]
        MA2 = cst[:, NC_MA2:NC_MA2 + 128]
        MB2 = cst[:, NC_MB2:NC_MB2 + 128]
        M3 = cst[:, NC_M3:NC_M3 + 3]

        xs = [p.sb(st, "m_xs%d" % i, [128, D], F32) for i in range(3)]
        junk = [p.sb(st, "m_junk%d" % i, [128, D], BF16) for i in range(2)]
        xn = [p.sb(st, "m_xn%d" % i, [128, D], BF16) for i in range(2)]
        xnT = [p.sb(st, "m_xnT%d" % i, [128, 8, 128], BF16) for i in range(2)]
        ss = [p.sb(st, "m_ss%d" % i, [128, 2], F32) for i in range(2)]
        raw = [p.sb(st, "m_raw%d" % i, [128, 2, 131], F32) for i in range(4)]
        acc = p.sb(st, "m_acc", [128, 2, 128], F32)
        sg = p.sb(st, "m_sg", [128, 2, 128], F32)
        qT_l = [p.sb(st, "m_qT%d" % i, [128, 128], F32) for i in range(2)]
        kT_l = [p.sb(st, "m_kT%d" % i, [128, 128], F32) for i in range(2)]
        qbA_l = [p.sb(st, "m_qbA%d" % i, [128, 128], F32) for i in range(2)]
        qbB_l = [p.sb(st, "m_qbB%d" % i, [128, 128], F32) for i in range(2)]
        kwA_l = [p.sb(st, "m_kwA%d" % i, [128, 128], F32) for i in range(2)]
        kwB_l = [p.sb(st, "m_kwB%d" % i, [128, 128], F32) for i in range(2)]
        v1_l = [p.sb(st, "m_v1%d" % i, [128, 129], F32) for i in range(4)]
        so_l = [p.sb(st, "m_so%d" % i, [128, 128], F32) for i in range(5)]
        gt_l = [p.sb(st, "m_gt%d" % i, [128, 2], F32) for i in range(2)]
        g2 = p.sb(st, "m_g2", [128, 2], F32)
        lf3_l = [p.sb(st, "m_lf3%d" % i, [128, 3], F32) for i in range(2)]
        lfc = p.sb(st, "m_lfc", [128, 1], F32)
        g4 = p.sb(st, "m_g4", [128, 4], F32)
        F2_l = [p.sb(st, "m_F2%d" % i, [128, 2], F32) for i in range(3)]
        acol = p.sb(st, "m_acol", [128, 1], F32)
        wcol = p.sb(st, "m_wcol", [128, 1], F32)
        wAB_l = [p.sb(st, "m_wAB%d" % i, [128, 2], F32) for i in range(2)]
        eb_l = [p.sb(st, "m_eb%d" % i, [128, 1], F32) for i in range(3)]
        trilf_l = [p.sb(st, "m_trilf%d" % i, [128, 128], F32) for i in range(2)]
        DT_l = [p.sb(st, "m_DT%d" % i, [128, 128], F32) for i in range(3)]
        wts = p.sb(st, "m_wts", [128, 128], F32)
        intra = p.sb(st, "m_intra", [128, 129], F32)
        tot_l = [p.sb(st, "m_tot%d" % i, [128, 129], F32) for i in range(2)]
        jB = p.sb(st, "m_jB", [128, 128], BF16)
        sm = p.sb(st, "m_sm", [128, 6], F32)
        yt = [p.sb(st, "m_yt%d" % i, [128, 128], F32) for i in range(2)]
        SA = p.sb(st, "m_SA", [128, 129], F32)
        SB = p.sb(st, "m_SB", [128, 129], F32)

        p.pool.op(lambda e: e.memset(SA[:, :], 0.0), writes=[SA])
        p.pool.op(lambda e: e.memset(raw[0][:, :, :], 0.0), writes=[raw[0]])
        for v1 in v1_l:
            p.pool.op(lambda e, v1=v1: e.memset(v1[:, 128:129], 1.0), writes=[v1])

        xT3 = [p.sb(st, "m_xT3_%d" % i, [128, 8, 128], BF16) for i in range(3)] if xnT_all is not None else None
        yTb = [p.sb(st, "m_yTb%d" % i, [128, 128], BF16) for i in range(2)] if yT_send is not None else None

        def load_x(T):
            if xnT_all is not None:
                rb = ((T % 8) * 16 + T // 8) * 128
                p.sp.dma(xT3[T % 3][:, :, :], xnT_all.t[rb:rb + 128, :].rearrange("p (c n) -> p c n", c=8),
                         reads=[xnT_all], writes=[xT3[T % 3]])
            else:
                p.sp.dma(xs[T % 3][:, :], x_full[T * 128:(T + 1) * 128, :], reads=[x_full], writes=[xs[T % 3]])

        load_x(0)
        if NTILE > 1:
            load_x(1)

        def F1(T):
            if T + 2 < NTILE:
                load_x(T + 2)
            yield
            v1 = v1_l[T % 4]; so = so_l[T % 5]; gt = gt_l[T % 2]; lf3 = lf3_l[T % 2]; trilf = trilf_l[T % 2]
            x = xs[T % 3]
            s_ = ss[T % 2]
            j = junk[T % 2]
            x_n = xn[T % 2]
            xT = xnT[T % 2]
            rw = raw[T % 4]
            rwn = raw[(T + 1) % 4]
            if xnT_all is not None:
                xT = xT3[T % 3]
            else:
                p.act.op(lambda e, x=x, j=j, s_=s_: e.activation(out=j[:, :], in_=x[:, :], func=AF.Square, accum_out=s_[:, 0:1]),
                         reads=[x], writes=[j, s_])
                p.act.op(lambda e, s_=s_: e.activation(out=s_[:, 1:2], in_=s_[:, 0:1], func=AF.Ln, scale=1.0 / D, bias=p.eps_t[:, 0:1]),
                         reads=[s_, p.eps_t], writes=[s_])
                p.act.op(lambda e, s_=s_: e.activation(out=s_[:, 1:2], in_=s_[:, 1:2], func=AF.Exp, scale=-0.5), reads=[s_], writes=[s_])
                p.dve.op(lambda e, x=x, x_n=x_n, s_=s_: e.scalar_tensor_tensor(
                    out=x_n[:, :], in0=x[:, :], scalar=s_[:, 1:2], in1=gbc[:, :], op0=ALU.mult, op1=ALU.mult),
                    reads=[x, s_, gbc], writes=[x_n])
                pvv = ps[0].t[:, :].bitcast(BF16)
                for dc in range(8):
                    p.pe.op(lambda e, dc=dc, x_n=x_n: e.transpose(out=pvv[:, dc * 128:(dc + 1) * 128], in_=x_n[:, dc * 128:(dc + 1) * 128],
                                                                 identity=p.ident_b[:, :]), reads=[x_n, p.ident_b], writes=[ps[0]])
                p.act.op(lambda e, xT=xT: e.copy(out=xT[:, :, :], in_=pvv.rearrange("p (c n) -> p c n", c=8)), reads=[ps[0]], writes=[xT])
            yield
            for qk in range(2):
                for dc in range(8):
                    p.pe.op(lambda e, dc=dc, qk=qk, xT=xT: e.matmul(out=ps[1][:, qk * 128:(qk + 1) * 128],
                                                                    lhsT=w_b[:, dc, qk * 128:(qk + 1) * 128], rhs=xT[:, dc, :],
                                                                    start=(dc == 0), stop=(dc == 7)), reads=[w_b, xT], writes=[ps[1]])
            yield
            for dc in range(8):
                p.pe.op(lambda e, dc=dc, xT=xT: e.matmul(out=ps[2][:, 0:258], lhsT=xT[:, dc, :], rhs=w_b[:, dc, 256:514],
                                                         start=(dc == 0), stop=(dc == 7)), reads=[xT, w_b], writes=[ps[2]])
            yield
            p.dve.op(lambda e: e.tensor_tensor(out=gt[:, :], in0=ps[2][:, 256:258], in1=gb_bc[:, :], op=ALU.add),
                     reads=[ps[2], gb_bc], writes=[gt])
            yield
            p.act.op(lambda e: e.activation(out=g2[:, :], in_=gt[:, :], func=AF.Exp, scale=2.0 / 15.0), reads=[gt], writes=[g2])
            yield
            p.dve.op(lambda e: e.tensor_scalar(out=g2[:, :], in0=g2[:, :], scalar1=1.0, scalar2=None, op0=ALU.add), reads=[g2], writes=[g2])
            yield
            p.dve.op(lambda e: e.reciprocal(out=g2[:, :], in_=g2[:, :]), reads=[g2], writes=[g2])
            yield
            p.dve.op(lambda e: e.tensor_scalar(out=gt[:, :], in0=g2[:, :], scalar1=-30.0, scalar2=15.0, op0=ALU.mult, op1=ALU.add),
                     reads=[g2], writes=[gt])
            yield
            p.act.op(lambda e: e.activation(out=lfc[:, :], in_=gt[:, 1:2], func=AF.Exp, scale=-1.0), reads=[gt], writes=[lfc])
            yield
            p.act.op(lambda e: e.activation(out=lfc[:, :], in_=lfc[:, :], func=AF.Ln, bias=one_c[:, 0:1]), reads=[lfc, one_c], writes=[lfc])
            yield
            p.dve.op(lambda e: e.tensor_scalar(out=lf3[:, :], in0=M3, scalar1=lfc[:, 0:1], scalar2=-1.0, op0=ALU.mult, op1=ALU.mult),
                     reads=[cst, lfc], writes=[lf3])
            yield
            p.dve.op(lambda e: e.tensor_scalar(out=trilf[:, :], in0=Tri, scalar1=lf3[:, 0:1], scalar2=None, op0=ALU.mult),
                     reads=[cst, lf3], writes=[trilf])
            yield
            p.act.op(lambda e, rw=rw: e.copy(out=rw[:, :, 3:131], in_=ps[1][:, 0:256].rearrange("p (a n) -> p a n", a=2)),
                     reads=[ps[1]], writes=[rw])
            yield
            p.pool.op(lambda e, rw=rw, rwn=rwn: e.tensor_copy(out=rwn[:, :, 0:3], in_=rw[:, :, 128:131]), reads=[rw], writes=[rwn])
            yield
            p.act.op(lambda e: e.copy(out=v1[:, 0:128], in_=ps[2][:, 0:128]), reads=[ps[2]], writes=[v1])
            yield
            p.act.op(lambda e: e.activation(out=so[:, :], in_=ps[2][:, 128:256], func=AF.Exp, scale=-1.0), reads=[ps[2]], writes=[so])
            yield
            p.pool.op(lambda e: e.tensor_scalar(out=so[:, :], in0=so[:, :], scalar1=1.0, scalar2=None, op0=ALU.add), reads=[so], writes=[so])
            yield
            p.dve.op(lambda e: e.reciprocal(out=so[:, :], in_=so[:, :]), reads=[so], writes=[so])
            yield


        def F2S(T):
            gt = gt_l[T % 2]; lf3 = lf3_l[T % 2]; trilf = trilf_l[T % 2]; F2 = F2_l[T % 3]; eb = eb_l[T % 3]; DT = DT_l[T % 3]; wAB = wAB_l[T % 2]
            p.pe.op(lambda e: e.matmul(out=ps[3][:, 0:1], lhsT=Tri, rhs=lf3[:, 0:1], start=True, stop=True), reads=[cst, lf3], writes=[ps[3]])
            yield
            p.pe.op(lambda e: e.matmul(out=ps[3][:, 1:2], lhsT=Blk, rhs=lf3[:, 0:1], start=True, stop=True), reads=[cst, lf3], writes=[ps[3]])
            yield
            p.pe.op(lambda e: e.matmul(out=ps[3][:, 2:4], lhsT=ones_f[:, :], rhs=lf3[:, 1:3], start=True, stop=True),
                    reads=[ones_f, lf3], writes=[ps[3]])
            yield
            p.act.op(lambda e: e.copy(out=g4[:, :], in_=ps[3][:, 0:4]), reads=[ps[3]], writes=[g4])
            yield
            p.act.op(lambda e: e.activation(out=F2[:, :], in_=g4[:, 2:4], func=AF.Exp), reads=[g4], writes=[F2])
            yield
            p.dve.op(lambda e: e.tensor_tensor(out=acol[:, :], in0=gt[:, 0:1], in1=g4[:, 0:1], op=ALU.subtract), reads=[gt, g4], writes=[acol])
            yield
            p.act.op(lambda e: e.activation(out=wcol[:, :], in_=g4[:, 1:2], func=AF.Exp, bias=acol[:, 0:1]), reads=[g4, acol], writes=[wcol])
            yield
            p.act.op(lambda e: e.activation(out=eb[:, :], in_=g4[:, 0:1], func=AF.Exp), reads=[g4], writes=[eb])
            yield
            p.dve.op(lambda e: e.tensor_scalar(out=wAB[:, :], in0=M3[:, 1:3], scalar1=wcol[:, 0:1], scalar2=None, op0=ALU.mult),
                     reads=[cst, wcol], writes=[wAB])
            yield
            p.pe.op(lambda e: e.matmul(out=ps[4][:, 0:128], lhsT=ones_f[:, :], rhs=trilf[:, :], start=True, stop=False),
                    reads=[ones_f, trilf], writes=[ps[4]])
            yield
            p.pe.op(lambda e: e.matmul(out=ps[4][:, 0:128], lhsT=p.ident_f[:, :], rhs=Neg, start=False, stop=True),
                    reads=[p.ident_f, cst], writes=[ps[4]])
            yield
            p.act.op(lambda e: e.activation(out=DT[:, :], in_=ps[4][:, 0:128], func=AF.Exp, bias=acol[:, 0:1]),
                     reads=[ps[4], acol], writes=[DT])
            yield

        def F3(T):
            rw = raw[T % 4]; wAB = wAB_l[T % 2]
            qT = qT_l[T % 2]; kT = kT_l[T % 2]; qbA = qbA_l[T % 2]; qbB = qbB_l[T % 2]; kwA = kwA_l[T % 2]; kwB = kwB_l[T % 2]
            for qk in range(2):
                p.dve.op(lambda e, qk=qk, rw=rw: e.tensor_scalar(out=acc[:, qk, :], in0=rw[:, qk, 3:131], scalar1=cw_t[:, qk * 4 + 3:qk * 4 + 4],
                                                                 scalar2=cb_t[:, qk:qk + 1], op0=ALU.mult, op1=ALU.add),
                         reads=[rw, cw_t, cb_t], writes=[acc])
                for dlt in range(1, 4):
                    p.dve.op(lambda e, qk=qk, rw=rw, dlt=dlt: e.scalar_tensor_tensor(
                        out=acc[:, qk, :], in0=rw[:, qk, 3 - dlt:131 - dlt], scalar=cw_t[:, qk * 4 + 3 - dlt:qk * 4 + 4 - dlt],
                        in1=acc[:, qk, :], op0=ALU.mult, op1=ALU.add), reads=[rw, cw_t, acc], writes=[acc])
            yield
            p.act.op(lambda e: e.activation(out=sg[:, :, :], in_=acc[:, :, :], func=AF.Exp, scale=-1.0), reads=[acc], writes=[sg])
            yield
            p.dve.op(lambda e: e.tensor_scalar(out=sg[:, :, :], in0=sg[:, :, :], scalar1=1.0, scalar2=None, op0=ALU.add), reads=[sg], writes=[sg])
            yield
            p.dve.op(lambda e: e.reciprocal(out=sg[:, :, :], in_=sg[:, :, :]), reads=[sg], writes=[sg])
            yield
            p.dve.op(lambda e: e.scalar_tensor_tensor(out=qT[:, :], in0=acc[:, 0, :], scalar=0.125, in1=sg[:, 0, :], op0=ALU.mult, op1=ALU.mult),
                     reads=[acc, sg], writes=[qT])
            yield
            p.dve.op(lambda e: e.tensor_tensor(out=kT[:, :], in0=acc[:, 1, :], in1=sg[:, 1, :], op=ALU.mult), reads=[acc, sg], writes=[kT])
            yield
            p.pool.op(lambda e: e.tensor_tensor(out=qbA[:, :], in0=qT[:, :], in1=MA2, op=ALU.mult), reads=[qT, cst], writes=[qbA])
            yield
            p.pool.op(lambda e: e.tensor_tensor(out=qbB[:, :], in0=qT[:, :], in1=MB2, op=ALU.mult), reads=[qT, cst], writes=[qbB])
            yield
            p.pe.op(lambda e: e.transpose(out=ps[5][:, 0:128], in_=kT[:, :], identity=p.ident_f[:, :]), reads=[kT, p.ident_f], writes=[ps[5]])
            yield
            p.act.op(lambda e: e.activation(out=kwA[:, :], in_=ps[5][:, 0:128], func=AF.Copy, scale=wAB[:, 0:1]), reads=[ps[5], wAB], writes=[kwA])
            yield
            p.dve.op(lambda e: e.tensor_scalar(out=kwB[:, :], in0=ps[5][:, 0:128], scalar1=wAB[:, 1:2], scalar2=None, op0=ALU.mult),
                     reads=[ps[5], wAB], writes=[kwB])
            yield

        def MIDDLE(T):
            qT = qT_l[T % 2]; kT = kT_l[T % 2]; qbA = qbA_l[T % 2]; qbB = qbB_l[T % 2]; kwA = kwA_l[T % 2]; kwB = kwB_l[T % 2]
            v1 = v1_l[T % 4]; F2 = F2_l[T % 3]; eb = eb_l[T % 3]; DT = DT_l[T % 3]; tot = tot_l[T % 2]
            p.pe.op(lambda e: e.matmul(out=ps[7][:, 130:258], lhsT=kT[:, :], rhs=qT[:, :], start=True, stop=True), reads=[kT, qT], writes=[ps[7]])
            yield
            p.dve.op(lambda e: e.scalar_tensor_tensor(out=wts[:, :], in0=ps[7][:, 130:258], scalar=0.5, in1=DT[:, :], op0=ALU.mult, op1=ALU.mult),
                     reads=[ps[7], DT], writes=[wts])
            yield
            p.pe.op(lambda e: e.matmul(out=ps[7][:, 0:129], lhsT=kwA[:, :], rhs=v1[:, :], start=True, stop=True), reads=[kwA, v1], writes=[ps[7]])
            yield
            p.dve.op(lambda e: e.scalar_tensor_tensor(out=SB[:, :], in0=SA[:, :], scalar=F2[:, 0:1], in1=ps[7][:, 0:129], op0=ALU.mult, op1=ALU.add),
                     reads=[SA, F2, ps[7]], writes=[SB])
            yield
            p.pe.op(lambda e: e.matmul(out=ps[6][:, 0:129], lhsT=wts[:, :], rhs=v1[:, :], start=True, stop=True), reads=[wts, v1], writes=[ps[6]])
            yield
            p.pe.op(lambda e: e.matmul(out=ps[6][:, 256:385], lhsT=qbA[:, :], rhs=SA[:, :], start=True, stop=False), reads=[qbA, SA], writes=[ps[6]])
            yield
            p.pe.op(lambda e: e.matmul(out=ps[6][:, 256:385], lhsT=qbB[:, :], rhs=SB[:, :], start=False, stop=True), reads=[qbB, SB], writes=[ps[6]])
            yield
            p.act.op(lambda e: e.copy(out=intra[:, :], in_=ps[6][:, 0:129]), reads=[ps[6]], writes=[intra])
            yield
            p.dve.op(lambda e: e.scalar_tensor_tensor(out=tot[:, :], in0=ps[6][:, 256:385], scalar=eb[:, 0:1], in1=intra[:, :], op0=ALU.mult, op1=ALU.add),
                     reads=[ps[6], eb, intra], writes=[tot])
            yield
            p.pe.op(lambda e: e.matmul(out=ps[7][:, 260:389], lhsT=kwB[:, :], rhs=v1[:, :], start=True, stop=True), reads=[kwB, v1], writes=[ps[7]])
            yield
            p.dve.op(lambda e: e.scalar_tensor_tensor(out=SA[:, :], in0=SB[:, :], scalar=F2[:, 1:2], in1=ps[7][:, 260:389], op0=ALU.mult, op1=ALU.add),
                     reads=[SB, F2, ps[7]], writes=[SA])
            yield

        def BACK(T):
            so = so_l[T % 5]; tot = tot_l[T % 2]; j = jB
            y = yt[T % 2]
            p.dve.op(lambda e: e.scalar_tensor_tensor(out=sm[:, 0:1], in0=tot[:, 128:129], scalar=-1.0, in1=tot[:, 128:129], op0=ALU.mult, op1=ALU.max), reads=[tot], writes=[sm])
            yield
            p.dve.op(lambda e: e.tensor_scalar(out=sm[:, 0:1], in0=sm[:, 0:1], scalar1=1.0, scalar2=None, op0=ALU.max), reads=[sm], writes=[sm])
            yield
            p.dve.op(lambda e: e.reciprocal(out=sm[:, 1:2], in_=sm[:, 0:1]), reads=[sm], writes=[sm])
            yield
            p.act.op(lambda e, j=j: e.activation(out=j[:, 0:128], in_=tot[:, 0:128], func=AF.Square, accum_out=sm[:, 2:3]), reads=[tot], writes=[j, sm])
            yield
            p.dve.op(lambda e: e.tensor_scalar(out=sm[:, 3:4], in0=sm[:, 1:2], scalar1=sm[:, 1:2], scalar2=1.0 / 128, op0=ALU.mult, op1=ALU.mult),
                     reads=[sm], writes=[sm])
            yield
            p.act.op(lambda e: e.activation(out=sm[:, 4:5], in_=sm[:, 2:3], func=AF.Ln, scale=sm[:, 3:4], bias=p.eps_t[:, 0:1]),
                     reads=[sm, p.eps_t], writes=[sm])
            yield
            p.act.op(lambda e: e.activation(out=sm[:, 4:5], in_=sm[:, 4:5], func=AF.Exp, scale=-0.5), reads=[sm], writes=[sm])
            yield
            p.dve.op(lambda e: e.tensor_tensor(out=sm[:, 5:6], in0=sm[:, 4:5], in1=sm[:, 1:2], op=ALU.mult), reads=[sm], writes=[sm])
            yield
            p.dve.op(lambda e, y=y: e.scalar_tensor_tensor(out=y[:, :], in0=tot[:, 0:128], scalar=sm[:, 5:6], in1=hn_bc[:, :], op0=ALU.mult, op1=ALU.mult),
                     reads=[tot, sm, hn_bc], writes=[y])
            yield
            p.pool.op(lambda e, y=y: e.tensor_tensor(out=y[:, :], in0=y[:, :], in1=so[:, :], op=ALU.mult), reads=[y, so], writes=[y])
            yield
            if yT_send is not None:
                yb = yTb[T % 2]
                p.pe.op(lambda e, y=y: e.transpose(out=ps[0][:, 0:128], in_=y[:, :], identity=p.ident_f[:, :]),
                        reads=[y, p.ident_f], writes=[ps[0]])
                p.act.op(lambda e, yb=yb: e.copy(out=yb[:, :], in_=ps[0][:, 0:128]), reads=[ps[0]], writes=[yb])
                c0 = (T % 8) * 2048 + (T // 8) * 128
                p.sp.dma(yT_send.t[:, c0:c0 + 128], yb[:, :], reads=[yb], writes=[yT_send])
            else:
                p.sp.dma(y_out[T * 128:(T + 1) * 128, :], y[:, :], reads=[y], writes=[y_out])

            yield
        for it in range(NTILE + 4):
            gens = []
            if 0 <= it - 4 < NTILE:
                gens.append(BACK(it - 4))
            if 0 <= it - 3 < NTILE:
                gens.append(MIDDLE(it - 3))
            if 0 <= it - 2 < NTILE:
                gens.append(F3(it - 2))
            if 0 <= it - 1 < NTILE:
                gens.append(F2S(it - 1))
            if it < NTILE:
                gens.append(F1(it))
            while gens:
                for g in list(gens):
                    try:
                        next(g)
                    except StopIteration:
                        gens.remove(g)
        p.barrier()

N_CORES = 8
S_TOK = 16384


def _make_masks(c):
    m = np.zeros((8, 128, 4, 128), np.float32)
    k = np.arange(128)[:, None]
    q = np.arange(128)[None, :]
    for j in range(8):
        if j == c:
            m[j] = np.where(k <= q, 0.0, -30000.0)[:, None, :]
        elif j > c:
            m[j] = -30000.0
    return m.reshape(8, 128, 512)


def build_A():
    p = Prog()
    with ExitStack() as st:
        setup_consts(p, st)
        di = lambda n, s, dt=F32: p.dram(n, s, dt, "ExternalInput")
        x_full = di("x_full", [S_TOK, 1024]); x_own = di("x_own", [2048, 1024])
        posT = di("posT", [128, 128], I32); pos_own = di("pos_own", [128, 16], I32)
        inv_freq = di("inv_freq", [32]); masks = di("masks", [8, 128, 512])
        g_mix0 = di("g_mix0", [1024]); w_in = di("w_in", [1024, 448]); q_norm = di("q_norm", [256])
        w_qb = di("w_qb", [256, 1536]); kv_norm = di("kv_norm", [128]); w_kvb = di("w_kvb", [128, 2048])
        w_out = di("w_out", [1024, 1024])
        g_ffn0 = di("g_ffn0", [1024]); fwg = di("fwg", [1, 1024, 3584]); fwu = di("fwu", [1, 1024, 3584]); fwd = di("fwd", [1, 3584, 1024])
        g_mix1 = di("g_mix1", [1024])
        x1_s = p.dram("x1_s", [2048, 1024], F32, "ExternalOutput")
        xnT_send = p.dram("xnT_send", [2048, 1024], BF16, "ExternalOutput")
        xa_s = p.dram_internal("xa_s", [2048, 1024], F32)
        phase_attn(p, x_full, x_own, posT, pos_own, inv_freq, masks, g_mix0, w_in, q_norm, w_qb, kv_norm, w_kvb, w_out, xa_s)
        phase_ffn(p, xa_s, x1_s, g_ffn0, fwg, fwu, fwd, 1)
        phase_norm_send(p, x1_s, g_mix1, xnT_send)
        nc = p.finish()
    return nc


def build_B():
    p = Prog()
    with ExitStack() as st:
        setup_consts(p, st)
        di = lambda n, s, dt=F32: p.dram(n, s, dt, "ExternalInput")
        xnT_all = di("xnT_all", [8 * 2048, 1024], BF16)
        g_mix1 = di("g_mix1", [1024]); w_h = di("w_h", [1024, 514]); cw = di("cw", [128, 8]); cb = di("cb", [128, 2])
        gb = di("gate_b", [2]); hn = di("head_norm", [128]); cs = di("consts", [128, NCONST])
        yT_send = p.dram("yT_send", [128, S_TOK], BF16, "ExternalOutput")
        phase_mlstm(p, None, g_mix1, w_h, cw, cb, gb, hn, cs, None, xnT_all=xnT_all, yT_send=yT_send)
        nc = p.finish()
    return nc


def build_C():
    p = Prog()
    with ExitStack() as st:
        setup_consts(p, st)
        di = lambda n, s, dt=F32: p.dram(n, s, dt, "ExternalInput")
        x1_s = di("x1_s", [2048, 1024]); yT_all = di("yT_all", [8 * 128, S_TOK], BF16); m_wo = di("m_wo", [1024, 1024])
        g_ffn1 = di("g_ffn1", [1024]); mwg = di("mwg", [8, 1024, 3584]); mwu = di("mwu", [8, 1024, 3584]); mwd = di("mwd", [8, 3584, 1024])
        rt = di("rt", [8, 1024]); gf = di("gf", [1024])
        xout = p.dram("xout", [2048, 1024], F32, "ExternalOutput")
        phase_ffn(p, x1_s, xout, g_ffn1, mwg, mwu, mwd, 8, routerT=rt, final_g=gf, ymT=yT_all, w_o=m_wo, ymT_dyn=True)
        nc = p.finish()
    return nc


def make_maps(inp):
    x = np.ascontiguousarray(inp["x"][0].astype(np.float32, copy=False))
    xt = x.reshape(128, 128, 1024)
    post = np.ascontiguousarray(inp["positions"][0].astype(np.int32)).reshape(128, 128)
    inv = (1.0 / (10000.0 ** (np.arange(0, 64, 2, dtype=np.float32) / 64))).astype(np.float32)
    w_in = inp["mlstm_w_in"][0]; conv_w = inp["mlstm_conv_w"][0]; conv_b = inp["mlstm_conv_b"][0]
    gate_b = inp["mlstm_gate_b"][0]; head_norm = inp["mlstm_head_norm"][0]
    consts = mlstm_consts_np()
    posT = np.ascontiguousarray(post.T)
    rtT = np.ascontiguousarray(inp["moe_router"][0].T)
    maps = []
    for c in range(N_CORES):
        own = [8 * i + c for i in range(16)]
        h = c
        q = w_in[:, h * 64:(h + 1) * 64]; k = w_in[:, 512 + h * 64:512 + (h + 1) * 64]
        v = w_in[:, 1024 + h * 128:1024 + (h + 1) * 128]; o = w_in[:, 2048 + h * 128:2048 + (h + 1) * 128]
        ig = w_in[:, 3072 + h:3073 + h]; fg = w_in[:, 3080 + h:3081 + h]
        wh = np.ascontiguousarray(np.concatenate([q, q, k, k, v, o, ig, fg], axis=1))
        cq = conv_w[:, h * 64:(h + 1) * 64].T; ck = conv_w[:, 512 + h * 64:512 + (h + 1) * 64].T
        cwh = np.ascontiguousarray(np.concatenate([np.concatenate([cq, cq], 0), np.concatenate([ck, ck], 0)], axis=1))
        bq = conv_b[h * 64:(h + 1) * 64]; bk = conv_b[512 + h * 64:512 + (h + 1) * 64]
        cbh = np.ascontiguousarray(np.stack([np.concatenate([bq, bq]), np.concatenate([bk, bk])], axis=1))
        maps.append({
            "x_full": x, "x_own": np.ascontiguousarray(xt[own].reshape(-1, 1024)),
            "posT": posT, "pos_own": np.ascontiguousarray(post[own].T), "inv_freq": inv, "masks": _make_masks(c),
            "g_mix0": inp["norm_mix"][0], "w_in": inp["mla_w_in"][0], "q_norm": inp["mla_q_norm"][0], "w_qb": inp["mla_w_qb"][0],
            "kv_norm": inp["mla_kv_norm"][0], "w_kvb": inp["mla_w_kvb"][0], "w_out": inp["mla_w_out"][0],
            "g_ffn0": inp["norm_ffn"][0], "fwg": inp["ffn_w_gate"], "fwu": inp["ffn_w_up"], "fwd": inp["ffn_w_down"],
            "g_mix1": inp["norm_mix"][1], "w_h": wh, "cw": cwh, "cb": cbh,
            "gate_b": np.ascontiguousarray(np.stack([gate_b[h], gate_b[8 + h]])),
            "head_norm": np.ascontiguousarray(head_norm[h * 128:(h + 1) * 128]), "consts": consts, "m_wo": inp["mlstm_w_out"][0],
            "g_ffn1": inp["norm_ffn"][1], "mwg": inp["moe_w_gate"][0], "mwu": inp["moe_w_up"][0], "mwd": inp["moe_w_down"][0],
            "rt": rtT, "gf": inp["final_norm"],
        })
    return maps


def kernel(**inputs):
    inp = {k: np.asarray(v) for k, v in inputs.items()}
    maps = make_maps(inp)
    ka = ["x_full", "x_own", "posT", "pos_own", "inv_freq", "masks", "g_mix0", "w_in", "q_norm", "w_qb", "kv_norm", "w_kvb", "w_out",
          "g_ffn0", "fwg", "fwu", "fwd", "g_mix1"]
    ra = run_bass_kernel_spmd(build_A(), [{k: m[k] for k in ka} for m in maps], core_ids=list(range(N_CORES))).results
    xnT_all = np.concatenate([ra[c]["xnT_send"] for c in range(N_CORES)], axis=0)
    kb = ["g_mix1", "w_h", "cw", "cb", "gate_b", "head_norm", "consts"]
    rb = run_bass_kernel_spmd(build_B(), [dict({k: m[k] for k in kb}, xnT_all=xnT_all) for m in maps], core_ids=list(range(N_CORES))).results
    yT_all = np.concatenate([rb[c]["yT_send"] for c in range(N_CORES)], axis=0)
    kc = ["m_wo", "g_ffn1", "mwg", "mwu", "mwd", "rt", "gf"]
    rc = run_bass_kernel_spmd(build_C(), [dict({k: m[k] for k in kc}, x1_s=ra[c]["x1_s"], yT_all=yT_all) for c, m in enumerate(maps)],
                              core_ids=list(range(N_CORES))).results
    out = np.empty((128, 128, 1024), np.float32)
    for c in range(N_CORES):
        own = [8 * i + c for i in range(16)]
        out[own] = rc[c]["xout"].reshape(16, 128, 1024)
    return out.reshape(1, S_TOK, 1024)
```

```python
from contextlib import ExitStack
import numpy as np
import concourse.bass as bass
import concourse.mybir as mybir
from concourse.bass_utils import run_bass_kernel_spmd

F32 = mybir.dt.float32
BF16 = mybir.dt.bfloat16
I32 = mybir.dt.int32
AF = mybir.ActivationFunctionType
ALU = mybir.AluOpType
AX = mybir.AxisListType


class T:
    def __init__(self, t, name):
        self.t = t
        self.name = name
        self.w = {}
        self.r = {}

    def __getitem__(self, k):
        return self.t[k]


class Eng:
    def __init__(self, prog, name, eng, nring):
        self.p = prog
        self.name = name
        self.eng = eng
        self.sem = prog.new_sem("c_" + name)
        self.count = 0
        self.seen = {}
        self.ring = [prog.new_sem("d_%s%d" % (name, i)) for i in range(nring)]
        self.ring_cnt = [0] * nring
        self.ring_pos = 0

    def _wait(self, mark):
        if mark is None:
            return
        sem, val = mark
        k = id(sem)
        if self.seen.get(k, 0) < val:
            self.eng.wait_ge(sem, val)
            self.seen[k] = val

    def _deps(self, reads, writes, skip_self, dma_ring=None):
        for b in reads:
            for m in b.w.values():
                if not (skip_self and m[0] is self.sem):
                    self._wait(m)
        for b in writes:
            for m in b.w.values():
                if dma_ring is not None and id(m[0]) in dma_ring:
                    continue
                if not (skip_self and m[0] is self.sem):
                    self._wait(m)
            for m in b.r.values():
                if not (skip_self and m[0] is self.sem):
                    self._wait(m)

    def _mark(self, reads, writes, mark, dma_ring=None):
        for b in reads:
            b.r[id(mark[0])] = mark
        for b in writes:
            if dma_ring is not None:
                b.w = {k: v for k, v in b.w.items() if k in dma_ring}
            else:
                b.w = {}
            b.w[id(mark[0])] = mark
            b.r = {}

    def op(self, fn, reads=(), writes=()):
        ex = [b for b in reads if getattr(b, "excl", False)]
        if ex:
            reads = [b for b in reads if not getattr(b, "excl", False)]
            writes = list(writes) + ex
        self._deps(reads, writes, skip_self=(self.name == "pe"))
        inst = fn(self.eng)
        self.count += 1
        inst.then_inc(self.sem, 1)
        self._mark(reads, writes, (self.sem, self.count))

    def dma(self, out, in_, reads=(), writes=(), **kw):
        ring_ids = set(id(x) for x in self.ring)
        self._deps(reads, writes, skip_self=False, dma_ring=ring_ids)
        j = self.ring_pos
        self.ring_pos = (j + 1) % len(self.ring)
        sem = self.ring[j]
        if self.ring_cnt[j] > 0:
            self._wait((sem, self.ring_cnt[j]))
        inst = self.eng.dma_start(out=out, in_=in_, **kw)
        self.ring_cnt[j] += 16
        inst.then_inc(sem, 16)
        self._mark(reads, writes, (sem, self.ring_cnt[j]), dma_ring=ring_ids)

    def wait_all(self, marks):
        for m in marks:
            self._wait(m)


class Prog:
    def __init__(self):
        self.nc = bass.Bass("TRN2", target_bir_lowering=False)
        self.es = ExitStack()
        self.nsem = 0
        nc = self.nc
        self.pe = Eng(self, "pe", nc.tensor, 0)
        self.act = Eng(self, "act", nc.scalar, 8)
        self.dve = Eng(self, "dve", nc.vector, 0)
        self.pool = Eng(self, "pool", nc.gpsimd, 24)
        self.sp = Eng(self, "sp", nc.sync, 24)
        self.engs = [self.pe, self.act, self.dve, self.pool, self.sp]
        self.dram_ts = []

    def new_sem(self, name):
        self.nsem += 1
        return self.es.enter_context(self.nc.semaphore(name))

    def dram(self, name, shape, dtype, kind, **kw):
        t = T(self.nc.dram_tensor(name, list(shape), dtype, kind=kind, **kw).ap(), name)
        self.dram_ts.append(t)
        return t

    def sb(self, stack, name, shape, dtype):
        self.uid = getattr(self, "uid", 0) + 1
        name = "%s_u%d" % (name, self.uid)
        return T(stack.enter_context(self.nc.sbuf_tensor(name, list(shape), dtype)), name)

    def ps(self, stack, name, shape, dtype):
        self.uid = getattr(self, "uid", 0) + 1
        name = "%s_u%d" % (name, self.uid)
        t = T(stack.enter_context(self.nc.psum_tensor(name, list(shape), dtype)), name)
        t.excl = True
        return t

    def dram_internal(self, name, shape, dtype):
        t = T(self.nc.dram_tensor(name, list(shape), dtype).ap(), name)
        self.dram_ts.append(t)
        return t

    def allgather(self, src, dst):
        e = self.pool
        e._deps([src], [dst], False)
        sem = self.new_sem("cc%d" % self.nsem)
        inst = self.nc.gpsimd.collective_compute("AllGather", ALU.bypass, replica_groups=[list(range(8))],
                                                 ins=[src.t], outs=[dst.t])
        inst.then_inc(sem, 1)
        e._mark([src], [dst], (sem, 1))
        self.extra_marks = getattr(self, "extra_marks", []) + [(sem, 1)]

    def barrier(self):
        marks = list(getattr(self, "extra_marks", []))
        for e in self.engs:
            if e.count > 0:
                marks.append((e.sem, e.count))
            for j, s in enumerate(e.ring):
                if e.ring_cnt[j] > 0:
                    marks.append((s, e.ring_cnt[j]))
        for e in self.engs:
            e.wait_all(marks)

    def finish(self):
        self.barrier()
        self.es.close()
        return self.nc


D = 1024
DFF = 3584
EPS = 1e-6


def rms_rstd(p, ss, rstd, n, n_feat):
    p.act.op(lambda e: e.activation(out=rstd[:, 0:n], in_=ss[:, 0:n], func=AF.Ln,
                                    scale=1.0 / n_feat, bias=p.eps_t[:, 0:1]),
             reads=[ss, p.eps_t], writes=[rstd])
    p.act.op(lambda e: e.activation(out=rstd[:, 0:n], in_=rstd[:, 0:n], func=AF.Exp, scale=-0.5),
             reads=[rstd], writes=[rstd])


def setup_consts(p, stack):
    nc = p.nc
    p.eps_t = p.sb(stack, "eps_t", [128, 1], F32)
    p.pool.op(lambda e: e.memset(p.eps_t[:, :], EPS), writes=[p.eps_t])
    p.ident_f = p.sb(stack, "ident_f", [128, 128], F32)
    p.pool.op(lambda e: e.memset(p.ident_f[:, :], 1.0), writes=[p.ident_f])
    p.pool.op(lambda e: e.affine_select(out=p.ident_f[:, :], in_=p.ident_f[:, :], pattern=[[-1, 128]],
                                        compare_op=ALU.is_equal, fill=0.0, base=0, channel_multiplier=1),
              reads=[p.ident_f], writes=[p.ident_f])
    p.ident_b = p.sb(stack, "ident_b", [128, 128], BF16)
    p.pool.op(lambda e: e.tensor_copy(out=p.ident_b[:, :], in_=p.ident_f[:, :]), reads=[p.ident_f], writes=[p.ident_b])


def phase_ffn(p, xin, xout, g_norm, wg, wu, wd, E, routerT=None, final_g=None, NT=16, FCP=2, ymT=None, w_o=None, ymT_dyn=False):
    nc = p.nc
    NTOK = NT * 128
    NTG = NTOK // 512
    FS = FCP * 128
    NFS = DFF // FS
    with ExitStack() as st:
        xres = [p.sb(st, "xres%d" % t, [128, D], F32) for t in range(NT)]
        xnT = p.sb(st, "xnT", [128, 8, NTOK], BF16)
        gbc = p.sb(st, "gbc", [128, D], F32)
        ss = p.sb(st, "ss", [128, NT], F32)
        rstd = p.sb(st, "rstd", [128, NT], F32)
        comb = p.sb(st, "comb", [128, NT, 8], F32)
        psb = [p.ps(st, "psb%d" % i, [128, 512], F32) for i in range(8)]

        p.sp.dma(gbc[:, :], g_norm.t.partition_broadcast(128), reads=[g_norm], writes=[gbc])
        for t in range(NT):
            p.sp.dma(xres[t][:, :], xin[t * 128:(t + 1) * 128, :], reads=[xin], writes=[xres[t]])

        if ymT is not None:
            with ExitStack() as s0:
                ymT_b = p.sb(s0, "ymT_b", [128, 8, NTOK], BF16)
                wo_b = p.sb(s0, "wo_b", [128, 8, D], BF16)
                if ymT_dyn:
                    pid = p.nc.gpsimd.partition_id()
                    for hc in range(8):
                        p.pool.dma(ymT_b[:, hc, :], ymT.t[hc * 128:(hc + 1) * 128, bass.ds(pid * NTOK, NTOK)],
                                   reads=[ymT], writes=[ymT_b])
                else:
                    p.pool.dma(ymT_b[:, :, :], ymT.t.rearrange("(hc p) t -> p hc t", p=128), reads=[ymT], writes=[ymT_b])
                p.pool.dma(wo_b[:, :, :], w_o.t.rearrange("(hc p) d -> p hc d", p=128), reads=[w_o], writes=[wo_b])
                for t in range(NT):
                    for half in range(2):
                        pd = psb[4 + (2 * t + half) % 4]
                        for hc in range(8):
                            p.pe.op(lambda e, t=t, half=half, hc=hc, pd=pd: e.matmul(
                                out=pd[:, :], lhsT=ymT_b[:, hc, t * 128:(t + 1) * 128], rhs=wo_b[:, hc, half * 512:(half + 1) * 512],
                                start=(hc == 0), stop=(hc == 7)), reads=[ymT_b, wo_b], writes=[pd])
                        p.dve.op(lambda e, t=t, half=half, pd=pd: e.tensor_tensor(
                            out=xres[t][:, half * 512:(half + 1) * 512], in0=pd[:, :], in1=xres[t][:, half * 512:(half + 1) * 512],
                            op=ALU.add), reads=[pd, xres[t]], writes=[xres[t]])
                p.barrier()
        with ExitStack() as st2:
            junk = [p.sb(st2, "junk%d" % i, [128, D], BF16) for i in range(2)]
            xn = [p.sb(st2, "xn%d" % i, [128, D], BF16) for i in range(2)]
            for t in range(NT):
                j = junk[t % 2]
                p.act.op(lambda e, t=t, j=j: e.activation(out=j[:, :], in_=xres[t][:, :], func=AF.Square,
                                                         accum_out=ss[:, t:t + 1]),
                         reads=[xres[t]], writes=[j, ss])
            rms_rstd(p, ss, rstd, NT, D)
            if E > 1:
                xn32 = [p.sb(st2, "xn32_%d" % i, [128, D], F32) for i in range(2)]
                rT = p.sb(st2, "rT", [128, 8, D], F32)
                logit = p.sb(st2, "logit", [128, NT, 8], F32)
                for e_ in range(8):
                    p.sp.dma(rT[:, e_, :], routerT.t[e_].partition_broadcast(128), reads=[routerT], writes=[rT])
            for t in range(NT):
                x_n = xn[t % 2]
                p.dve.op(lambda e, t=t, x_n=x_n: e.scalar_tensor_tensor(
                    out=x_n[:, :], in0=xres[t][:, :], scalar=rstd[:, t:t + 1], in1=gbc[:, :],
                    op0=ALU.mult, op1=ALU.mult), reads=[xres[t], rstd, gbc], writes=[x_n])
                pst = psb[t % 2]
                pv = pst.t[:, :].bitcast(BF16)
                for dc in range(8):
                    p.pe.op(lambda e, dc=dc, x_n=x_n, pv=pv: e.transpose(
                        out=pv[:, dc * 128:(dc + 1) * 128], in_=x_n[:, dc * 128:(dc + 1) * 128], identity=p.ident_b[:, :]),
                        reads=[x_n, p.ident_b], writes=[pst])
                ev = p.act if t % 2 == 0 else p.dve
                if ev is p.act:
                    ev.op(lambda e, t=t, pv=pv: e.copy(out=xnT[:, :, t * 128:(t + 1) * 128],
                                                      in_=pv.rearrange("p (c n) -> p c n", c=8)),
                          reads=[pst], writes=[xnT])
                else:
                    ev.op(lambda e, t=t, pv=pv: e.tensor_copy(out=xnT[:, :, t * 128:(t + 1) * 128],
                                                             in_=pv.rearrange("p (c n) -> p c n", c=8)),
                          reads=[pst], writes=[xnT])
                if E > 1:
                    x32 = xn32[t % 2]
                    p.pool.op(lambda e, t=t, x32=x32: e.tensor_scalar(
                        out=x32[:, :], in0=xres[t][:, :], scalar1=rstd[:, t:t + 1], scalar2=None, op0=ALU.mult),
                        reads=[xres[t], rstd], writes=[x32])
                    p.pool.op(lambda e, x32=x32: e.tensor_tensor(out=x32[:, :], in0=x32[:, :], in1=gbc[:, :], op=ALU.mult),
                              reads=[x32, gbc], writes=[x32])
                    for e_ in range(8):
                        jj = junk[e_ % 2]
                        p.dve.op(lambda e, t=t, e_=e_, x32=x32, jj=jj: e.scalar_tensor_tensor(
                            out=jj[:, :], in0=x32[:, :], scalar=1.0, in1=rT[:, e_, :], op0=ALU.mult, op1=ALU.mult,
                            accum_out=logit[:, t, e_:e_ + 1]), reads=[x32, rT], writes=[jj, logit])
            if E > 1:
                m1 = p.sb(st2, "m1", [128, NT], F32)
                m2 = p.sb(st2, "m2", [128, NT], F32)
                tmp = p.sb(st2, "tmp", [128, NT, 8], F32)
                tmp2 = p.sb(st2, "tmp2", [128, NT, 8], F32)
                sh = [128, NT, 8]
                p.dve.op(lambda e: e.tensor_reduce(out=m1[:, :], in_=logit[:, :, :], axis=AX.X, op=ALU.max),
                         reads=[logit], writes=[m1])
                p.dve.op(lambda e: e.tensor_tensor(out=tmp[:, :, :], in0=logit[:, :, :],
                                                   in1=m1[:, :].unsqueeze(2).to_broadcast(sh), op=ALU.is_equal),
                         reads=[logit, m1], writes=[tmp])
                p.dve.op(lambda e: e.scalar_tensor_tensor(out=tmp[:, :, :], in0=tmp[:, :, :], scalar=-1e30,
                                                          in1=logit[:, :, :], op0=ALU.mult, op1=ALU.add),
                         reads=[tmp, logit], writes=[tmp])
                p.dve.op(lambda e: e.tensor_reduce(out=m2[:, :], in_=tmp[:, :, :], axis=AX.X, op=ALU.max),
                         reads=[tmp], writes=[m2])
                p.dve.op(lambda e: e.tensor_tensor(out=tmp[:, :, :], in0=logit[:, :, :],
                                                   in1=m2[:, :].unsqueeze(2).to_broadcast(sh), op=ALU.is_ge),
                         reads=[logit, m2], writes=[tmp])
                p.dve.op(lambda e: e.tensor_tensor(out=tmp2[:, :, :], in0=logit[:, :, :],
                                                   in1=m1[:, :].unsqueeze(2).to_broadcast(sh), op=ALU.subtract),
                         reads=[logit, m1], writes=[tmp2])
                p.act.op(lambda e: e.activation(out=tmp2[:, :, :], in_=tmp2[:, :, :], func=AF.Exp),
                         reads=[tmp2], writes=[tmp2])
                p.dve.op(lambda e: e.tensor_tensor(out=tmp2[:, :, :], in0=tmp2[:, :, :], in1=tmp[:, :, :], op=ALU.mult),
                         reads=[tmp2, tmp], writes=[tmp2])
                p.dve.op(lambda e: e.tensor_reduce(out=m1[:, :], in_=tmp2[:, :, :], axis=AX.X, op=ALU.add),
                         reads=[tmp2], writes=[m1])
                p.dve.op(lambda e: e.reciprocal(out=m1[:, :], in_=m1[:, :]), reads=[m1], writes=[m1])
                p.dve.op(lambda e: e.tensor_tensor(out=comb[:, :, :], in0=tmp2[:, :, :],
                                                   in1=m1[:, :].unsqueeze(2).to_broadcast(sh), op=ALU.mult),
                         reads=[tmp2, m1], writes=[comb])
            p.barrier()

        with ExitStack() as st3:
            NB = 3
            wgb = [p.sb(st3, "wgb%d" % i, [128, 8, FS], BF16) for i in range(NB)]
            wub = [p.sb(st3, "wub%d" % i, [128, 8, FS], BF16) for i in range(NB)]
            wdb = [p.sb(st3, "wdb%d" % i, [128, FCP, D], BF16) for i in range(NB)]
            sg = [p.sb(st3, "sg%d" % i, [128, 512], BF16) for i in range(3)]
            hT = [p.sb(st3, "hT%d" % i, [128, FCP, 512], BF16) for i in range(3)]

            units = [(e_, fs) for e_ in range(E) for fs in range(NFS)]

            def load(ui):
                e_, fs = units[ui]
                b = ui % NB
                p.pool.dma(wgb[b][:, :, :], wg.t[e_].rearrange("(dc p) f -> p dc f", p=128)[:, :, fs * FS:(fs + 1) * FS],
                           reads=[wg], writes=[wgb[b]])
                p.pool.dma(wub[b][:, :, :], wu.t[e_].rearrange("(dc p) f -> p dc f", p=128)[:, :, fs * FS:(fs + 1) * FS],
                           reads=[wu], writes=[wub[b]])
                p.pool.dma(wdb[b][:, :, :], wd.t[e_].rearrange("(fc p) d -> p fc d", p=128)[:, fs * FCP:(fs + 1) * FCP, :],
                           reads=[wd], writes=[wdb[b]])

            work = [(ui, tg) for ui in range(len(units)) for tg in range(NTG)]
            gu_i = [0]

            def GU(wi):
                ui, tg = work[wi]
                b = ui % NB
                hb = hT[wi % 3]
                for fc in range(FCP):
                    k = gu_i[0]
                    gu_i[0] += 1
                    pg = psb[(k % 2) * 2]
                    pu = psb[(k % 2) * 2 + 1]
                    for dc in range(8):
                        p.pe.op(lambda e, dc=dc, fc=fc, pg=pg: e.matmul(
                            out=pg[:, :], lhsT=wgb[b][:, dc, fc * 128:(fc + 1) * 128],
                            rhs=xnT[:, dc, tg * 512:(tg + 1) * 512], start=(dc == 0), stop=(dc == 7)),
                            reads=[wgb[b], xnT], writes=[pg])
                    for dc in range(8):
                        p.pe.op(lambda e, dc=dc, fc=fc, pu=pu: e.matmul(
                            out=pu[:, :], lhsT=wub[b][:, dc, fc * 128:(fc + 1) * 128],
                            rhs=xnT[:, dc, tg * 512:(tg + 1) * 512], start=(dc == 0), stop=(dc == 7)),
                            reads=[wub[b], xnT], writes=[pu])
                    s = sg[k % 3]
                    p.act.op(lambda e, s=s, pg=pg: e.activation(out=s[:, :], in_=pg[:, :], func=AF.Silu),
                             reads=[pg], writes=[s])
                    p.dve.op(lambda e, s=s, pu=pu, fc=fc: e.tensor_tensor(out=hb[:, fc, :], in0=pu[:, :], in1=s[:, :], op=ALU.mult),
                             reads=[pu, s], writes=[hb])

            dn_i = [0]

            def DN(wi):
                ui, tg = work[wi]
                e_, fs = units[ui]
                b = ui % NB
                hb = hT[wi % 3]
                for tl in range(4):
                    t = tg * 4 + tl
                    for half in range(2):
                        k = dn_i[0]
                        dn_i[0] += 1
                        pd = psb[4 + k % 4]
                        for fc in range(FCP):
                            p.pe.op(lambda e, fc=fc, pd=pd, tl=tl, half=half: e.matmul(
                                out=pd[:, :], lhsT=hb[:, fc, tl * 128:(tl + 1) * 128],
                                rhs=wdb[b][:, fc, half * 512:(half + 1) * 512], start=(fc == 0), stop=(fc == FCP - 1)),
                                reads=[hb, wdb[b]], writes=[pd])
                        sc = comb[:, t, e_:e_ + 1] if E > 1 else 1.0
                        rd = [pd, xres[t]] + ([comb] if E > 1 else [])
                        p.dve.op(lambda e, pd=pd, t=t, half=half, sc=sc: e.scalar_tensor_tensor(
                            out=xres[t][:, half * 512:(half + 1) * 512], in0=pd[:, :], scalar=sc,
                            in1=xres[t][:, half * 512:(half + 1) * 512], op0=ALU.mult, op1=ALU.add),
                            reads=rd, writes=[xres[t]])

            nload = 0
            for _ in range(min(NB - 1, len(units))):
                load(nload)
                nload += 1
            GU(0)
            for wi in range(len(work)):
                ui, tg = work[wi]
                if tg == 0 and nload < len(units):
                    load(nload)
                    nload += 1
                if wi + 1 < len(work):
                    GU(wi + 1)
                DN(wi)

            if final_g is not None:
                p.sp.dma(gbc[:, :], final_g.t.partition_broadcast(128), reads=[final_g], writes=[gbc])
                fj = [p.sb(st3, "fj%d" % i, [128, D], BF16) for i in range(2)]
                for t in range(NT):
                    j = fj[t % 2]
                    p.act.op(lambda e, t=t, j=j: e.activation(out=j[:, :], in_=xres[t][:, :], func=AF.Square,
                                                             accum_out=ss[:, t:t + 1]),
                             reads=[xres[t]], writes=[j, ss])
                rms_rstd(p, ss, rstd, NT, D)
                for t in range(NT):
                    p.dve.op(lambda e, t=t: e.scalar_tensor_tensor(
                        out=xres[t][:, :], in0=xres[t][:, :], scalar=rstd[:, t:t + 1], in1=gbc[:, :],
                        op0=ALU.mult, op1=ALU.mult), reads=[xres[t], rstd, gbc], writes=[xres[t]])
            for t in range(NT):
                p.sp.dma(xout[t * 128:(t + 1) * 128, :], xres[t][:, :], reads=[xres[t]], writes=[xout])
            p.barrier()


def phase_norm_send(p, xin, g_norm, xnT_send, NT=16):
    with ExitStack() as st:
        gbc = p.sb(st, "ns_gbc", [128, D], F32)
        xs = [p.sb(st, "ns_x%d" % i, [128, D], F32) for i in range(3)]
        junk = [p.sb(st, "ns_j%d" % i, [128, D], BF16) for i in range(2)]
        xn = [p.sb(st, "ns_xn%d" % i, [128, D], BF16) for i in range(2)]
        xT = [p.sb(st, "ns_xT%d" % i, [128, D], BF16) for i in range(2)]
        ss = [p.sb(st, "ns_ss%d" % i, [128, 2], F32) for i in range(2)]
        psb = [p.ps(st, "ns_ps%d" % i, [128, 512], F32) for i in range(2)]
        p.sp.dma(gbc[:, :], g_norm.t.partition_broadcast(128), reads=[g_norm], writes=[gbc])
        for t in range(NT):
            x = xs[t % 3]
            s_ = ss[t % 2]
            j = junk[t % 2]
            x_n = xn[t % 2]
            xt = xT[t % 2]
            p.sp.dma(x[:, :], xin[t * 128:(t + 1) * 128, :], reads=[xin], writes=[x])
            p.act.op(lambda e, x=x, j=j, s_=s_: e.activation(out=j[:, :], in_=x[:, :], func=AF.Square, accum_out=s_[:, 0:1]),
                     reads=[x], writes=[j, s_])
            p.act.op(lambda e, s_=s_: e.activation(out=s_[:, 1:2], in_=s_[:, 0:1], func=AF.Ln, scale=1.0 / D, bias=p.eps_t[:, 0:1]),
                     reads=[s_, p.eps_t], writes=[s_])
            p.act.op(lambda e, s_=s_: e.activation(out=s_[:, 1:2], in_=s_[:, 1:2], func=AF.Exp, scale=-0.5), reads=[s_], writes=[s_])
            p.dve.op(lambda e, x=x, x_n=x_n, s_=s_: e.scalar_tensor_tensor(
                out=x_n[:, :], in0=x[:, :], scalar=s_[:, 1:2], in1=gbc[:, :], op0=ALU.mult, op1=ALU.mult),
                reads=[x, s_, gbc], writes=[x_n])
            pb = psb[t % 2]
            pv = pb.t[:, :].bitcast(BF16)
            for dc in range(8):
                p.pe.op(lambda e, dc=dc, x_n=x_n, pv=pv: e.transpose(out=pv[:, dc * 128:(dc + 1) * 128], in_=x_n[:, dc * 128:(dc + 1) * 128],
                                                                    identity=p.ident_b[:, :]), reads=[x_n, p.ident_b], writes=[pb])
            p.act.op(lambda e, xt=xt, pv=pv: e.copy(out=xt[:, :], in_=pv), reads=[pb], writes=[xt])
            p.sp.dma(xnT_send.t[t * 128:(t + 1) * 128, :], xt[:, :], reads=[xt], writes=[xnT_send])
        p.barrier()

import math
ASTEP = 99

S_ALL = 16384
NTILE = 128
NSLOT = 16
MAGIC = 12582912.0
TWO_PI_HI = 6.28125
TWO_PI_LO = 2.0 * math.pi - 6.28125
SCALE = 192.0 ** -0.5


def trig_tables(p, st, posf_ap, n, inv_bc, cos_t, sin_t, reads):
    sh = [128, n, 32]
    ang = p.sb(st, "tg_ang", sh, F32)
    a2 = p.sb(st, "tg_a2", sh, F32)
    nn = p.sb(st, "tg_n", sh, F32)
    p.dve.op(lambda e: e.tensor_tensor(out=ang[:, :, :], in0=posf_ap.unsqueeze(2).to_broadcast(sh),
                                       in1=inv_bc[:, :].unsqueeze(1).to_broadcast(sh), op=ALU.mult),
             reads=reads + [inv_bc], writes=[ang])
    for which, dst in ((0, sin_t), (1, cos_t)):
        if which == 1:
            p.dve.op(lambda e: e.tensor_scalar(out=ang[:, :, :], in0=ang[:, :, :], scalar1=math.pi / 2, scalar2=None,
                                               op0=ALU.add), reads=[ang], writes=[ang])
        p.dve.op(lambda e: e.tensor_scalar(out=nn[:, :, :], in0=ang[:, :, :], scalar1=1.0 / (2 * math.pi), scalar2=None,
                                           op0=ALU.mult), reads=[ang], writes=[nn])
        p.dve.op(lambda e: e.tensor_scalar(out=nn[:, :, :], in0=nn[:, :, :], scalar1=MAGIC, scalar2=None, op0=ALU.add),
                 reads=[nn], writes=[nn])
        p.dve.op(lambda e: e.tensor_scalar(out=nn[:, :, :], in0=nn[:, :, :], scalar1=MAGIC, scalar2=None, op0=ALU.subtract),
                 reads=[nn], writes=[nn])
        p.dve.op(lambda e: e.scalar_tensor_tensor(out=a2[:, :, :], in0=nn[:, :, :], scalar=-TWO_PI_HI, in1=ang[:, :, :],
                                                  op0=ALU.mult, op1=ALU.add), reads=[nn, ang], writes=[a2])
        p.dve.op(lambda e: e.scalar_tensor_tensor(out=a2[:, :, :], in0=nn[:, :, :], scalar=-TWO_PI_LO, in1=a2[:, :, :],
                                                  op0=ALU.mult, op1=ALU.add), reads=[nn, a2], writes=[a2])
        p.dve.op(lambda e: e.tensor_scalar(out=a2[:, :, :], in0=a2[:, :, :], scalar1=-3.14159, scalar2=3.14159,
                                           op0=ALU.max, op1=ALU.min), reads=[a2], writes=[a2])
        p.act.op(lambda e, dst=dst: e.activation(out=dst[:, :, :], in_=a2[:, :, :], func=AF.Sin),
                 reads=[a2], writes=[dst])


def phase_attn(p, x_full, x_own, posT, pos_own, inv_freq, masks, g_norm, w_in, q_norm, w_qb, kv_norm, w_kvb, w_out,
               xa_out, NTILE=NTILE, NSLOT=NSLOT, STOP=99):
    nc = p.nc
    NKEY = NTILE * 128
    with ExitStack() as st:
        KlatT = p.sb(st, "KlatT", [128, NKEY], BF16)
        KrotT = p.sb(st, "KrotT", [128, NKEY], BF16)
        Vlat = p.sb(st, "Vlat", [128, NTILE, 128], BF16)
        w_in_b = p.sb(st, "w_in_b", [128, 8, 448], BF16)
        w_qb_b = p.sb(st, "w_qb_b", [128, 2, 1536], BF16)
        w_kvb_b = p.sb(st, "w_kvb_b", [128, 2048], BF16)
        WkbT = p.sb(st, "WkbT", [128, 8, 128], BF16)
        w_out_b = p.sb(st, "w_out_b", [128, 8, 1024], BF16)
        gbc = p.sb(st, "a_gbc", [128, D], F32)
        qn_bc = p.sb(st, "qn_bc", [128, 256], F32)
        kvn_bc = p.sb(st, "kvn_bc", [128, 128], F32)
        inv_bc = p.sb(st, "inv_bc", [128, 32], F32)
        mask_b = p.sb(st, "mask_b", [128, 8, 512], BF16)
        ones_b = p.sb(st, "ones_b", [128, 128], BF16)
        cos_o = p.sb(st, "cos_o", [128, NSLOT, 32], BF16)
        sin_o = p.sb(st, "sin_o", [128, NSLOT, 32], BF16)
        ps = [p.ps(st, "aps%d" % i, [128, 512], F32) for i in range(8)]

        def psbf(i):
            return ps[i].t[:, :].bitcast(BF16)

        p.pool.dma(w_in_b[:, :, :], w_in.t.rearrange("(dc p) f -> p dc f", p=128), reads=[w_in], writes=[w_in_b])
        p.pool.dma(w_qb_b[:, :, :], w_qb.t.rearrange("(dc p) f -> p dc f", p=128), reads=[w_qb], writes=[w_qb_b])
        p.pool.dma(w_kvb_b[:, :], w_kvb.t, reads=[w_kvb], writes=[w_kvb_b])
        p.pool.dma(w_out_b[:, :, :], w_out.t.rearrange("(h p) d -> p h d", p=128), reads=[w_out], writes=[w_out_b])
        p.pool.dma(mask_b[:, :, :], masks.t.rearrange("j p n -> p j n"), reads=[masks], writes=[mask_b])
        p.sp.dma(gbc[:, :], g_norm.t.partition_broadcast(128), reads=[g_norm], writes=[gbc])
        p.sp.dma(qn_bc[:, :], q_norm.t.partition_broadcast(128), reads=[q_norm], writes=[qn_bc])
        p.sp.dma(kvn_bc[:, :], kv_norm.t.partition_broadcast(128), reads=[kv_norm], writes=[kvn_bc])
        p.sp.dma(inv_bc[:, :], inv_freq.t.partition_broadcast(128), reads=[inv_freq], writes=[inv_bc])
        p.pool.op(lambda e: e.memset(ones_b[:, :], 1.0), writes=[ones_b])
        pv = psbf(0)
        for h in range(8):
            p.pe.op(lambda e, h=h: e.transpose(out=pv[:, h * 128:(h + 1) * 128], in_=w_kvb_b[:, h * 256:h * 256 + 128],
                                               identity=p.ident_b[:, :]), reads=[w_kvb_b, p.ident_b], writes=[ps[0]])
        p.dve.op(lambda e: e.tensor_copy(out=WkbT[:, :, :], in_=pv.rearrange("p (h n) -> p h n", h=8)),
                 reads=[ps[0]], writes=[WkbT])

        if STOP <= 1:
            p.barrier()
            return
        with ExitStack() as sa:
            cos_a = p.sb(sa, "cos_a", [128, NTILE, 32], BF16)
            sin_a = p.sb(sa, "sin_a", [128, NTILE, 32], BF16)
            with ExitStack() as stt_:
                posi = p.sb(stt_, "posi", [128, NTILE], I32)
                posf = p.sb(stt_, "posf", [128, NTILE], F32)
                posoi = p.sb(stt_, "posoi", [128, NSLOT], I32)
                posof = p.sb(stt_, "posof", [128, NSLOT], F32)
                p.sp.dma(posi[:, :], posT.t, reads=[posT], writes=[posi])
                p.sp.dma(posoi[:, :], pos_own.t, reads=[pos_own], writes=[posoi])
                p.dve.op(lambda e: e.tensor_copy(out=posf[:, :], in_=posi[:, :]), reads=[posi], writes=[posf])
                p.dve.op(lambda e: e.tensor_copy(out=posof[:, :], in_=posoi[:, :]), reads=[posoi], writes=[posof])
                CH = min(32, NTILE)
                for c0 in range(0, NTILE, CH):
                    with ExitStack() as s3:
                        ct = p.sb(s3, "ct", [128, CH, 32], BF16)
                        stt = p.sb(s3, "stt", [128, CH, 32], BF16)
                        trig_tables(p, s3, posf[:, c0:c0 + CH], CH, inv_bc, ct, stt, [posf])
                        p.pool.op(lambda e, c0=c0: e.tensor_copy(out=cos_a[:, c0:c0 + CH, :], in_=ct[:, :, :]),
                                  reads=[ct], writes=[cos_a])
                        p.pool.op(lambda e, c0=c0: e.tensor_copy(out=sin_a[:, c0:c0 + CH, :], in_=stt[:, :, :]),
                                  reads=[stt], writes=[sin_a])
                        p.barrier()
                with ExitStack() as s3:
                    trig_tables(p, s3, posof[:, :], NSLOT, inv_bc, cos_o, sin_o, [posof])
                    p.barrier()

            if STOP <= 2:
                p.barrier()
                return
            xs = [p.sb(sa, "xs%d" % i, [128, D], F32) for i in range(3)]
            junk = [p.sb(sa, "ajunk%d" % i, [128, D], BF16) for i in range(2)]
            xn = [p.sb(sa, "axn%d" % i, [128, D], BF16) for i in range(2)]
            xnT = [p.sb(sa, "axnT%d" % i, [128, 8, 128], BF16) for i in range(2)]
            ssA = [p.sb(sa, "ssA%d" % i, [128, 4], F32) for i in range(3)]
            ckn = [p.sb(sa, "ckn%d" % i, [128, 128], BF16) for i in range(2)]
            krA = [p.sb(sa, "krA%d" % i, [128, 2, 32], F32) for i in range(2)]
            krB = [p.sb(sa, "krB%d" % i, [128, 2, 32], F32) for i in range(2)]
            krot = [p.sb(sa, "krot%d" % i, [128, 128], BF16) for i in range(2)]
            for kk in krot:
                p.pool.op(lambda e, kk=kk: e.memset(kk[:, :], 0.0), writes=[kk])

            def load_x(T):
                p.sp.dma(xs[T % 3][:, :], x_full[T * 128:(T + 1) * 128, :], reads=[x_full], writes=[xs[T % 3]])

            junk2 = [p.sb(sa, "ajunk2_%d" % i, [128, 128], BF16) for i in range(2)]
            load_x(0)
            load_x(1)

            def A1(T):
                if T + 2 < NTILE:
                    load_x(T + 2)
                yield
                x = xs[T % 3]
                s_ = ssA[T % 3]
                j = junk[T % 2]
                x_n = xn[T % 2]
                xT = xnT[T % 2]
                p.act.op(lambda e, x=x, j=j, s_=s_: e.activation(out=j[:, :], in_=x[:, :], func=AF.Square, accum_out=s_[:, 0:1]),
                         reads=[x], writes=[j, s_])
                yield
                p.act.op(lambda e, s_=s_: e.activation(out=s_[:, 1:2], in_=s_[:, 0:1], func=AF.Ln, scale=1.0 / D, bias=p.eps_t[:, 0:1]),
                         reads=[s_, p.eps_t], writes=[s_])
                yield
                p.act.op(lambda e, s_=s_: e.activation(out=s_[:, 1:2], in_=s_[:, 1:2], func=AF.Exp, scale=-0.5),
                         reads=[s_], writes=[s_])
                yield
                p.dve.op(lambda e, x=x, x_n=x_n, s_=s_: e.scalar_tensor_tensor(
                    out=x_n[:, :], in0=x[:, :], scalar=s_[:, 1:2], in1=gbc[:, :], op0=ALU.mult, op1=ALU.mult),
                    reads=[x, s_, gbc], writes=[x_n])
                yield
                pb = ps[T % 2]
                pvv = psbf(T % 2)
                for dc in range(8):
                    p.pe.op(lambda e, dc=dc, x_n=x_n, pvv=pvv: e.transpose(
                        out=pvv[:, dc * 128:(dc + 1) * 128], in_=x_n[:, dc * 128:(dc + 1) * 128], identity=p.ident_b[:, :]),
                        reads=[x_n, p.ident_b], writes=[pb])
                yield
                p.act.op(lambda e, xT=xT, pvv=pvv: e.copy(out=xT[:, :, :], in_=pvv.rearrange("p (c n) -> p c n", c=8)),
                         reads=[pb], writes=[xT])
                yield

            def A2(T):
                s_ = ssA[T % 3]
                j = junk2[T % 2]
                xT = xnT[T % 2]
                pk = ps[2 + T % 2]
                for dc in range(8):
                    p.pe.op(lambda e, dc=dc, xT=xT, pk=pk: e.matmul(out=pk[:, 0:192], lhsT=xT[:, dc, :], rhs=w_in_b[:, dc, 256:448],
                                                                   start=(dc == 0), stop=(dc == 7)),
                            reads=[xT, w_in_b], writes=[pk])
                yield
                p.act.op(lambda e, pk=pk, j=j, s_=s_: e.activation(out=j[:, 0:128], in_=pk[:, 0:128], func=AF.Square,
                                                                  accum_out=s_[:, 2:3]), reads=[pk], writes=[j, s_])
                yield
                p.act.op(lambda e, s_=s_: e.activation(out=s_[:, 3:4], in_=s_[:, 2:3], func=AF.Ln, scale=1.0 / 128, bias=p.eps_t[:, 0:1]),
                         reads=[s_, p.eps_t], writes=[s_])
                yield
                p.act.op(lambda e, s_=s_: e.activation(out=s_[:, 3:4], in_=s_[:, 3:4], func=AF.Exp, scale=-0.5),
                         reads=[s_], writes=[s_])
                yield
                ck = ckn[T % 2]
                p.dve.op(lambda e, pk=pk, ck=ck, s_=s_: e.scalar_tensor_tensor(
                    out=ck[:, :], in0=pk[:, 0:128], scalar=s_[:, 3:4], in1=kvn_bc[:, :], op0=ALU.mult, op1=ALU.mult),
                    reads=[pk, s_, kvn_bc], writes=[ck])
                yield
                p.pool.op(lambda e, T=T, ck=ck: e.tensor_copy(out=Vlat[:, T, :], in_=ck[:, :]), reads=[ck], writes=[Vlat])
                yield
                A = krA[T % 2]
                B = krB[T % 2]
                kr = krot[T % 2]
                kv3 = pk[:, 128:192].rearrange("p (a f) -> p a f", a=2)
                sh = [128, 2, 32]
                p.dve.op(lambda e, A=A, kv3=kv3, T=T: e.tensor_tensor(
                    out=A[:, :, :], in0=kv3, in1=cos_a[:, T, :].unsqueeze(1).to_broadcast(sh), op=ALU.mult),
                    reads=[pk, cos_a], writes=[A])
                yield
                p.dve.op(lambda e, B=B, kv3=kv3, T=T: e.tensor_tensor(
                    out=B[:, :, :], in0=kv3, in1=sin_a[:, T, :].unsqueeze(1).to_broadcast(sh), op=ALU.mult),
                    reads=[pk, sin_a], writes=[B])
                yield
                p.pool.op(lambda e, A=A, B=B, kr=kr: e.tensor_tensor(out=kr[:, 0:32], in0=A[:, 0, :], in1=B[:, 1, :], op=ALU.subtract),
                          reads=[A, B], writes=[kr])
                yield
                p.pool.op(lambda e, A=A, B=B, kr=kr: e.tensor_tensor(out=kr[:, 32:64], in0=B[:, 0, :], in1=A[:, 1, :], op=ALU.add),
                          reads=[A, B], writes=[kr])
                yield

            def A3(T):
                ck = ckn[T % 2]
                kr = krot[T % 2]
                pt_ = ps[4 + T % 2]
                ptv = psbf(4 + T % 2)
                p.pe.op(lambda e, ck=ck, ptv=ptv: e.transpose(out=ptv[:, 0:128], in_=ck[:, :], identity=p.ident_b[:, :]),
                        reads=[ck, p.ident_b], writes=[pt_])
                yield
                p.pe.op(lambda e, kr=kr, ptv=ptv: e.transpose(out=ptv[:, 128:256], in_=kr[:, :], identity=p.ident_b[:, :]),
                        reads=[kr, p.ident_b], writes=[pt_])
                yield
                p.act.op(lambda e, T=T, ptv=ptv: e.copy(out=KlatT[:, T * 128:(T + 1) * 128], in_=ptv[:, 0:128]),
                         reads=[pt_], writes=[KlatT])
                yield
                p.dve.op(lambda e, T=T, ptv=ptv: e.tensor_copy(out=KrotT[:, T * 128:(T + 1) * 128], in_=ptv[:, 128:256]),
                         reads=[pt_], writes=[KrotT])
                yield

            for it in range(NTILE + 2):
                gens = []
                if 0 <= it - 2 < NTILE:
                    gens.append(A3(it - 2))
                if 0 <= it - 1 < NTILE:
                    gens.append(A2(it - 1))
                if it < NTILE:
                    gens.append(A1(it))
                while gens:
                    for g in list(gens):
                        try:
                            next(g)
                        except StopIteration:
                            gens.remove(g)
            p.barrier()

        if STOP <= 3:
            p.barrier()
            return
        with ExitStack() as sb_:
            xo = [p.sb(sb_, "xo%d" % i, [128, D], F32) for i in range(2)]
            junk = p.sb(sb_, "bjunk", [128, D], BF16)
            xn = p.sb(sb_, "bxn", [128, D], BF16)
            xnT = p.sb(sb_, "bxnT", [128, 8, 128], BF16)
            ssB = p.sb(sb_, "ssB", [128, 4], F32)
            cqn = p.sb(sb_, "cqn", [128, 256], BF16)
            cqnT = p.sb(sb_, "cqnT", [128, 2, 128], BF16)
            qnT = p.sb(sb_, "qnT", [128, 8, 128], BF16)
            Q1T_l = [p.sb(sb_, "Q1T%d" % i, [128, 1024], BF16) for i in range(2)]
            Q2T_l = [p.sb(sb_, "Q2T%d" % i, [128, 1024], BF16) for i in range(2)]
            dacc = p.sb(sb_, "dacc", [128, 512], F32)
            ones_f32 = p.sb(sb_, "ones_f32", [128, 128], F32)
            p.pool.op(lambda e: e.memset(ones_f32[:, :], 1.0), writes=[ones_f32])
            qA = p.sb(sb_, "qA", [128, 8, 2, 32], F32)
            qB = p.sb(sb_, "qB", [128, 8, 2, 32], F32)
            qrot = p.sb(sb_, "qrot", [128, 8, 128], BF16)
            p.pool.op(lambda e: e.memset(qrot[:, :, :], 0.0), writes=[qrot])
            PT = [p.sb(sb_, "PT%d" % i, [128, 512], BF16) for i in range(4)]
            rden = p.sb(sb_, "rden", [128, 512], F32)
            OnT = p.sb(sb_, "OnT", [128, 1024], BF16)
            ohT = p.sb(sb_, "ohT", [128, 8, 128], BF16)
            xout_t = [p.sb(sb_, "xout_t%d" % i, [128, D], F32) for i in range(2)]

            p.sp.dma(xo[0][:, :], x_own[0:128, :], reads=[x_own], writes=[xo[0]])
            def QPREP(i):
                Q1T = Q1T_l[i % 2]
                Q2T = Q2T_l[i % 2]
                x = xo[i % 2]
                p.act.op(lambda e, x=x: e.activation(out=junk[:, :], in_=x[:, :], func=AF.Square, accum_out=ssB[:, 0:1]),
                         reads=[x], writes=[junk, ssB])
                yield
                p.act.op(lambda e: e.activation(out=ssB[:, 1:2], in_=ssB[:, 0:1], func=AF.Ln, scale=1.0 / D, bias=p.eps_t[:, 0:1]),
                         reads=[ssB, p.eps_t], writes=[ssB])
                yield
                p.act.op(lambda e: e.activation(out=ssB[:, 1:2], in_=ssB[:, 1:2], func=AF.Exp, scale=-0.5), reads=[ssB], writes=[ssB])
                yield
                p.dve.op(lambda e, x=x: e.scalar_tensor_tensor(out=xn[:, :], in0=x[:, :], scalar=ssB[:, 1:2], in1=gbc[:, :],
                                                               op0=ALU.mult, op1=ALU.mult), reads=[x, ssB, gbc], writes=[xn])
                yield
                pvv = psbf(7)
                for dc in range(8):
                    p.pe.op(lambda e, dc=dc: e.transpose(out=pvv[:, dc * 128:(dc + 1) * 128], in_=xn[:, dc * 128:(dc + 1) * 128],
                                                         identity=p.ident_b[:, :]), reads=[xn, p.ident_b], writes=[ps[7]])
                yield
                p.act.op(lambda e: e.copy(out=xnT[:, :, :], in_=pvv.rearrange("p (c n) -> p c n", c=8)), reads=[ps[7]], writes=[xnT])
                yield
                for dc in range(8):
                    p.pe.op(lambda e, dc=dc: e.matmul(out=ps[6][:, 0:256], lhsT=xnT[:, dc, :], rhs=w_in_b[:, dc, 0:256],
                                                      start=(dc == 0), stop=(dc == 7)), reads=[xnT, w_in_b], writes=[ps[6]])
                yield
                p.act.op(lambda e: e.activation(out=junk[:, 0:256], in_=ps[6][:, 0:256], func=AF.Square, accum_out=ssB[:, 2:3]),
                         reads=[ps[6]], writes=[junk, ssB])
                yield
                p.act.op(lambda e: e.activation(out=ssB[:, 3:4], in_=ssB[:, 2:3], func=AF.Ln, scale=1.0 / 256, bias=p.eps_t[:, 0:1]),
                         reads=[ssB, p.eps_t], writes=[ssB])
                yield
                p.act.op(lambda e: e.activation(out=ssB[:, 3:4], in_=ssB[:, 3:4], func=AF.Exp, scale=-0.5), reads=[ssB], writes=[ssB])
                yield
                p.dve.op(lambda e: e.scalar_tensor_tensor(out=cqn[:, :], in0=ps[6][:, 0:256], scalar=ssB[:, 3:4], in1=qn_bc[:, :],
                                                          op0=ALU.mult, op1=ALU.mult), reads=[ps[6], ssB, qn_bc], writes=[cqn])
                yield
                for kc in range(2):
                    p.pe.op(lambda e, kc=kc: e.transpose(out=pvv[:, kc * 128:(kc + 1) * 128], in_=cqn[:, kc * 128:(kc + 1) * 128],
                                                         identity=p.ident_b[:, :]), reads=[cqn, p.ident_b], writes=[ps[7]])
                yield
                p.act.op(lambda e: e.copy(out=cqnT[:, :, :], in_=pvv[:, 0:256].rearrange("p (c n) -> p c n", c=2)),
                         reads=[ps[7]], writes=[cqnT])
                yield
                for h in range(8):
                    pb = ps[6 + h // 4]
                    for kc in range(2):
                        p.pe.op(lambda e, h=h, kc=kc, pb=pb: e.matmul(
                            out=pb[:, (h % 4) * 128:(h % 4 + 1) * 128], lhsT=w_qb_b[:, kc, h * 192:h * 192 + 128],
                            rhs=cqnT[:, kc, :], start=(kc == 0), stop=(kc == 1)), reads=[w_qb_b, cqnT], writes=[pb])
                yield
                p.act.op(lambda e: e.copy(out=qnT[:, 0:4, :], in_=ps[6][:, :].rearrange("p (h n) -> p h n", h=4)),
                         reads=[ps[6]], writes=[qnT])
                yield
                p.dve.op(lambda e: e.tensor_copy(out=qnT[:, 4:8, :], in_=ps[7][:, :].rearrange("p (h n) -> p h n", h=4)),
                         reads=[ps[7]], writes=[qnT])
                yield
                for h in range(8):
                    pb = ps[6 + h // 4]
                    p.pe.op(lambda e, h=h, pb=pb: e.matmul(out=pb[:, (h % 4) * 128:(h % 4 + 1) * 128], lhsT=WkbT[:, h, :],
                                                           rhs=qnT[:, h, :], start=True, stop=True), reads=[WkbT, qnT], writes=[pb])
                yield
                p.act.op(lambda e: e.copy(out=Q1T[:, 0:512], in_=ps[6][:, :]), reads=[ps[6]], writes=[Q1T])
                yield
                p.dve.op(lambda e: e.tensor_copy(out=Q1T[:, 512:1024], in_=ps[7][:, :]), reads=[ps[7]], writes=[Q1T])
                yield
                wr = w_qb_b[:, :, :].rearrange("p c (h f) -> p c h f", h=8)
                for kc in range(2):
                    p.pe.op(lambda e, kc=kc: e.matmul(out=ps[6][:, :].rearrange("p (h f) -> p h f", h=8), lhsT=cqnT[:, kc, :],
                                                      rhs=wr[:, kc, :, 128:192], start=(kc == 0), stop=(kc == 1)),
                            reads=[cqnT, w_qb_b], writes=[ps[6]])
                yield
                q4 = ps[6][:, :].rearrange("p (h a f) -> p h a f", h=8, a=2)
                sh4 = [128, 8, 2, 32]
                cb = cos_o[:, i, :].unsqueeze(1).unsqueeze(1).to_broadcast(sh4)
                sbb = sin_o[:, i, :].unsqueeze(1).unsqueeze(1).to_broadcast(sh4)
                p.dve.op(lambda e, cb=cb: e.tensor_tensor(out=qA[:, :, :, :], in0=q4, in1=cb, op=ALU.mult),
                         reads=[ps[6], cos_o], writes=[qA])
                yield
                p.dve.op(lambda e, sbb=sbb: e.tensor_tensor(out=qB[:, :, :, :], in0=q4, in1=sbb, op=ALU.mult),
                         reads=[ps[6], sin_o], writes=[qB])
                yield
                p.pool.op(lambda e: e.tensor_tensor(out=qrot[:, :, 0:32], in0=qA[:, :, 0, :], in1=qB[:, :, 1, :], op=ALU.subtract),
                          reads=[qA, qB], writes=[qrot])
                yield
                p.pool.op(lambda e: e.tensor_tensor(out=qrot[:, :, 32:64], in0=qB[:, :, 0, :], in1=qA[:, :, 1, :], op=ALU.add),
                          reads=[qA, qB], writes=[qrot])
                yield
                for h in range(8):
                    p.pe.op(lambda e, h=h: e.transpose(out=pvv[:, h * 128:(h + 1) * 128], in_=qrot[:, h, :],
                                                       identity=p.ident_b[:, :]), reads=[qrot, p.ident_b], writes=[ps[7]])
                yield
                p.act.op(lambda e: e.copy(out=Q2T[:, :], in_=pvv[:, :]), reads=[ps[7]], writes=[Q2T])
                yield


            pt_i = 0
            for _ in QPREP(0):
                pass
            for i in range(NSLOT):
                if i + 1 < NSLOT:
                    p.sp.dma(xo[(i + 1) % 2][:, :], x_own[(i + 1) * 128:(i + 2) * 128, :], reads=[x_own], writes=[xo[(i + 1) % 2]])
                x = xo[i % 2]
                Q1T = Q1T_l[i % 2]
                Q2T = Q2T_l[i % 2]
                qgen = QPREP(i + 1) if i + 1 < NSLOT else iter(())
                nkb = 8 * i + 8
                for g in range(2):
                    po = ps[3 + g]
                    pd = ps[5]
                    qs = slice(g * 512, (g + 1) * 512)

                    def S(kb, g=g, qs=qs):
                        pS = ps[kb % 3]
                        masked = kb >= 8 * i
                        p.pe.op(lambda e: e.matmul(out=pS[:, :], lhsT=KlatT[:, kb * 128:(kb + 1) * 128], rhs=Q1T[:, qs],
                                                   start=True, stop=False), reads=[KlatT, Q1T], writes=[pS])
                        p.pe.op(lambda e: e.matmul(out=pS[:, :], lhsT=KrotT[:, kb * 128:(kb + 1) * 128], rhs=Q2T[:, qs],
                                                   start=False, stop=not masked), reads=[KrotT, Q2T], writes=[pS])
                        if masked:
                            p.pe.op(lambda e: e.matmul(out=pS[:, :], lhsT=p.ident_b[:, :], rhs=mask_b[:, kb - 8 * i, :],
                                                       start=False, stop=True), reads=[p.ident_b, mask_b], writes=[pS])

                    S(0)
                    if nkb > 1:
                        S(1)
                    for kb in range(nkb):
                        if kb + 2 < nkb:
                            S(kb + 2)
                        pS = ps[kb % 3]
                        Pt = PT[pt_i % 4]
                        pt_i += 1
                        p.act.op(lambda e, pS=pS, Pt=Pt: e.activation(out=Pt[:, :], in_=pS[:, :], func=AF.Exp, scale=SCALE),
                                 reads=[pS], writes=[Pt])
                        next(qgen, None)
                        p.pe.op(lambda e, kb=kb, Pt=Pt, po=po: e.matmul(out=po[:, :], lhsT=Vlat[:, kb, :], rhs=Pt[:, :],
                                                                        start=(kb == 0), stop=(kb == nkb - 1)),
                                reads=[Vlat, Pt], writes=[po])
                        if kb == 0:
                            p.dve.op(lambda e, Pt=Pt: e.tensor_copy(out=dacc[:, :], in_=Pt[:, :]), reads=[Pt], writes=[dacc])
                        else:
                            p.dve.op(lambda e, Pt=Pt: e.tensor_tensor(out=dacc[:, :], in0=dacc[:, :], in1=Pt[:, :], op=ALU.add),
                                     reads=[dacc, Pt], writes=[dacc])
                    p.pe.op(lambda e, pd=pd: e.matmul(out=pd[:, :], lhsT=ones_f32[:, :], rhs=dacc[:, :], start=True, stop=True),
                            reads=[ones_f32, dacc], writes=[pd])
                    p.dve.op(lambda e, pd=pd: e.reciprocal(out=rden[:, :], in_=pd[:, :]), reads=[pd], writes=[rden])
                    p.dve.op(lambda e, po=po, qs=qs: e.tensor_tensor(out=OnT[:, qs], in0=po[:, :], in1=rden[:, :], op=ALU.mult),
                             reads=[po, rden], writes=[OnT])
                for _ in qgen:
                    pass
                for h in range(8):
                    pb = ps[h // 4]
                    p.pe.op(lambda e, h=h, pb=pb: e.matmul(out=pb[:, (h % 4) * 128:(h % 4 + 1) * 128],
                                                           lhsT=w_kvb_b[:, h * 256 + 128:(h + 1) * 256],
                                                           rhs=OnT[:, h * 128:(h + 1) * 128], start=True, stop=True),
                            reads=[w_kvb_b, OnT], writes=[pb])
                p.act.op(lambda e: e.copy(out=ohT[:, 0:4, :], in_=ps[0][:, :].rearrange("p (h n) -> p h n", h=4)),
                         reads=[ps[0]], writes=[ohT])
                p.dve.op(lambda e: e.tensor_copy(out=ohT[:, 4:8, :], in_=ps[1][:, :].rearrange("p (h n) -> p h n", h=4)),
                         reads=[ps[1]], writes=[ohT])
                xt = xout_t[i % 2]
                for half in range(2):
                    pb = ps[6 + half]
                    for h in range(8):
                        p.pe.op(lambda e, h=h, half=half, pb=pb: e.matmul(out=pb[:, :], lhsT=ohT[:, h, :],
                                                                          rhs=w_out_b[:, h, half * 512:(half + 1) * 512],
                                                                          start=(h == 0), stop=(h == 7)),
                                reads=[ohT, w_out_b], writes=[pb])
                    p.dve.op(lambda e, half=half, pb=pb, xt=xt, x=x: e.tensor_tensor(
                        out=xt[:, half * 512:(half + 1) * 512], in0=pb[:, :], in1=x[:, half * 512:(half + 1) * 512], op=ALU.add),
                        reads=[pb, x], writes=[xt])
                p.sp.dma(xa_out[i * 128:(i + 1) * 128, :], xt[:, :], reads=[xt], writes=[xa_out])
            p.barrier()


NC_TRI, NC_BLK, NC_NEG, NC_MA2, NC_MB2, NC_M3 = 0, 128, 256, 384, 512, 640
NCONST = 643


def mlstm_consts_np():
    s = np.arange(128)[:, None]
    t = np.arange(128)[None, :]
    same = (s // 64) == (t // 64)
    c = np.zeros((128, NCONST), np.float32)
    c[:, NC_TRI:NC_TRI + 128] = (same & (s <= t))
    c[:, NC_BLK:NC_BLK + 128] = same
    c[:, NC_NEG:NC_NEG + 128] = np.where(same & (s <= t), 0.0, -30000.0)
    c[:, NC_MA2:NC_MA2 + 128] = (s < 64) & (t < 64)
    c[:, NC_MB2:NC_MB2 + 128] = (s >= 64) & (t >= 64)
    c[:, NC_M3 + 0] = 1.0
    c[:, NC_M3 + 1] = (np.arange(128) < 64)
    c[:, NC_M3 + 2] = (np.arange(128) >= 64)
    return c


def phase_mlstm(p, x_full, g_norm, w_h, cw, cb, gate_b, head_norm, consts, y_out, NTILE=128, xnT_all=None, yT_send=None):
    nc = p.nc
    with ExitStack() as st:
        w_b = p.sb(st, "m_w", [128, 8, 514], BF16)
        gbc = p.sb(st, "m_gbc", [128, D], F32)
        cst = p.sb(st, "m_cst", [128, NCONST], F32)
        cw_t = p.sb(st, "m_cw", [128, 8], F32)
        cb_t = p.sb(st, "m_cb", [128, 2], F32)
        gb_bc = p.sb(st, "m_gb", [128, 2], F32)
        hn_bc = p.sb(st, "m_hn", [128, 128], F32)
        ones_f = p.sb(st, "m_ones", [128, 128], F32)
        one_c = p.sb(st, "m_one_c", [128, 1], F32)
        ps = [p.ps(st, "mps%d" % i, [128, 512], F32) for i in range(8)]

        p.pool.dma(w_b[:, :, :], w_h.t.rearrange("(dc p) f -> p dc f", p=128), reads=[w_h], writes=[w_b])
        p.sp.dma(gbc[:, :], g_norm.t.partition_broadcast(128), reads=[g_norm], writes=[gbc])
        p.sp.dma(cst[:, :], consts.t, reads=[consts], writes=[cst])
        p.sp.dma(cw_t[:, :], cw.t, reads=[cw], writes=[cw_t])
        p.sp.dma(cb_t[:, :], cb.t, reads=[cb], writes=[cb_t])
        p.sp.dma(gb_bc[:, :], gate_b.t.partition_broadcast(128), reads=[gate_b], writes=[gb_bc])
        p.sp.dma(hn_bc[:, :], head_norm.t.partition_broadcast(128), reads=[head_norm], writes=[hn_bc])
        p.pool.op(lambda e: e.memset(ones_f[:, :], 1.0), writes=[ones_f])
        p.pool.op(lambda e: e.memset(one_c[:, :], 1.0), writes=[one_c])

        Tri = cst[:, NC_TRI:NC_TRI + 128]
        Blk = cst[:, NC_BLK:NC_BLK + 128]
        Neg = cst[:, NC_NEG:NC_NEG + 128]
        MA2 = cst[:, NC_MA2:NC_MA2 + 128]
        MB2 = cst[:, NC_MB2:NC_MB2 + 128]
        M3 = cst[:, NC_M3:NC_M3 + 3]

        xs = [p.sb(st, "m_xs%d" % i, [128, D], F32) for i in range(3)]
        junk = [p.sb(st, "m_junk%d" % i, [128, D], BF16) for i in range(2)]
        xn = [p.sb(st, "m_xn%d" % i, [128, D], BF16) for i in range(2)]
        xnT = [p.sb(st, "m_xnT%d" % i, [128, 8, 128], BF16) for i in range(2)]
        ss = [p.sb(st, "m_ss%d" % i, [128, 2], F32) for i in range(2)]
        raw = [p.sb(st, "m_raw%d" % i, [128, 2, 131], F32) for i in range(4)]
        acc = p.sb(st, "m_acc", [128, 2, 128], F32)
        sg = p.sb(st, "m_sg", [128, 2, 128], F32)
        qT_l = [p.sb(st, "m_qT%d" % i, [128, 128], F32) for i in range(2)]
        kT_l = [p.sb(st, "m_kT%d" % i, [128, 128], F32) for i in range(2)]
        qbA_l = [p.sb(st, "m_qbA%d" % i, [128, 128], F32) for i in range(2)]
        qbB_l = [p.sb(st, "m_qbB%d" % i, [128, 128], F32) for i in range(2)]
        kwA_l = [p.sb(st, "m_kwA%d" % i, [128, 128], F32) for i in range(2)]
        kwB_l = [p.sb(st, "m_kwB%d" % i, [128, 128], F32) for i in range(2)]
        v1_l = [p.sb(st, "m_v1%d" % i, [128, 129], F32) for i in range(4)]
        so_l = [p.sb(st, "m_so%d" % i, [128, 128], F32) for i in range(5)]
        gt_l = [p.sb(st, "m_gt%d" % i, [128, 2], F32) for i in range(2)]
        g2 = p.sb(st, "m_g2", [128, 2], F32)
        lf3_l = [p.sb(st, "m_lf3%d" % i, [128, 3], F32) for i in range(2)]
        lfc = p.sb(st, "m_lfc", [128, 1], F32)
        g4 = p.sb(st, "m_g4", [128, 4], F32)
        F2_l = [p.sb(st, "m_F2%d" % i, [128, 2], F32) for i in range(3)]
        acol = p.sb(st, "m_acol", [128, 1], F32)
        wcol = p.sb(st, "m_wcol", [128, 1], F32)
        wAB_l = [p.sb(st, "m_wAB%d" % i, [128, 2], F32) for i in range(2)]
        eb_l = [p.sb(st, "m_eb%d" % i, [128, 1], F32) for i in range(3)]
        trilf_l = [p.sb(st, "m_trilf%d" % i, [128, 128], F32) for i in range(2)]
        DT_l = [p.sb(st, "m_DT%d" % i, [128, 128], F32) for i in range(3)]
        wts = p.sb(st, "m_wts", [128, 128], F32)
        intra = p.sb(st, "m_intra", [128, 129], F32)
        tot_l = [p.sb(st, "m_tot%d" % i, [128, 129], F32) for i in range(2)]
        jB = p.sb(st, "m_jB", [128, 128], BF16)
        sm = p.sb(st, "m_sm", [128, 6], F32)
        yt = [p.sb(st, "m_yt%d" % i, [128, 128], F32) for i in range(2)]
        SA = p.sb(st, "m_SA", [128, 129], F32)
        SB = p.sb(st, "m_SB", [128, 129], F32)

        p.pool.op(lambda e: e.memset(SA[:, :], 0.0), writes=[SA])
        p.pool.op(lambda e: e.memset(raw[0][:, :, :], 0.0), writes=[raw[0]])
        for v1 in v1_l:
            p.pool.op(lambda e, v1=v1: e.memset(v1[:, 128:129], 1.0), writes=[v1])

        xT3 = [p.sb(st, "m_xT3_%d" % i, [128, 8, 128], BF16) for i in range(3)] if xnT_all is not None else None
        yTb = [p.sb(st, "m_yTb%d" % i, [128, 128], BF16) for i in range(2)] if yT_send is not None else None

        def load_x(T):
            if xnT_all is not None:
                rb = ((T % 8) * 16 + T // 8) * 128
                p.sp.dma(xT3[T % 3][:, :, :], xnT_all.t[rb:rb + 128, :].rearrange("p (c n) -> p c n", c=8),
                         reads=[xnT_all], writes=[xT3[T % 3]])
            else:
                p.sp.dma(xs[T % 3][:, :], x_full[T * 128:(T + 1) * 128, :], reads=[x_full], writes=[xs[T % 3]])

        load_x(0)
        if NTILE > 1:
            load_x(1)

        def F1(T):
            if T + 2 < NTILE:
                load_x(T + 2)
            yield
            v1 = v1_l[T % 4]; so = so_l[T % 5]; gt = gt_l[T % 2]; lf3 = lf3_l[T % 2]; trilf = trilf_l[T % 2]
            x = xs[T % 3]
            s_ = ss[T % 2]
            j = junk[T % 2]
            x_n = xn[T % 2]
            xT = xnT[T % 2]
            rw = raw[T % 4]
            rwn = raw[(T + 1) % 4]
            if xnT_all is not None:
                xT = xT3[T % 3]
            else:
                p.act.op(lambda e, x=x, j=j, s_=s_: e.activation(out=j[:, :], in_=x[:, :], func=AF.Square, accum_out=s_[:, 0:1]),
                         reads=[x], writes=[j, s_])
                p.act.op(lambda e, s_=s_: e.activation(out=s_[:, 1:2], in_=s_[:, 0:1], func=AF.Ln, scale=1.0 / D, bias=p.eps_t[:, 0:1]),
                         reads=[s_, p.eps_t], writes=[s_])
                p.act.op(lambda e, s_=s_: e.activation(out=s_[:, 1:2], in_=s_[:, 1:2], func=AF.Exp, scale=-0.5), reads=[s_], writes=[s_])
                p.dve.op(lambda e, x=x, x_n=x_n, s_=s_: e.scalar_tensor_tensor(
                    out=x_n[:, :], in0=x[:, :], scalar=s_[:, 1:2], in1=gbc[:, :], op0=ALU.mult, op1=ALU.mult),
                    reads=[x, s_, gbc], writes=[x_n])
                pvv = ps[0].t[:, :].bitcast(BF16)
                for dc in range(8):
                    p.pe.op(lambda e, dc=dc, x_n=x_n: e.transpose(out=pvv[:, dc * 128:(dc + 1) * 128], in_=x_n[:, dc * 128:(dc + 1) * 128],
                                                                 identity=p.ident_b[:, :]), reads=[x_n, p.ident_b], writes=[ps[0]])
                p.act.op(lambda e, xT=xT: e.copy(out=xT[:, :, :], in_=pvv.rearrange("p (c n) -> p c n", c=8)), reads=[ps[0]], writes=[xT])
            yield
            for qk in range(2):
                for dc in range(8):
                    p.pe.op(lambda e, dc=dc, qk=qk, xT=xT: e.matmul(out=ps[1][:, qk * 128:(qk + 1) * 128],
                                                                    lhsT=w_b[:, dc, qk * 128:(qk + 1) * 128], rhs=xT[:, dc, :],
                                                                    start=(dc == 0), stop=(dc == 7)), reads=[w_b, xT], writes=[ps[1]])
            yield
            for dc in range(8):
                p.pe.op(lambda e, dc=dc, xT=xT: e.matmul(out=ps[2][:, 0:258], lhsT=xT[:, dc, :], rhs=w_b[:, dc, 256:514],
                                                         start=(dc == 0), stop=(dc == 7)), reads=[xT, w_b], writes=[ps[2]])
            yield
            p.dve.op(lambda e: e.tensor_tensor(out=gt[:, :], in0=ps[2][:, 256:258], in1=gb_bc[:, :], op=ALU.add),
                     reads=[ps[2], gb_bc], writes=[gt])
            yield
            p.act.op(lambda e: e.activation(out=g2[:, :], in_=gt[:, :], func=AF.Exp, scale=2.0 / 15.0), reads=[gt], writes=[g2])
            yield
            p.dve.op(lambda e: e.tensor_scalar(out=g2[:, :], in0=g2[:, :], scalar1=1.0, scalar2=None, op0=ALU.add), reads=[g2], writes=[g2])
            yield
            p.dve.op(lambda e: e.reciprocal(out=g2[:, :], in_=g2[:, :]), reads=[g2], writes=[g2])
            yield
            p.dve.op(lambda e: e.tensor_scalar(out=gt[:, :], in0=g2[:, :], scalar1=-30.0, scalar2=15.0, op0=ALU.mult, op1=ALU.add),
                     reads=[g2], writes=[gt])
            yield
            p.act.op(lambda e: e.activation(out=lfc[:, :], in_=gt[:, 1:2], func=AF.Exp, scale=-1.0), reads=[gt], writes=[lfc])
            yield
            p.act.op(lambda e: e.activation(out=lfc[:, :], in_=lfc[:, :], func=AF.Ln, bias=one_c[:, 0:1]), reads=[lfc, one_c], writes=[lfc])
            yield
            p.dve.op(lambda e: e.tensor_scalar(out=lf3[:, :], in0=M3, scalar1=lfc[:, 0:1], scalar2=-1.0, op0=ALU.mult, op1=ALU.mult),
                     reads=[cst, lfc], writes=[lf3])
            yield
            p.dve.op(lambda e: e.tensor_scalar(out=trilf[:, :], in0=Tri, scalar1=lf3[:, 0:1], scalar2=None, op0=ALU.mult),
                     reads=[cst, lf3], writes=[trilf])
            yield
            p.act.op(lambda e, rw=rw: e.copy(out=rw[:, :, 3:131], in_=ps[1][:, 0:256].rearrange("p (a n) -> p a n", a=2)),
                     reads=[ps[1]], writes=[rw])
            yield
            p.pool.op(lambda e, rw=rw, rwn=rwn: e.tensor_copy(out=rwn[:, :, 0:3], in_=rw[:, :, 128:131]), reads=[rw], writes=[rwn])
            yield
            p.act.op(lambda e: e.copy(out=v1[:, 0:128], in_=ps[2][:, 0:128]), reads=[ps[2]], writes=[v1])
            yield
            p.act.op(lambda e: e.activation(out=so[:, :], in_=ps[2][:, 128:256], func=AF.Exp, scale=-1.0), reads=[ps[2]], writes=[so])
            yield
            p.pool.op(lambda e: e.tensor_scalar(out=so[:, :], in0=so[:, :], scalar1=1.0, scalar2=None, op0=ALU.add), reads=[so], writes=[so])
            yield
            p.dve.op(lambda e: e.reciprocal(out=so[:, :], in_=so[:, :]), reads=[so], writes=[so])
            yield


        def F2S(T):
            gt = gt_l[T % 2]; lf3 = lf3_l[T % 2]; trilf = trilf_l[T % 2]; F2 = F2_l[T % 3]; eb = eb_l[T % 3]; DT = DT_l[T % 3]; wAB = wAB_l[T % 2]
            p.pe.op(lambda e: e.matmul(out=ps[3][:, 0:1], lhsT=Tri, rhs=lf3[:, 0:1], start=True, stop=True), reads=[cst, lf3], writes=[ps[3]])
            yield
            p.pe.op(lambda e: e.matmul(out=ps[3][:, 1:2], lhsT=Blk, rhs=lf3[:, 0:1], start=True, stop=True), reads=[cst, lf3], writes=[ps[3]])
            yield
            p.pe.op(lambda e: e.matmul(out=ps[3][:, 2:4], lhsT=ones_f[:, :], rhs=lf3[:, 1:3], start=True, stop=True),
                    reads=[ones_f, lf3], writes=[ps[3]])
            yield
            p.act.op(lambda e: e.copy(out=g4[:, :], in_=ps[3][:, 0:4]), reads=[ps[3]], writes=[g4])
            yield
            p.act.op(lambda e: e.activation(out=F2[:, :], in_=g4[:, 2:4], func=AF.Exp), reads=[g4], writes=[F2])
            yield
            p.dve.op(lambda e: e.tensor_tensor(out=acol[:, :], in0=gt[:, 0:1], in1=g4[:, 0:1], op=ALU.subtract), reads=[gt, g4], writes=[acol])
            yield
            p.act.op(lambda e: e.activation(out=wcol[:, :], in_=g4[:, 1:2], func=AF.Exp, bias=acol[:, 0:1]), reads=[g4, acol], writes=[wcol])
            yield
            p.act.op(lambda e: e.activation(out=eb[:, :], in_=g4[:, 0:1], func=AF.Exp), reads=[g4], writes=[eb])
            yield
            p.dve.op(lambda e: e.tensor_scalar(out=wAB[:, :], in0=M3[:, 1:3], scalar1=wcol[:, 0:1], scalar2=None, op0=ALU.mult),
                     reads=[cst, wcol], writes=[wAB])
            yield
            p.pe.op(lambda e: e.matmul(out=ps[4][:, 0:128], lhsT=ones_f[:, :], rhs=trilf[:, :], start=True, stop=False),
                    reads=[ones_f, trilf], writes=[ps[4]])
            yield
            p.pe.op(lambda e: e.matmul(out=ps[4][:, 0:128], lhsT=p.ident_f[:, :], rhs=Neg, start=False, stop=True),
                    reads=[p.ident_f, cst], writes=[ps[4]])
            yield
            p.act.op(lambda e: e.activation(out=DT[:, :], in_=ps[4][:, 0:128], func=AF.Exp, bias=acol[:, 0:1]),
                     reads=[ps[4], acol], writes=[DT])
            yield

        def F3(T):
            rw = raw[T % 4]; wAB = wAB_l[T % 2]
            qT = qT_l[T % 2]; kT = kT_l[T % 2]; qbA = qbA_l[T % 2]; qbB = qbB_l[T % 2]; kwA = kwA_l[T % 2]; kwB = kwB_l[T % 2]
            for qk in range(2):
                p.dve.op(lambda e, qk=qk, rw=rw: e.tensor_scalar(out=acc[:, qk, :], in0=rw[:, qk, 3:131], scalar1=cw_t[:, qk * 4 + 3:qk * 4 + 4],
                                                                 scalar2=cb_t[:, qk:qk + 1], op0=ALU.mult, op1=ALU.add),
                         reads=[rw, cw_t, cb_t], writes=[acc])
                for dlt in range(1, 4):
                    p.dve.op(lambda e, qk=qk, rw=rw, dlt=dlt: e.scalar_tensor_tensor(
                        out=acc[:, qk, :], in0=rw[:, qk, 3 - dlt:131 - dlt], scalar=cw_t[:, qk * 4 + 3 - dlt:qk * 4 + 4 - dlt],
                        in1=acc[:, qk, :], op0=ALU.mult, op1=ALU.add), reads=[rw, cw_t, acc], writes=[acc])
            yield
            p.act.op(lambda e: e.activation(out=sg[:, :, :], in_=acc[:, :, :], func=AF.Exp, scale=-1.0), reads=[acc], writes=[sg])
            yield
            p.dve.op(lambda e: e.tensor_scalar(out=sg[:, :, :], in0=sg[:, :, :], scalar1=1.0, scalar2=None, op0=ALU.add), reads=[sg], writes=[sg])
            yield
            p.dve.op(lambda e: e.reciprocal(out=sg[:, :, :], in_=sg[:, :, :]), reads=[sg], writes=[sg])
            yield
            p.dve.op(lambda e: e.scalar_tensor_tensor(out=qT[:, :], in0=acc[:, 0, :], scalar=0.125, in1=sg[:, 0, :], op0=ALU.mult, op1=ALU.mult),
                     reads=[acc, sg], writes=[qT])
            yield
            p.dve.op(lambda e: e.tensor_tensor(out=kT[:, :], in0=acc[:, 1, :], in1=sg[:, 1, :], op=ALU.mult), reads=[acc, sg], writes=[kT])
            yield
            p.pool.op(lambda e: e.tensor_tensor(out=qbA[:, :], in0=qT[:, :], in1=MA2, op=ALU.mult), reads=[qT, cst], writes=[qbA])
            yield
            p.pool.op(lambda e: e.tensor_tensor(out=qbB[:, :], in0=qT[:, :], in1=MB2, op=ALU.mult), reads=[qT, cst], writes=[qbB])
            yield
            p.pe.op(lambda e: e.transpose(out=ps[5][:, 0:128], in_=kT[:, :], identity=p.ident_f[:, :]), reads=[kT, p.ident_f], writes=[ps[5]])
            yield
            p.act.op(lambda e: e.activation(out=kwA[:, :], in_=ps[5][:, 0:128], func=AF.Copy, scale=wAB[:, 0:1]), reads=[ps[5], wAB], writes=[kwA])
            yield
            p.dve.op(lambda e: e.tensor_scalar(out=kwB[:, :], in0=ps[5][:, 0:128], scalar1=wAB[:, 1:2], scalar2=None, op0=ALU.mult),
                     reads=[ps[5], wAB], writes=[kwB])
            yield

        def MIDDLE(T):
            qT = qT_l[T % 2]; kT = kT_l[T % 2]; qbA = qbA_l[T % 2]; qbB = qbB_l[T % 2]; kwA = kwA_l[T % 2]; kwB = kwB_l[T % 2]
            v1 = v1_l[T % 4]; F2 = F2_l[T % 3]; eb = eb_l[T % 3]; DT = DT_l[T % 3]; tot = tot_l[T % 2]
            p.pe.op(lambda e: e.matmul(out=ps[7][:, 130:258], lhsT=kT[:, :], rhs=qT[:, :], start=True, stop=True), reads=[kT, qT], writes=[ps[7]])
            yield
            p.dve.op(lambda e: e.scalar_tensor_tensor(out=wts[:, :], in0=ps[7][:, 130:258], scalar=0.5, in1=DT[:, :], op0=ALU.mult, op1=ALU.mult),
                     reads=[ps[7], DT], writes=[wts])
            yield
            p.pe.op(lambda e: e.matmul(out=ps[7][:, 0:129], lhsT=kwA[:, :], rhs=v1[:, :], start=True, stop=True), reads=[kwA, v1], writes=[ps[7]])
            yield
            p.dve.op(lambda e: e.scalar_tensor_tensor(out=SB[:, :], in0=SA[:, :], scalar=F2[:, 0:1], in1=ps[7][:, 0:129], op0=ALU.mult, op1=ALU.add),
                     reads=[SA, F2, ps[7]], writes=[SB])
            yield
            p.pe.op(lambda e: e.matmul(out=ps[6][:, 0:129], lhsT=wts[:, :], rhs=v1[:, :], start=True, stop=True), reads=[wts, v1], writes=[ps[6]])
            yield
            p.pe.op(lambda e: e.matmul(out=ps[6][:, 256:385], lhsT=qbA[:, :], rhs=SA[:, :], start=True, stop=False), reads=[qbA, SA], writes=[ps[6]])
            yield
            p.pe.op(lambda e: e.matmul(out=ps[6][:, 256:385], lhsT=qbB[:, :], rhs=SB[:, :], start=False, stop=True), reads=[qbB, SB], writes=[ps[6]])
            yield
            p.act.op(lambda e: e.copy(out=intra[:, :], in_=ps[6][:, 0:129]), reads=[ps[6]], writes=[intra])
            yield
            p.dve.op(lambda e: e.scalar_tensor_tensor(out=tot[:, :], in0=ps[6][:, 256:385], scalar=eb[:, 0:1], in1=intra[:, :], op0=ALU.mult, op1=ALU.add),
                     reads=[ps[6], eb, intra], writes=[tot])
            yield
            p.pe.op(lambda e: e.matmul(out=ps[7][:, 260:389], lhsT=kwB[:, :], rhs=v1[:, :], start=True, stop=True), reads=[kwB, v1], writes=[ps[7]])
            yield
            p.dve.op(lambda e: e.scalar_tensor_tensor(out=SA[:, :], in0=SB[:, :], scalar=F2[:, 1:2], in1=ps[7][:, 260:389], op0=ALU.mult, op1=ALU.add),
                     reads=[SB, F2, ps[7]], writes=[SA])
            yield

        def BACK(T):
            so = so_l[T % 5]; tot = tot_l[T % 2]; j = jB
            y = yt[T % 2]
            p.dve.op(lambda e: e.scalar_tensor_tensor(out=sm[:, 0:1], in0=tot[:, 128:129], scalar=-1.0, in1=tot[:, 128:129], op0=ALU.mult, op1=ALU.max), reads=[tot], writes=[sm])
            yield
            p.dve.op(lambda e: e.tensor_scalar(out=sm[:, 0:1], in0=sm[:, 0:1], scalar1=1.0, scalar2=None, op0=ALU.max), reads=[sm], writes=[sm])
            yield
            p.dve.op(lambda e: e.reciprocal(out=sm[:, 1:2], in_=sm[:, 0:1]), reads=[sm], writes=[sm])
            yield
            p.act.op(lambda e, j=j: e.activation(out=j[:, 0:128], in_=tot[:, 0:128], func=AF.Square, accum_out=sm[:, 2:3]), reads=[tot], writes=[j, sm])
            yield
            p.dve.op(lambda e: e.tensor_scalar(out=sm[:, 3:4], in0=sm[:, 1:2], scalar1=sm[:, 1:2], scalar2=1.0 / 128, op0=ALU.mult, op1=ALU.mult),
                     reads=[sm], writes=[sm])
            yield
            p.act.op(lambda e: e.activation(out=sm[:, 4:5], in_=sm[:, 2:3], func=AF.Ln, scale=sm[:, 3:4], bias=p.eps_t[:, 0:1]),
                     reads=[sm, p.eps_t], writes=[sm])
            yield
            p.act.op(lambda e: e.activation(out=sm[:, 4:5], in_=sm[:, 4:5], func=AF.Exp, scale=-0.5), reads=[sm], writes=[sm])
            yield
            p.dve.op(lambda e: e.tensor_tensor(out=sm[:, 5:6], in0=sm[:, 4:5], in1=sm[:, 1:2], op=ALU.mult), reads=[sm], writes=[sm])
            yield
            p.dve.op(lambda e, y=y: e.scalar_tensor_tensor(out=y[:, :], in0=tot[:, 0:128], scalar=sm[:, 5:6], in1=hn_bc[:, :], op0=ALU.mult, op1=ALU.mult),
                     reads=[tot, sm, hn_bc], writes=[y])
            yield
            p.pool.op(lambda e, y=y: e.tensor_tensor(out=y[:, :], in0=y[:, :], in1=so[:, :], op=ALU.mult), reads=[y, so], writes=[y])
            yield
            if yT_send is not None:
                yb = yTb[T % 2]
                p.pe.op(lambda e, y=y: e.transpose(out=ps[0][:, 0:128], in_=y[:, :], identity=p.ident_f[:, :]),
                        reads=[y, p.ident_f], writes=[ps[0]])
                p.act.op(lambda e, yb=yb: e.copy(out=yb[:, :], in_=ps[0][:, 0:128]), reads=[ps[0]], writes=[yb])
                c0 = (T % 8) * 2048 + (T // 8) * 128
                p.sp.dma(yT_send.t[:, c0:c0 + 128], yb[:, :], reads=[yb], writes=[yT_send])
            else:
                p.sp.dma(y_out[T * 128:(T + 1) * 128, :], y[:, :], reads=[y], writes=[y_out])

            yield
        for it in range(NTILE + 4):
            gens = []
            if 0 <= it - 4 < NTILE:
                gens.append(BACK(it - 4))
            if 0 <= it - 3 < NTILE:
                gens.append(MIDDLE(it - 3))
            if 0 <= it - 2 < NTILE:
                gens.append(F3(it - 2))
            if 0 <= it - 1 < NTILE:
                gens.append(F2S(it - 1))
            if it < NTILE:
                gens.append(F1(it))
            while gens:
                for g in list(gens):
                    try:
                        next(g)
                    except StopIteration:
                        gens.remove(g)
        p.barrier()

N_CORES = 8
S_TOK = 16384


def _make_masks(c):
    m = np.zeros((8, 128, 4, 128), np.float32)
    k = np.arange(128)[:, None]
    q = np.arange(128)[None, :]
    for j in range(8):
        if j == c:
            m[j] = np.where(k <= q, 0.0, -30000.0)[:, None, :]
        elif j > c:
            m[j] = -30000.0
    return m.reshape(8, 128, 512)


def build_A():
    p = Prog()
    with ExitStack() as st:
        setup_consts(p, st)
        di = lambda n, s, dt=F32: p.dram(n, s, dt, "ExternalInput")
        x_full = di("x_full", [S_TOK, 1024]); x_own = di("x_own", [2048, 1024])
        posT = di("posT", [128, 128], I32); pos_own = di("pos_own", [128, 16], I32)
        inv_freq = di("inv_freq", [32]); masks = di("masks", [8, 128, 512])
        g_mix0 = di("g_mix0", [1024]); w_in = di("w_in", [1024, 448]); q_norm = di("q_norm", [256])
        w_qb = di("w_qb", [256, 1536]); kv_norm = di("kv_norm", [128]); w_kvb = di("w_kvb", [128, 2048])
        w_out = di("w_out", [1024, 1024])
        g_ffn0 = di("g_ffn0", [1024]); fwg = di("fwg", [1, 1024, 3584]); fwu = di("fwu", [1, 1024, 3584]); fwd = di("fwd", [1, 3584, 1024])
        g_mix1 = di("g_mix1", [1024])
        x1_s = p.dram("x1_s", [2048, 1024], F32, "ExternalOutput")
        xnT_send = p.dram("xnT_send", [2048, 1024], BF16, "ExternalOutput")
        xa_s = p.dram_internal("xa_s", [2048, 1024], F32)
        phase_attn(p, x_full, x_own, posT, pos_own, inv_freq, masks, g_mix0, w_in, q_norm, w_qb, kv_norm, w_kvb, w_out, xa_s)
        phase_ffn(p, xa_s, x1_s, g_ffn0, fwg, fwu, fwd, 1)
        phase_norm_send(p, x1_s, g_mix1, xnT_send)
        nc = p.finish()
    return nc


def build_B():
    p = Prog()
    with ExitStack() as st:
        setup_consts(p, st)
        di = lambda n, s, dt=F32: p.dram(n, s, dt, "ExternalInput")
        xnT_all = di("xnT_all", [8 * 2048, 1024], BF16)
        g_mix1 = di("g_mix1", [1024]); w_h = di("w_h", [1024, 514]); cw = di("cw", [128, 8]); cb = di("cb", [128, 2])
        gb = di("gate_b", [2]); hn = di("head_norm", [128]); cs = di("consts", [128, NCONST])
        yT_send = p.dram("yT_send", [128, S_TOK], BF16, "ExternalOutput")
        phase_mlstm(p, None, g_mix1, w_h, cw, cb, gb, hn, cs, None, xnT_all=xnT_all, yT_send=yT_send)
        nc = p.finish()
    return nc


def build_C():
    p = Prog()
    with ExitStack() as st:
        setup_consts(p, st)
        di = lambda n, s, dt=F32: p.dram(n, s, dt, "ExternalInput")
        x1_s = di("x1_s", [2048, 1024]); yT_all = di("yT_all", [8 * 128, S_TOK], BF16); m_wo = di("m_wo", [1024, 1024])
        g_ffn1 = di("g_ffn1", [1024]); mwg = di("mwg", [8, 1024, 3584]); mwu = di("mwu", [8, 1024, 3584]); mwd = di("mwd", [8, 3584, 1024])
        rt = di("rt", [8, 1024]); gf = di("gf", [1024])
        xout = p.dram("xout", [2048, 1024], F32, "ExternalOutput")
        phase_ffn(p, x1_s, xout, g_ffn1, mwg, mwu, mwd, 8, routerT=rt, final_g=gf, ymT=yT_all, w_o=m_wo, ymT_dyn=True)
        nc = p.finish()
    return nc


def make_maps(inp):
    x = np.ascontiguousarray(inp["x"][0].astype(np.float32, copy=False))
    xt = x.reshape(128, 128, 1024)
    post = np.ascontiguousarray(inp["positions"][0].astype(np.int32)).reshape(128, 128)
    inv = (1.0 / (10000.0 ** (np.arange(0, 64, 2, dtype=np.float32) / 64))).astype(np.float32)
    w_in = inp["mlstm_w_in"][0]; conv_w = inp["mlstm_conv_w"][0]; conv_b = inp["mlstm_conv_b"][0]
    gate_b = inp["mlstm_gate_b"][0]; head_norm = inp["mlstm_head_norm"][0]
    consts = mlstm_consts_np()
    posT = np.ascontiguousarray(post.T)
    rtT = np.ascontiguousarray(inp["moe_router"][0].T)
    maps = []
    for c in range(N_CORES):
        own = [8 * i + c for i in range(16)]
        h = c
        q = w_in[:, h * 64:(h + 1) * 64]; k = w_in[:, 512 + h * 64:512 + (h + 1) * 64]
        v = w_in[:, 1024 + h * 128:1024 + (h + 1) * 128]; o = w_in[:, 2048 + h * 128:2048 + (h + 1) * 128]
        ig = w_in[:, 3072 + h:3073 + h]; fg = w_in[:, 3080 + h:3081 + h]
        wh = np.ascontiguousarray(np.concatenate([q, q, k, k, v, o, ig, fg], axis=1))
        cq = conv_w[:, h * 64:(h + 1) * 64].T; ck = conv_w[:, 512 + h * 64:512 + (h + 1) * 64].T
        cwh = np.ascontiguousarray(np.concatenate([np.concatenate([cq, cq], 0), np.concatenate([ck, ck], 0)], axis=1))
        bq = conv_b[h * 64:(h + 1) * 64]; bk = conv_b[512 + h * 64:512 + (h + 1) * 64]
        cbh = np.ascontiguousarray(np.stack([np.concatenate([bq, bq]), np.concatenate([bk, bk])], axis=1))
        maps.append({
            "x_full": x, "x_own": np.ascontiguousarray(xt[own].reshape(-1, 1024)),
            "posT": posT, "pos_own": np.ascontiguousarray(post[own].T), "inv_freq": inv, "masks": _make_masks(c),
            "g_mix0": inp["norm_mix"][0], "w_in": inp["mla_w_in"][0], "q_norm": inp["mla_q_norm"][0], "w_qb": inp["mla_w_qb"][0],
            "kv_norm": inp["mla_kv_norm"][0], "w_kvb": inp["mla_w_kvb"][0], "w_out": inp["mla_w_out"][0],
            "g_ffn0": inp["norm_ffn"][0], "fwg": inp["ffn_w_gate"], "fwu": inp["ffn_w_up"], "fwd": inp["ffn_w_down"],
            "g_mix1": inp["norm_mix"][1], "w_h": wh, "cw": cwh, "cb": cbh,
            "gate_b": np.ascontiguousarray(np.stack([gate_b[h], gate_b[8 + h]])),
            "head_norm": np.ascontiguousarray(head_norm[h * 128:(h + 1) * 128]), "consts": consts, "m_wo": inp["mlstm_w_out"][0],
            "g_ffn1": inp["norm_ffn"][1], "mwg": inp["moe_w_gate"][0], "mwu": inp["moe_w_up"][0], "mwd": inp["moe_w_down"][0],
            "rt": rtT, "gf": inp["final_norm"],
        })
    return maps


def kernel(**inputs):
    inp = {k: np.asarray(v) for k, v in inputs.items()}
    maps = make_maps(inp)
    ka = ["x_full", "x_own", "posT", "pos_own", "inv_freq", "masks", "g_mix0", "w_in", "q_norm", "w_qb", "kv_norm", "w_kvb", "w_out",
          "g_ffn0", "fwg", "fwu", "fwd", "g_mix1"]
    ra = run_bass_kernel_spmd(build_A(), [{k: m[k] for k in ka} for m in maps], core_ids=list(range(N_CORES))).results
    xnT_all = np.concatenate([ra[c]["xnT_send"] for c in range(N_CORES)], axis=0)
    kb = ["g_mix1", "w_h", "cw", "cb", "gate_b", "head_norm", "consts"]
    rb = run_bass_kernel_spmd(build_B(), [dict({k: m[k] for k in kb}, xnT_all=xnT_all) for m in maps], core_ids=list(range(N_CORES))).results
    yT_all = np.concatenate([rb[c]["yT_send"] for c in range(N_CORES)], axis=0)
    kc = ["m_wo", "g_ffn1", "mwg", "mwu", "mwd", "rt", "gf"]
    rc = run_bass_kernel_spmd(build_C(), [dict({k: m[k] for k in kc}, x1_s=ra[c]["x1_s"], yT_all=yT_all) for c, m in enumerate(maps)],
                              core_ids=list(range(N_CORES))).results
    out = np.empty((128, 128, 1024), np.float32)
    for c in range(N_CORES):
        own = [8 * i + c for i in range(16)]
        out[own] = rc[c]["xout"].reshape(16, 128, 1024)
    return out.reshape(1, S_TOK, 1024)
```
